# Optimizing a Trainium2 kernel written in Bass

```python
import jax, jax.numpy as jnp
from jax import lax
import numpy as np

D_MODEL = 2048
BATCH = 2
SEQ = 16384
DEPTH = 1

GRID_W = 64
CTX_LEN = 256

GLA_HEADS = 4
GLA_DK = 128
GLA_DV = 256
GLA_KEY_WIDTH = GLA_HEADS * GLA_DK
GLA_WIDTH = GLA_HEADS * GLA_DV
GLA_GATE_RANK = 16
GLA_GATE_TAU = 16.0
GLA_CHUNK = 64

CMLP_GROUPS = 8
CMLP_GROUP_DIM = 128
CMLP_WIDTH = CMLP_GROUPS * CMLP_GROUP_DIM
CMLP_CHUNK = 128
ROWS_PER_CHUNK = CMLP_CHUNK // GRID_W

MIX_WIDTH = GLA_WIDTH + CMLP_WIDTH

IN_SIZES = (GLA_KEY_WIDTH, GLA_KEY_WIDTH, GLA_WIDTH, GLA_WIDTH, 2 * GLA_GATE_RANK, CMLP_WIDTH, CMLP_WIDTH)
IN_WIDTH = sum(IN_SIZES)
IN_OFFSETS = tuple(int(o) for o in np.cumsum(IN_SIZES)[:-1])

PEER_HEADS = 8
PEER_NKEYS = 128
PEER_EXPERTS = PEER_NKEYS * PEER_NKEYS
PEER_QDIM = 256
PEER_HALF = PEER_QDIM // 2
PEER_TOPK = 16
PEER_BLOCK = 128

N_MOD = 6
EPS = 1e-6

kernel_name = "hybrid_gla_chunkmlp_peer_dit_block"


def rmsnorm(x, g):
    xf = x.astype(jnp.float32)
    y = xf * lax.rsqrt(jnp.mean(xf * xf, axis=-1, keepdims=True) + EPS)
    return (y * g.astype(jnp.float32)).astype(x.dtype)


def modulate(x, shift, scale):
    return x * (1.0 + scale) + shift


def gla_scan(q, k, v, log_a, s0):
    b, l, h, _ = q.shape
    n = l // GLA_CHUNK

    def to_chunks(t):
        return t.astype(jnp.float32).reshape(b, n, GLA_CHUNK, h, t.shape[-1]).transpose(1, 0, 3, 2, 4)

    prefix_mask = jnp.tril(jnp.ones((GLA_CHUNK, GLA_CHUNK), dtype=bool))

    def step(state, inp):
        qc, kc, vc, ac = inp
        cum = jnp.cumsum(ac, axis=2)
        tot = cum[:, :, -1:, :]
        q_dec = qc * jnp.exp(cum)
        scores = jnp.einsum('bhid,bhjd->bhij', q_dec, kc * jnp.exp(-cum))
        scores = jnp.where(prefix_mask, scores, 0.0)
        out = (jnp.einsum('bhij,bhje->bhie', scores, vc)
               + jnp.einsum('bhid,bhde->bhie', q_dec, state))
        k_dec = kc * jnp.exp(tot - cum)
        state = (jnp.exp(tot[:, :, 0, :])[..., None] * state
                 + jnp.einsum('bhjd,bhje->bhde', k_dec, vc))
        return state, out

    state, out = lax.scan(step, s0, (to_chunks(q), to_chunks(k), to_chunks(v), to_chunks(log_a)))
    out = out.transpose(1, 0, 3, 2, 4).reshape(b, l, h, v.shape[-1])
    return out, state


def gla_heads(parts, w_gate_up, b_gate):
    q, k, v, g, lr = parts
    b, l, _ = q.shape
    q = q.reshape(b, l, GLA_HEADS, GLA_DK) * (GLA_DK ** -0.5)
    k = k.reshape(b, l, GLA_HEADS, GLA_DK)
    v = v.reshape(b, l, GLA_HEADS, GLA_DV)

    def log_decay(d):
        z = lr[..., d * GLA_GATE_RANK:(d + 1) * GLA_GATE_RANK] @ w_gate_up[d] + b_gate[d]
        return (jax.nn.log_sigmoid(z.astype(jnp.float32)) / GLA_GATE_TAU).reshape(b, l, GLA_HEADS, GLA_DK)

    return q, k, v, log_decay(0), log_decay(1), g


def gla_out(o, g, norm_g):
    b, l = o.shape[:2]
    o = o * lax.rsqrt(jnp.mean(o * o, axis=-1, keepdims=True) + EPS)
    o = o.reshape(b, l, GLA_WIDTH) * norm_g.astype(jnp.float32)
    return (o * jax.nn.silu(g.astype(jnp.float32))).astype(g.dtype)


def gla_mixer(lat_parts, ctx_parts, w_gate_up, b_gate, norm_g, need_ctx):
    ql, kl, vl, afl, abl, gl = gla_heads(lat_parts, w_gate_up, b_gate)
    qc, kc, vc, afc, abc, gc = gla_heads(ctx_parts, w_gate_up, b_gate)
    flip = lambda t: t[:, ::-1]
    s0 = jnp.zeros((qc.shape[0], GLA_HEADS, GLA_DK, GLA_DV), jnp.float32)
    o_cf, s_f = gla_scan(qc, kc, vc, afc, s0)
    o_cb, s_b = gla_scan(flip(qc), flip(kc), flip(vc), flip(abc), s0)
    o_lf, _ = gla_scan(ql, kl, vl, afl, s_f)
    o_lb, _ = gla_scan(flip(ql), flip(kl), flip(vl), flip(abl), s_b)
    y_lat = gla_out(o_lf + flip(o_lb), gl, norm_g)
    y_ctx = gla_out(o_cf + flip(o_cb), gc, norm_g) if need_ctx else None
    return y_lat, y_ctx


def chunk_mlp(u, v, ln_g, ln_b, w_s, b_s, n_chunks):
    b, l, _ = u.shape
    u = jax.nn.gelu(u, approximate=False)
    vf = jax.nn.gelu(v.astype(jnp.float32), approximate=False)
    mu = jnp.mean(vf, axis=-1, keepdims=True)
    var = jnp.mean(jnp.square(vf - mu), axis=-1, keepdims=True)
    vn = ((vf - mu) * lax.rsqrt(var + EPS) * ln_g.astype(jnp.float32) + ln_b.astype(jnp.float32)).astype(u.dtype)
    vn = vn.reshape(b, n_chunks, CMLP_CHUNK, CMLP_GROUPS, CMLP_GROUP_DIM)
    s = jnp.einsum('gpq,bnqgc->bnpgc', w_s, vn) + b_s.T[:, :, None]
    return u * s.reshape(b, l, CMLP_WIDTH)


def peer_ffn(xn, w_q, sub_keys, exp_u, exp_v):
    b, l, d = xn.shape
    blocks = xn.reshape(-1, PEER_BLOCK, d)

    def block(xb):
        p = xb.shape[0]
        q = (xb @ w_q).reshape(p, PEER_HEADS, 2, PEER_HALF)
        s1 = jnp.einsum('phd,hkd->phk', q[:, :, 0], sub_keys[0])
        s2 = jnp.einsum('phd,hkd->phk', q[:, :, 1], sub_keys[1])
        v1, i1 = lax.top_k(s1, PEER_TOPK)
        v2, i2 = lax.top_k(s2, PEER_TOPK)
        cand = (v1[..., :, None] + v2[..., None, :]).reshape(p, PEER_HEADS, PEER_TOPK * PEER_TOPK)
        cidx = (i1[..., :, None] * PEER_NKEYS + i2[..., None, :]).reshape(p, PEER_HEADS, PEER_TOPK * PEER_TOPK)
        best, pos = lax.top_k(cand, PEER_TOPK)
        eidx = jnp.take_along_axis(cidx, pos, axis=-1)
        gate = jax.nn.softmax(best.astype(jnp.float32), axis=-1).astype(xb.dtype)
        u = exp_u[eidx]
        act = jax.nn.gelu(jnp.einsum('pd,phkd->phk', xb, u), approximate=False)
        return jnp.einsum('phk,phkd->pd', gate * act, exp_v[eidx])

    return lax.map(block, blocks).reshape(b, l, d)


def setup_inputs(seed: int = 0) -> dict:
    key = jax.random.key(seed)
    ks = jax.random.split(key, 24)
    f32 = jnp.float32
    nrm = lambda k, shape, s: jax.random.normal(k, shape, f32) * s
    D = D_MODEL
    return {
        "x": nrm(ks[0], (BATCH, SEQ, D), 1.0),
        "c": nrm(ks[1], (BATCH, D), 1.0),
        "ctx": nrm(ks[2], (BATCH, CTX_LEN, D), 1.0),
        "c_ctx": nrm(ks[3], (D,), 1.0),
        "norm1_g": 1.0 + nrm(ks[4], (DEPTH, D), 0.02),
        "norm2_g": 1.0 + nrm(ks[5], (DEPTH, D), 0.02),
        "w_mod": nrm(ks[6], (DEPTH, D, N_MOD * D), D ** -0.5),
        "b_mod": nrm(ks[7], (DEPTH, N_MOD * D), 0.02),
        "w_in": nrm(ks[8], (DEPTH, D, IN_WIDTH), D ** -0.5),
        "w_gate_up": nrm(ks[9], (DEPTH, 2, GLA_GATE_RANK, GLA_KEY_WIDTH), GLA_GATE_RANK ** -0.5),
        "b_gate": nrm(ks[10], (DEPTH, 2, GLA_KEY_WIDTH), 0.1),
        "gla_norm_g": 1.0 + nrm(ks[11], (DEPTH, GLA_WIDTH), 0.02),
        "cmlp_ln_g": 1.0 + nrm(ks[12], (DEPTH, CMLP_WIDTH), 0.02),
        "cmlp_ln_b": nrm(ks[13], (DEPTH, CMLP_WIDTH), 0.02),
        "w_spatial": nrm(ks[14], (DEPTH, CMLP_GROUPS, CMLP_CHUNK, CMLP_CHUNK), CMLP_CHUNK ** -0.5),
        "b_spatial": nrm(ks[15], (DEPTH, CMLP_GROUPS, CMLP_CHUNK), 0.02),
        "w_out": nrm(ks[16], (DEPTH, MIX_WIDTH, D), MIX_WIDTH ** -0.5),
        "peer_wq": nrm(ks[17], (DEPTH, D, PEER_HEADS * PEER_QDIM), D ** -0.5),
        "peer_sub_keys": nrm(ks[18], (DEPTH, 2, PEER_HEADS, PEER_NKEYS, PEER_HALF), PEER_HALF ** -0.5),
        "peer_u": nrm(ks[19], (DEPTH, PEER_EXPERTS, D), D ** -0.5),
        "peer_v": nrm(ks[20], (DEPTH, PEER_EXPERTS, D), PEER_HEADS ** -0.5),
        "final_norm_g": 1.0 + nrm(ks[21], (D,), 0.02),
    }


def reference(x, c, ctx, c_ctx, norm1_g, norm2_g, w_mod, b_mod, w_in, w_gate_up, b_gate,
              gla_norm_g, cmlp_ln_g, cmlp_ln_b, w_spatial, b_spatial, w_out,
              peer_wq, peer_sub_keys, peer_u, peer_v, final_norm_g):
    rows = x.shape[1] // GRID_W
    lat_chunks = rows // ROWS_PER_CHUNK
    ctx_chunks = ctx.shape[1] // CMLP_CHUNK
    silu_c = jax.nn.silu(c)
    silu_cc = jax.nn.silu(c_ctx)
    h, hc = x, ctx
    for i in range(DEPTH):
        need_ctx = i + 1 < DEPTH
        m = silu_c @ w_mod[i] + b_mod[i]
        mc = silu_cc @ w_mod[i] + b_mod[i]
        sh1, sc1, g1, sh2, sc2, g2 = jnp.split(m[:, None, :], N_MOD, axis=-1)
        csh1, csc1, cg1, csh2, csc2, cg2 = jnp.split(mc, N_MOD, axis=-1)

        p_lat = jnp.split(modulate(rmsnorm(h, norm1_g[i]), sh1, sc1) @ w_in[i], IN_OFFSETS, axis=-1)
        p_ctx = jnp.split(modulate(rmsnorm(hc, norm1_g[i]), csh1, csc1) @ w_in[i], IN_OFFSETS, axis=-1)
        gla_lat, gla_ctx = gla_mixer(p_lat[:5], p_ctx[:5], w_gate_up[i], b_gate[i], gla_norm_g[i], need_ctx)
        cm_lat = chunk_mlp(p_lat[5], p_lat[6], cmlp_ln_g[i], cmlp_ln_b[i], w_spatial[i], b_spatial[i], lat_chunks)
        h = h + g1 * (jnp.concatenate([gla_lat, cm_lat], axis=-1) @ w_out[i])

        h = h + g2 * peer_ffn(modulate(rmsnorm(h, norm2_g[i]), sh2, sc2),
                              peer_wq[i], peer_sub_keys[i], peer_u[i], peer_v[i])

        if need_ctx:
            cm_ctx = chunk_mlp(p_ctx[5], p_ctx[6], cmlp_ln_g[i], cmlp_ln_b[i], w_spatial[i], b_spatial[i], ctx_chunks)
            hc = hc + cg1 * (jnp.concatenate([gla_ctx, cm_ctx], axis=-1) @ w_out[i])
            hc = hc + cg2 * peer_ffn(modulate(rmsnorm(hc, norm2_g[i]), csh2, csc2),
                                     peer_wq[i], peer_sub_keys[i], peer_u[i], peer_v[i])
    return rmsnorm(h, final_norm_g)
```

```python
import numpy as np
import concourse.bass as bass
import concourse.mybir as mybir
from concourse.bass_utils import run_bass_kernel_spmd

F32 = mybir.dt.float32
BF16 = mybir.dt.bfloat16
ALU = mybir.AluOpType
AF = mybir.ActivationFunctionType
AX = mybir.AxisListType


class Tl:
    __slots__ = ("ap", "name", "w", "r")

    def __init__(self, ap, name=""):
        self.ap = ap
        self.name = name
        self.w = None
        self.r = {}

    def __getitem__(self, k):
        return self.ap[k]


class Op:
    __slots__ = ("idx", "eng", "fn", "deps", "dma_key", "val", "signal", "dma_waits")


class FW:
    ENGS = ("pe", "act", "dve", "pool", "sp")

    def __init__(self, nc):
        self.nc = nc
        self.ops = []
        self.dma_cnt = {}

    def op(self, eng, fn, reads=(), writes=(), dma_key=None):
        o = Op()
        o.idx = len(self.ops)
        o.eng = eng
        o.fn = fn
        o.dma_key = dma_key
        o.signal = False
        o.val = 0
        deps = set()
        for t in reads:
            if t.w is not None:
                deps.add(t.w)
        for t in writes:
            if t.w is not None:
                deps.add(t.w)
            deps.update(t.r.values())
        o.deps = deps
        o.dma_waits = {}
        for d in deps:
            od = self.ops[d]
            if od.dma_key is not None:
                o.dma_waits[od.dma_key] = self.dma_cnt[od.dma_key]
        if dma_key is not None:
            self.dma_cnt[dma_key] = self.dma_cnt.get(dma_key, 0) + 16
            o.val = self.dma_cnt[dma_key]
        rkey = ("dma", dma_key) if dma_key is not None else eng
        for t in reads:
            t.r[rkey] = o.idx
        for t in writes:
            t.w = o.idx
            t.r = {}
        self.ops.append(o)
        return o

    def emit(self, stack):
        nc = self.nc
        ops = self.ops
        for o in ops:
            for d in o.deps:
                if ops[d].dma_key is None:
                    ops[d].signal = True
        cnt = {e: 0 for e in self.ENGS}
        for o in ops:
            if o.dma_key is None and o.signal:
                cnt[o.eng] += 1
                o.val = cnt[o.eng]
        esem = {e: stack.enter_context(nc.semaphore("s_" + e)) for e in self.ENGS}
        dsem = {k: stack.enter_context(nc.semaphore("d_%d" % i))
                for i, k in enumerate(self.dma_cnt)}
        streams = {e: [o for o in ops if o.eng == e] for e in self.ENGS}

        def run(eng_name, e):
            waited = {}
            for o in streams[eng_name]:
                waits = {}
                for d in o.deps:
                    od = ops[d]
                    if od.dma_key is not None:
                        key = ("d", od.dma_key)
                        v = o.dma_waits[od.dma_key]
                    else:
                        if od.eng == eng_name and eng_name == "pe":
                            continue
                        key = ("e", od.eng)
                        v = od.val
                    if waits.get(key, 0) < v:
                        waits[key] = v
                for key, v in waits.items():
                    if waited.get(key, 0) >= v:
                        continue
                    s = dsem[key[1]] if key[0] == "d" else esem[key[1]]
                    e.wait_ge(s, v)
                    waited[key] = v
                ins = o.fn(e)
                if o.dma_key is not None:
                    ins.then_inc(dsem[o.dma_key], 16)
                elif o.signal:
                    ins.then_inc(esem[eng_name], 1)

        block = stack.enter_context(nc.Block())

        @block.tensor
        def _(e):
            run("pe", e)

        @block.scalar
        def _(e):
            run("act", e)

        @block.vector
        def _(e):
            run("dve", e)

        @block.gpsimd
        def _(e):
            run("pool", e)

        @block.sync
        def _(e):
            run("sp", e)

    def barrier(self):
        last = {}
        for o in self.ops:
            last[o.eng if o.dma_key is None else ("d", o.dma_key)] = o.idx
        deps = set(last.values())
        for e in self.ENGS:
            o = self.op(e, lambda en: en.nop())
            o.deps |= deps
            for d in deps:
                k = self.ops[d].dma_key
                if k is not None:
                    o.dma_waits[k] = self.dma_cnt[k]


D = 2048
KC = 16
NE = 16384
EPS = 1e-6
IN_W = 5152


class Cfg:
    def __init__(self, seq, ctx=256):
        self.SEQ = seq
        self.CTX = ctx
        self.TPC = seq // 4
        self.NB = self.TPC // 128
        self.TS = min(4, self.NB)


class Arena:
    def __init__(self, ap, words):
        self.ap = ap
        self.ptr = 0
        self.words = words

    def f32(self, n, name=""):
        off = self.ptr
        self.ptr += n
        assert self.ptr <= self.words, (name, self.ptr, self.words)
        return Tl(self.ap[:, off:off + n], name)

    def bf16(self, n, name=""):
        w = (n + 1) // 2
        off = self.ptr
        self.ptr += w
        assert self.ptr <= self.words, (name, self.ptr, self.words)
        return Tl(self.ap[:, off:off + w].bitcast(BF16), name)


def v3(ap, a):
    return ap.rearrange("p (a b) -> p a b", a=a)


class KB:
    def __init__(self, nc, fw):
        self.nc = nc
        self.fw = fw

    def dma(self, q, out_ap, in_ap, key, reads=(), writes=(), nc_ok=False):
        nc = self.nc

        def fn(e):
            if nc_ok:
                with nc.allow_non_contiguous_dma(reason="small strided load"):
                    return e.dma_start(out=out_ap, in_=in_ap)
            return e.dma_start(out=out_ap, in_=in_ap)
        return self.fw.op(q, fn, reads, writes, dma_key=key)

    def act(self, out, in_, func, reads, writes, **kw):
        return self.fw.op("act", lambda e: e.activation(out=out, in_=in_, func=func, **kw), reads, writes)

    def acts(self, specs, reads, writes):
        def fn(e):
            ins = None
            for (out, in_, func, kw) in specs:
                ins = e.activation(out=out, in_=in_, func=func, **kw)
            return ins
        return self.fw.op("act", fn, reads, writes)

    def tt(self, eng, out, in0, in1, op, reads, writes):
        return self.fw.op(eng, lambda e: e.tensor_tensor(out=out, in0=in0, in1=in1, op=op), reads, writes)

    def ts(self, eng, out, in0, s1, s2, op0, op1, reads, writes):
        if s2 is None:
            return self.fw.op(eng, lambda e: e.tensor_scalar(out=out, in0=in0, scalar1=s1, scalar2=None, op0=op0), reads, writes)
        return self.fw.op(eng, lambda e: e.tensor_scalar(out=out, in0=in0, scalar1=s1, scalar2=s2, op0=op0, op1=op1), reads, writes)

    def stt(self, eng, out, in0, scalar, in1, op0, op1, reads, writes):
        return self.fw.op(eng, lambda e: e.scalar_tensor_tensor(out=out, in0=in0, scalar=scalar, in1=in1, op0=op0, op1=op1), reads, writes)

    def stts(self, eng, specs, reads, writes):
        def fn(e):
            ins = None
            for (out, in0, scalar, in1, op0, op1) in specs:
                ins = e.scalar_tensor_tensor(out=out, in0=in0, scalar=scalar, in1=in1, op0=op0, op1=op1)
            return ins
        return self.fw.op(eng, fn, reads, writes)

    def copy(self, eng, out, in_, reads, writes):
        if eng == "act":
            return self.fw.op("act", lambda e: e.activation(out=out, in_=in_, func=AF.Copy), reads, writes)
        return self.fw.op(eng, lambda e: e.tensor_copy(out=out, in_=in_), reads, writes)

    def memset(self, eng, out, val, writes):
        return self.fw.op(eng, lambda e: e.memset(out, val), (), writes)

    def mm(self, specs, reads, writes):
        def fn(e):
            ins = None
            for (out, lhsT, rhs, start, stop) in specs:
                ins = e.matmul(out, lhsT=lhsT, rhs=rhs, start=start, stop=stop)
            return ins
        return self.fw.op("pe", fn, reads, writes)

    def tr(self, specs, reads, writes):
        def fn(e):
            ins = None
            for (out, in_, ident) in specs:
                ins = e.transpose(out=out, in_=in_, identity=ident)
            return ins
        return self.fw.op("pe", fn, reads, writes)

    def reduce(self, eng, out, in_, axis, op, reads, writes):
        nc = self.nc

        def fn(e):
            with nc.allow_low_precision(reason="bf16 gate sum feeds a bf16 matmul operand"):
                return e.tensor_reduce(out=out, in_=in_, axis=axis, op=op)
        return self.fw.op(eng, fn, reads, writes)

    def recip(self, out, in_, reads, writes):
        return self.fw.op("dve", lambda e: e.reciprocal(out=out, in_=in_), reads, writes)

    def rstd(self, out, ss, n, tmp, reads, writes):
        self.ts("dve", tmp, ss, 1.0 / n, EPS, ALU.mult, ALU.add, reads, writes)
        self.act(tmp, tmp, AF.Sqrt, writes, writes)
        self.recip(out, tmp, writes, writes)


AW = 53200


def build_program(cfg, debug=False):
    from contextlib import ExitStack
    NB, TPC, TS = cfg.NB, cfg.TPC, cfg.TS
    nc = bass.Bass("TRN2", target_bir_lowering=False)

    def din(name, shape, dt=F32):
        return nc.dram_tensor(name, list(shape), dt, kind="ExternalInput").ap()

    def dscr(name, shape, dt):
        return nc.dram_tensor(name, list(shape), dt, kind="ExternalOutput" if debug else "Internal").ap()

    xm = din("xm", [TPC, D])
    xo = din("xo", [3 * TPC, D])
    ctxb = din("ctxb", [cfg.CTX, D])
    cvec = din("cvec", [D])
    cctx = din("cctx", [D])
    flags = din("flags", [128, 8])
    norm1_g = din("norm1_g", [D])
    norm2_g = din("norm2_g", [D])
    w_mod = din("w_mod", [D, 6 * D])
    b_mod = din("b_mod", [6 * D])
    w_in = din("w_in", [D, IN_W])
    w_gate_up = din("w_gate_up", [2, 16, 512])
    b_gate = din("b_gate", [2, 512])
    gla_norm_g = din("gla_norm_g", [1024])
    cmlp_ln_g = din("cmlp_ln_g", [1024])
    cmlp_ln_b = din("cmlp_ln_b", [1024])
    w_spatial = din("w_spatial", [8, 128, 128])
    b_spatial = din("b_spatial", [8, 128])
    w_out = din("w_out", [D, D])
    peer_wq = din("peer_wq", [D, D])
    peer_sk = din("peer_sk", [2, 8, 128, 128])
    peer_u = din("peer_u", [NE, D])
    peer_v = din("peer_v", [NE, D])
    final_g = din("final_g", [D])
    y = nc.dram_tensor("y", [TPC, D], F32, kind="ExternalOutput").ap()

    of_d = dscr("of_d", [TPC, 1024], F32)
    mix_d = dscr("mix_d", [TPC, D], BF16)
    h1_d = dscr("h1_d", [TPC, D], F32)
    xn2T_d = dscr("xn2T_d", [NB, 128, D], BF16)
    st_nega = dscr("st_nega", [NB, 128, 1024], F32)
    st_s2 = dscr("st_s2", [NB, 128, 1024], F32)
    st_ea = dscr("st_ea", [NB, 128, 1024], BF16)
    st_eb = dscr("st_eb", [NB, 128, 1024], BF16)
    UT_d = dscr("UT_d", [32, 128, 8192], BF16)
    V_d = dscr("V_d", [NE, D], BF16)

    st = ExitStack()
    with st:
        fw = FW(nc)
        kb = KB(nc, fw)
        arena_t = st.enter_context(nc.sbuf_tensor("arena", [128, AW], F32))
        psum_t = st.enter_context(nc.psum_tensor("psum", [128, 4096], F32))
        ar = Arena(arena_t, AW)
        PB = [Tl(psum_t[:, b * 512:(b + 1) * 512], "B%d" % b) for b in range(8)]

        def pf(b0, nb=1):
            return psum_t[:, b0 * 512:(b0 + nb) * 512]

        def pbf(b0, nb=1):
            return psum_t[:, b0 * 512:(b0 + nb) * 512].bitcast(BF16)

        ident_f = ar.f32(128, "ident_f")
        A_le = ar.f32(128, "A_le")
        A_ge = ar.f32(128, "A_ge")
        A_gt = ar.f32(128, "A_gt")
        A_lt = ar.f32(128, "A_lt")
        ident_b = ar.bf16(128, "ident_b")
        cols = ar.f32(16 * 8, "cols")
        fl = ar.f32(8, "fl")
        fl2 = ar.f32(24, "fl2")
        g2_bc = ar.f32(D, "g2_bc")

        def tri(tl, cmp, sign):
            kb.memset("pool", tl.ap, 0.0, [tl])
            fw.op("pool", lambda e: e.affine_select(out=tl.ap, in_=tl.ap, pattern=[[-sign, 128]], compare_op=cmp,
                                                    fill=1.0, base=0, channel_multiplier=sign), [tl], [tl])
        tri(ident_f, ALU.not_equal, 1)
        tri(A_le, ALU.is_gt, 1)
        tri(A_ge, ALU.is_gt, -1)
        kb.tt("dve", A_gt.ap, A_ge.ap, ident_f.ap, ALU.subtract, [A_ge, ident_f], [A_gt])
        kb.tt("dve", A_lt.ap, A_le.ap, ident_f.ap, ALU.subtract, [A_le, ident_f], [A_lt])
        kb.copy("dve", ident_b.ap, ident_f.ap, [ident_f], [ident_b])
        kb.dma("sp", fl.ap, flags[:, :], "misc", (), [fl])
        kb.ts("dve", fl2[:, 0:8], fl.ap, -1.0 / 16.0, None, ALU.mult, None, [fl], [fl2])
        kb.ts("dve", fl2[:, 16:24], fl.ap, -1.0, 1.0, ALU.mult, ALU.add, [fl], [fl2])
        kb.ts("dve", fl2[:, 8:16], fl2[:, 16:24], -1.0 / 16.0, None, ALU.mult, None, [fl2], [fl2])
        colv = v3(cols.ap, 8)
        C_G1, C_SH1, C_CG1, C_CSH1, C_G2, C_SH2, C_N1, C_N2 = range(8)
        const_end_p6 = ar.ptr
        g1_bc = ar.f32(D, "g1_bc")
        const_end = ar.ptr

        M = ar.f32(6 * D, "M")
        Mc = ar.f32(2 * D, "Mc")
        rep = ar.f32(2 * KC * 128, "rep")
        ccol = ar.f32(32, "ccol")
        wb = [ar.f32(KC * 512, "wb0"), ar.f32(KC * 512, "wb1")]
        repv = rep.ap.rearrange("p (t k m) -> p t k m", t=2, k=KC)
        kb.dma("sp", ccol[:, 0:16], cvec.rearrange("(k p) -> p k", p=128), "misc", (), [ccol], nc_ok=True)
        kb.dma("sp", ccol[:, 16:32], cctx.rearrange("(k p) -> p k", p=128), "misc", (), [ccol], nc_ok=True)
        kb.dma("sp", colv[:, C_N1, :], norm1_g.rearrange("(k p) -> p k", p=128), "misc", (), [cols], nc_ok=True)
        kb.dma("sp", colv[:, C_N2, :], norm2_g.rearrange("(k p) -> p k", p=128), "misc", (), [cols], nc_ok=True)
        kb.dma("sp", M.ap, b_mod.partition_broadcast(128), "misc", (), [M])
        kb.dma("sp", Mc.ap, b_mod[0:2 * D].partition_broadcast(128), "misc", (), [Mc])
        kb.act(ccol.ap, ccol.ap, AF.Silu, [ccol], [ccol])
        kb.copy("dve", repv[:, 0], ccol[:, 0:16].unsqueeze(2).to_broadcast([128, KC, 128]), [ccol], [rep])
        kb.copy("dve", repv[:, 1], ccol[:, 16:32].unsqueeze(2).to_broadcast([128, KC, 128]), [ccol], [rep])
        wmv = w_mod.rearrange("(k p) n -> p k n", p=128)
        for cb in range(24):
            w = wb[cb % 2]
            kb.dma("sp", v3(w.ap, KC), wmv[:, :, cb * 512:(cb + 1) * 512], "wb%d" % (cb % 2), (), [w])
            bk = cb % 2
            kb.mm([(pf(bk), repv[:, 0, k, :], v3(w.ap, KC)[:, k, :], k == 0, k == KC - 1) for k in range(KC)],
                  [rep, w], [PB[bk]])
            kb.tt("dve", M[:, cb * 512:(cb + 1) * 512], pf(bk), M[:, cb * 512:(cb + 1) * 512], ALU.add, [PB[bk], M], [M])
            if cb < 8:
                bk2 = 2 + cb % 2
                kb.mm([(pf(bk2), repv[:, 1, k, :], v3(w.ap, KC)[:, k, :], k == 0, k == KC - 1) for k in range(KC)],
                      [rep, w], [PB[bk2]])
                kb.tt("dve", Mc[:, cb * 512:(cb + 1) * 512], pf(bk2), Mc[:, cb * 512:(cb + 1) * 512], ALU.add, [PB[bk2], Mc], [Mc])
        tmpc = ar.f32(6 * 16, "tmpc")
        tmpcv = v3(tmpc.ap, 6)
        srcs = [(M, 0), (M, 1), (M, 3), (M, 4), (Mc, 0), (Mc, 1)]
        for i, (src, ch) in enumerate(srcs):
            kb.tr([(pf(4, 4)[:, k * 128:(k + 1) * 128], src[:, ch * D + k * 128: ch * D + (k + 1) * 128], ident_f.ap)
                   for k in range(KC)], [src, ident_f], PB[4:8])
            kb.copy("dve", tmpcv[:, i, :], v3(pf(4, 4), KC)[:, :, 0], PB[4:8], [tmpc])
        kb.copy("act", g1_bc.ap, M[:, 2 * D:3 * D], [M], [g1_bc])
        kb.copy("act", g2_bc.ap, M[:, 5 * D:6 * D], [M], [g2_bc])
        for (dst, sc_i, n_i) in ((C_G1, 1, C_N1), (C_G2, 3, C_N2), (C_CG1, 5, C_N1)):
            kb.stt("dve", colv[:, dst, :], tmpcv[:, sc_i, :], 1.0, colv[:, n_i, :], ALU.add, ALU.mult, [tmpc, cols], [cols])
        for (dst, sh_i) in ((C_SH1, 0), (C_SH2, 2), (C_CSH1, 4)):
            kb.copy("dve", colv[:, dst, :], tmpcv[:, sh_i, :], [tmpc], [cols])
        fw.barrier()
        ar.ptr = const_end

        class FE:
            def __init__(self, tb0):
                self.xt = [ar.f32(D, "xt0"), ar.f32(D, "xt1")]
                self.xs = ar.bf16(D, "xs")
                self.xnT = ar.bf16(D, "xnT")
                self.stt_ = ar.f32(4, "fe_st")
                self.n = 0
                self.tb0 = tb0

            def load(self, rows_ap, key="xt"):
                t = self.xt[self.n % 2]
                kb.dma("sp", t.ap, rows_ap, "%s%d" % (key, self.n % 2), (), [t])
                return t

            def norm_T(self, t, gi, si):
                s = self.stt_
                tb0 = self.tb0
                kb.act(self.xs.ap, t.ap, AF.Square, [t], [self.xs, s], accum_out=s[:, 0:1])
                kb.ts("dve", s[:, 1:2], s[:, 0:1], 1.0 / D, EPS, ALU.mult, ALU.add, [s], [s])
                kb.act(s[:, 1:2], s[:, 1:2], AF.Sqrt, [s], [s])
                kb.recip(s[:, 2:3], s[:, 1:2], [s], [s])
                kb.ts("dve", self.xs.ap, t.ap, s[:, 2:3], None, ALU.mult, None, [t, s], [self.xs])
                pv = v3(pbf(tb0, 2), KC)
                kb.tr([(pv[:, k, :], self.xs[:, k * 128:(k + 1) * 128], ident_b.ap) for k in range(KC)],
                      [self.xs, ident_b], PB[tb0:tb0 + 2])
                xv = v3(self.xnT.ap, KC)
                kb.acts([(xv[:, k, :], pv[:, k, :], AF.Identity,
                          dict(scale=colv[:, gi, k:k + 1], bias=colv[:, si, k:k + 1])) for k in range(KC)],
                        PB[tb0:tb0 + 2] + [cols], [self.xnT])
                self.n += 1
                return xv

        GW = 3104
        wg = ar.bf16(KC * GW, "wg")
        wgv = v3(wg.ap, KC)
        kb.dma("pool", wgv, w_in.rearrange("(k p) n -> p k n", p=128)[:, :, 0:GW], "wg", (), [wg])
        Wga = [ar.f32(512, "Wga0"), ar.f32(512, "Wga1")]
        for d in range(2):
            kb.memset("pool", Wga[d].ap, 0.0, [Wga[d]])
            kb.dma("sp", Wga[d][16 * d:16 * d + 16, :], w_gate_up[d], "misc", (), [Wga[d]])
            kb.dma("sp", Wga[d][32:33, :], b_gate[d:d + 1, :], "misc", (), [Wga[d]])
        ngbc = ar.f32(1024, "ngbc")
        kb.dma("sp", ngbc.ap, gla_norm_g.partition_broadcast(128), "misc", (), [ngbc])
        S = [ar.f32(1024, "S_f"), ar.f32(1024, "S_b")]
        Sbf = ar.bf16(1024, "Sbf")
        for d in range(2):
            kb.memset("pool", S[d].ap, 0.0, [S[d]])
        fe = FE(6)
        qT = ar.f32(512, "qT")
        kT = ar.f32(512, "kT")
        k_sb = ar.f32(512, "k_sb")
        v_sb = ar.bf16(1024, "v_sb")
        lrT = ar.f32(128, "lrT")
        e1 = ar.f32(512, "e1")
        la = ar.f32(512, "la")
        ecT = ar.f32(512, "ecT")
        encT = ar.f32(512, "encT")
        ercum = ar.f32(512, "ercum")
        kdec = ar.bf16(512, "kdec")
        qd = ar.bf16(512, "qd")
        ki = ar.bf16(512, "ki")
        scm = ar.bf16(512, "scm")
        o_sb = ar.f32(1024, "o_sb")
        of_sb = ar.f32(1024, "of_sb")
        silug = ar.f32(1024, "silug")
        ybf = ar.bf16(1024, "ybf")
        gst = ar.f32(16, "gst")
        kb.memset("pool", lrT.ap, 1.0, [lrT])
        ONE_NF16 = fl2[:, 3:4]
        ONE_F = fl[:, 3:4]

        TRI = {0: (A_le, A_gt, 127), 1: (A_ge, A_lt, 0)}

        def inproj_state(xv):
            kb.mm([(pf(2), xv[:, k, :], wgv[:, k, 512:1024], k == 0, k == KC - 1) for k in range(KC)],
                  [fe.xnT, wg], [PB[2]])
            kb.copy("act", k_sb.ap, pf(2), [PB[2]], [k_sb])
            for hf in range(2):
                kb.mm([(pf(3 + hf), xv[:, k, :], wgv[:, k, 1024 + hf * 512:1536 + hf * 512], k == 0, k == KC - 1)
                       for k in range(KC)], [fe.xnT, wg], [PB[3 + hf]])
            kb.copy("act", v_sb.ap, pf(3, 2), PB[3:5], [v_sb])
            kb.mm([(pf(5)[0:32, 0:128], wgv[:, k, 3072:3104], xv[:, k, :], k == 0, k == KC - 1) for k in range(KC)],
                  [fe.xnT, wg], [PB[5]])
            kb.copy("dve", lrT[0:32, :], pf(5)[0:32, 0:128], [PB[5]], [lrT])

        def decay_parts(d, nf16_ap):
            cumm, rcm, _ = TRI[d]
            kb.mm([(pf(2), lrT[0:33, :], Wga[d][0:33, :], True, True)], [lrT, Wga[d]], [PB[2]])
            kb.act(e1.ap, pf(2), AF.Exp, [PB[2]], [e1], scale=-1.0)
            kb.act(e1.ap, e1.ap, AF.Ln, [e1], [e1], bias=1.0)
            kb.ts("dve", la.ap, e1.ap, nf16_ap, None, ALU.mult, None, [e1, fl2], [la])
            kb.mm([(pf(5), rcm.ap, la.ap, True, True)], [rcm, la], [PB[5]])
            kb.mm([(pf(1)[:, h * 128:(h + 1) * 128], la[:, h * 128:(h + 1) * 128], cumm.ap, True, True)
                   for h in range(4)], [la, cumm], [PB[1]])
            kb.act(ecT.ap, pf(1), AF.Exp, [PB[1]], [ecT])
            kb.act(ercum.ap, pf(5), AF.Exp, [PB[5]], [ercum])

        def state_update(d, f_ap):
            col = TRI[d][2]
            kb.stt("dve", kdec.ap, ercum.ap, f_ap, k_sb.ap, ALU.mult, ALU.mult, [ercum, k_sb, fl, fl2], [kdec])
            kb.mm([(pf(3, 2)[:, h * 256:(h + 1) * 256], kdec[:, h * 128:(h + 1) * 128],
                    v_sb[:, h * 256:(h + 1) * 256], True, True) for h in range(4)], [kdec, v_sb], PB[3:5])
            kb.stts("dve", [(S[d][:, h * 256:(h + 1) * 256], S[d][:, h * 256:(h + 1) * 256],
                             ecT[:, h * 128 + col:h * 128 + col + 1], pf(3, 2)[:, h * 256:(h + 1) * 256],
                             ALU.mult, ALU.add) for h in range(4)], [S[d], ecT] + PB[3:5], [S[d]])

        def state_block(rows_ap, gi, si, fcol):
            t = fe.load(rows_ap)
            xv = fe.norm_T(t, gi, si)
            inproj_state(xv)
            decay_parts(0, fl2[:, fcol:fcol + 1])
            state_update(0, fl[:, fcol:fcol + 1])
            decay_parts(1, fl2[:, 8 + fcol:9 + fcol])
            state_update(1, fl2[:, 16 + fcol:17 + fcol])

        def main_block(d, blk):
            cumm, rcm, col = TRI[d]
            t = fe.load(xm[blk * 128:(blk + 1) * 128, :])
            xv = fe.norm_T(t, C_G1, C_SH1)
            if d == 1:
                for hf in range(2):
                    kb.mm([(pf(6 + hf), xv[:, k, :], wgv[:, k, 2048 + hf * 512:2560 + hf * 512], k == 0, k == KC - 1)
                           for k in range(KC)], [fe.xnT, wg], [PB[6 + hf]])
                kb.act(silug.ap, pf(6, 2), AF.Silu, PB[6:8], [silug])
                kb.dma("sp", of_sb.ap, of_d[blk * 128:(blk + 1) * 128, :], "of_sb", [ofd_tl[blk]], [of_sb])
            kb.mm([(pf(0)[:, h * 128:(h + 1) * 128], wgv[:, k, h * 128:(h + 1) * 128], xv[:, k, :], k == 0, k == KC - 1)
                   for h in range(4) for k in range(KC)], [fe.xnT, wg], [PB[0]])
            kb.act(qT.ap, pf(0), AF.Identity, [PB[0]], [qT], scale=128.0 ** -0.5)
            kb.mm([(pf(1)[:, h * 128:(h + 1) * 128], wgv[:, k, 512 + h * 128:512 + (h + 1) * 128], xv[:, k, :], k == 0, k == KC - 1)
                   for h in range(4) for k in range(KC)], [fe.xnT, wg], [PB[1]])
            kb.copy("dve", kT.ap, pf(1), [PB[1]], [kT])
            inproj_state(xv)
            decay_parts(d, ONE_NF16)
            kb.act(encT.ap, pf(1), AF.Exp, [PB[1]], [encT], scale=-1.0)
            kb.tt("dve", qd.ap, qT.ap, ecT.ap, ALU.mult, [qT, ecT], [qd])
            kb.tt("dve", ki.ap, kT.ap, encT.ap, ALU.mult, [kT, encT], [ki])
            kb.mm([(pf(0)[:, h * 128:(h + 1) * 128], ki[:, h * 128:(h + 1) * 128], qd[:, h * 128:(h + 1) * 128], True, True)
                   for h in range(4)], [ki, qd], [PB[0]])
            kb.tt("dve", v3(scm.ap, 4), v3(pf(0), 4), cumm.ap.unsqueeze(1).to_broadcast([128, 4, 128]), ALU.mult,
                  [PB[0], cumm], [scm])
            sp = []
            for h in range(4):
                o_ap = pf(6, 2)[:, h * 256:(h + 1) * 256]
                sp.append((o_ap, scm[:, h * 128:(h + 1) * 128], v_sb[:, h * 256:(h + 1) * 256], True, False))
                sp.append((o_ap, qd[:, h * 128:(h + 1) * 128], Sbf[:, h * 256:(h + 1) * 256], False, True))
            kb.mm(sp, [scm, v_sb, qd, Sbf], PB[6:8])
            if d == 0:
                kb.copy("act", o_sb.ap, pf(6, 2), PB[6:8], [o_sb])
                kb.dma("sp", of_d[blk * 128:(blk + 1) * 128, :], o_sb.ap, "of_st", [o_sb], [ofd_tl[blk]])
            else:
                kb.tt("dve", o_sb.ap, pf(6, 2), of_sb.ap, ALU.add, PB[6:8] + [of_sb], [o_sb])
                kb.acts([(of_sb[:, h * 256:(h + 1) * 256], o_sb[:, h * 256:(h + 1) * 256], AF.Square,
                          dict(accum_out=gst[:, h:h + 1])) for h in range(4)], [o_sb], [of_sb, gst])
                kb.ts("dve", gst[:, 4:8], gst[:, 0:4], 1.0 / 256.0, EPS, ALU.mult, ALU.add, [gst], [gst])
                kb.act(gst[:, 4:8], gst[:, 4:8], AF.Sqrt, [gst], [gst])
                kb.recip(gst[:, 8:12], gst[:, 4:8], [gst], [gst])
                kb.stts("dve", [(o_sb[:, h * 256:(h + 1) * 256], o_sb[:, h * 256:(h + 1) * 256], gst[:, 8 + h:9 + h],
                                 ngbc[:, h * 256:(h + 1) * 256], ALU.mult, ALU.mult) for h in range(4)],
                        [o_sb, gst, ngbc], [o_sb])
                kb.tt("dve", ybf.ap, o_sb.ap, silug.ap, ALU.mult, [o_sb, silug], [ybf])
                kb.dma("sp", mix_d[blk * 128:(blk + 1) * 128, 0:1024], ybf.ap, "y_st", [ybf], ())
            state_update(d, ONE_F)
            kb.copy("pool", Sbf.ap, S[d].ap, [S[d]], [Sbf])

        ofd_tl = [Tl(None, 'ofd') for _ in range(NB)]
        nctx = cfg.CTX // 128
        for b in range(nctx):
            state_block(ctxb[b * 128:(b + 1) * 128, :], C_CG1, C_CSH1, 3)
        for b in range(nctx - 1, -1, -1):
            state_block(ctxb[b * 128:(b + 1) * 128, :], C_CG1, C_CSH1, 4)
        for j in range(3):
            for b in range(NB):
                r0 = (j * NB + b) * 128
                state_block(xo[r0:r0 + 128, :], C_G1, C_SH1, j)
        kb.copy("pool", Sbf.ap, S[0].ap, [S[0]], [Sbf])
        for b in range(NB):
            main_block(0, b)
        kb.copy("pool", Sbf.ap, S[1].ap, [S[1]], [Sbf])
        for b in range(NB - 1, -1, -1):
            main_block(1, b)
        fw.barrier()
        ar.ptr = const_end

        wc = ar.bf16(KC * 2048, "wc")
        wcv = v3(wc.ap, KC)
        kb.dma("pool", wcv, w_in.rearrange("(k p) n -> p k n", p=128)[:, :, GW:IN_W], "wc", (), [wc])
        wsT = ar.bf16(1024, "wsT")
        wstg = ar.f32(1024, "wstg")
        bs_col = ar.f32(8, "bs_col")
        lng = ar.f32(1024, "lng")
        lnb = ar.f32(1024, "lnb")
        kb.dma("sp", v3(wstg.ap, 8), w_spatial.rearrange("g p q -> p g q"), "misc", (), [wstg])
        kb.dma("sp", bs_col.ap, b_spatial.rearrange("g p -> p g"), "misc", (), [bs_col], nc_ok=True)
        kb.dma("sp", lng.ap, cmlp_ln_g.partition_broadcast(128), "misc", (), [lng])
        kb.dma("sp", lnb.ap, cmlp_ln_b.partition_broadcast(128), "misc", (), [lnb])
        kb.tr([(pf(0, 2)[:, g * 128:(g + 1) * 128], wstg[:, g * 128:(g + 1) * 128], ident_f.ap) for g in range(8)],
              [wstg, ident_f], PB[0:2])
        kb.copy("dve", wsT.ap, pf(0, 2), PB[0:2], [wsT])
        fe = FE(6)
        gu = ar.f32(1024, "gu")
        gv = ar.f32(1024, "gv")
        vn = ar.bf16(1024, "vn")
        cm = ar.bf16(1024, "cm")
        cst = ar.f32(8, "cst")
        for blk in range(NB):
            t = fe.load(xm[blk * 128:(blk + 1) * 128, :])
            xv = fe.norm_T(t, C_G1, C_SH1)
            for q4 in range(4):
                kb.mm([(pf(q4), xv[:, k, :], wcv[:, k, q4 * 512:(q4 + 1) * 512], k == 0, k == KC - 1) for k in range(KC)],
                      [fe.xnT, wc], [PB[q4]])
            kb.act(gu.ap, pf(0, 2), AF.Gelu, PB[0:2], [gu])
            kb.act(gv.ap, pf(2, 2), AF.Gelu, PB[2:4], [gv, cst], accum_out=cst[:, 0:1])
            kb.ts("dve", cst[:, 1:2], cst[:, 0:1], -1.0 / 1024.0, None, ALU.mult, None, [cst], [cst])
            kb.act(vn.ap, gv.ap, AF.Square, [gv, cst], [vn, cst], bias=cst[:, 1:2], accum_out=cst[:, 2:3])
            kb.ts("dve", cst[:, 3:4], cst[:, 2:3], 1.0 / 1024.0, EPS, ALU.mult, ALU.add, [cst], [cst])
            kb.act(cst[:, 3:4], cst[:, 3:4], AF.Sqrt, [cst], [cst])
            kb.recip(cst[:, 4:5], cst[:, 3:4], [cst], [cst])
            kb.ts("dve", gv.ap, gv.ap, cst[:, 1:2], cst[:, 4:5], ALU.add, ALU.mult, [gv, cst], [gv])
            kb.tt("dve", gv.ap, gv.ap, lng.ap, ALU.mult, [gv, lng], [gv])
            kb.tt("dve", vn.ap, gv.ap, lnb.ap, ALU.add, [gv, lnb], [vn])
            kb.mm([(pf(4, 2)[:, g * 128:(g + 1) * 128], wsT[:, g * 128:(g + 1) * 128], vn[:, g * 128:(g + 1) * 128], True, True)
                   for g in range(8)], [wsT, vn], PB[4:6])
            kb.stts("dve", [(cm[:, g * 128:(g + 1) * 128], pf(4, 2)[:, g * 128:(g + 1) * 128], bs_col[:, g:g + 1],
                             gu[:, g * 128:(g + 1) * 128], ALU.add, ALU.mult) for g in range(8)],
                    PB[4:6] + [bs_col, gu], [cm])
            kb.dma("sp", mix_d[blk * 128:(blk + 1) * 128, 1024:2048], cm.ap, "cm_st", [cm], ())
        fw.barrier()
        ar.ptr = const_end

        wo = ar.bf16(KC * D, "wo")
        wov = v3(wo.ap, KC)
        wq = ar.bf16(KC * D, "wq")
        wqv = v3(wq.ap, KC)
        skT = ar.bf16(D, "skT")
        skTv = v3(skT.ap, 16)
        kb.dma("pool", wqv, peer_wq.rearrange("(k p) n -> p k n", p=128), "wq", (), [wq])
        h1 = ar.f32(D, "h1")
        sc = ar.f32(D, "sc")
        tmp = ar.f32(D, "tmp")
        stg = [h1, sc]
        for k in range(KC):
            sg = stg[k % 2]
            kb.dma("sp", sg.ap, w_out[k * 128:(k + 1) * 128, :], "wo_st%d" % (k % 2), (), [sg])
            kb.tt("dve", wov[:, k, :], sg.ap, g1_bc.ap, ALU.mult, [sg, g1_bc], [wo])
        kb.dma("sp", v3(tmp.ap, 16), peer_sk.rearrange("t h k d -> k (t h) d"), "misc", (), [tmp])
        specs = []
        for half in range(2):
            for hh in range(8):
                c = hh * 2 + half
                specs.append((pf(0, 4)[:, c * 128:(c + 1) * 128], tmp[:, (half * 8 + hh) * 128:(half * 8 + hh + 1) * 128], ident_f.ap))
        kb.tr(specs, [tmp, ident_f], PB[0:4])
        kb.copy("dve", skT.ap, pf(0, 4), PB[0:4], [skT])
        mixT = ar.bf16(D, "mixT")
        mixTv = v3(mixT.ap, KC)
        xt5 = sc
        xn2T = ar.bf16(D, "xn2T")
        xn2v = v3(xn2T.ap, KC)
        qTb = ar.bf16(D, "qTb")
        qTv = v3(qTb.ap, 16)
        v16 = ar.f32(256, "v16")
        v16v = v3(v16.ap, 16)
        cand = ar.f32(D, "cand")
        mix_sb = cand
        mix_ap = cand.ap[:, 0:1024].bitcast(BF16)
        xs2 = mixT
        best = ar.f32(128, "best")
        bestv = v3(best.ap, 8)
        nega = tmp
        nega_ap = tmp.ap[:, 1024:2048]
        eat = tmp
        eat_ap = tmp.ap[:, 0:1024]
        pst = ar.f32(64, "pst")
        exw = ar.f32(128, "exw")
        scv = sc.ap.rearrange("p (h t k) -> p h t k", h=8, t=2)
        v16q = v16.ap.rearrange("p (h t r) -> p h t r", h=8, t=2)
        for blk in range(NB):
            r0 = blk * 128
            kb.dma("sp", mix_ap, mix_d[r0:r0 + 128, :], "mix_ld", (), [mix_sb])
            kb.dma("sp", xt5.ap, xm[r0:r0 + 128, :], "xt5", (), [xt5])
            pv = v3(pbf(0, 2), KC)
            kb.tr([(pv[:, k, :], mix_ap[:, k * 128:(k + 1) * 128], ident_b.ap) for k in range(KC)], [mix_sb, ident_b], PB[0:2])
            kb.copy("act", mixT.ap, pbf(0, 2), PB[0:2], [mixT])
            for q4 in range(4):
                kb.mm([(pf(2 + q4), mixTv[:, k, :], wov[:, k, q4 * 512:(q4 + 1) * 512], k == 0, k == KC - 1) for k in range(KC)],
                      [mixT, wo], [PB[2 + q4]])
            kb.tt("dve", h1.ap, pf(2, 4), xt5.ap, ALU.add, PB[2:6] + [xt5], [h1])
            kb.dma("sp", h1_d[r0:r0 + 128, :], h1.ap, "h1_st", [h1], ())
            kb.act(xs2.ap, h1.ap, AF.Square, [h1], [xs2, pst], accum_out=pst[:, 0:1])
            kb.ts("dve", pst[:, 1:2], pst[:, 0:1], 1.0 / D, EPS, ALU.mult, ALU.add, [pst], [pst])
            kb.act(pst[:, 1:2], pst[:, 1:2], AF.Sqrt, [pst], [pst])
            kb.recip(pst[:, 2:3], pst[:, 1:2], [pst], [pst])
            kb.ts("dve", xs2.ap, h1.ap, pst[:, 2:3], None, ALU.mult, None, [h1, pst], [xs2])
            pv2 = v3(pbf(6, 2), KC)
            kb.tr([(pv2[:, k, :], xs2[:, k * 128:(k + 1) * 128], ident_b.ap) for k in range(KC)], [xs2, ident_b], PB[6:8])
            kb.acts([(xn2v[:, k, :], pv2[:, k, :], AF.Identity,
                      dict(scale=colv[:, C_G2, k:k + 1], bias=colv[:, C_SH2, k:k + 1])) for k in range(KC)],
                    PB[6:8] + [cols], [xn2T])
            kb.dma("sp", xn2T_d[blk], xn2T.ap, "xn2_st", [xn2T], ())
            kb.mm([(pf(2, 4)[:, c * 128:(c + 1) * 128], wqv[:, k, c * 128:(c + 1) * 128], xn2v[:, k, :], k == 0, k == KC - 1)
                   for c in range(16) for k in range(KC)], [wq, xn2T], PB[2:6])
            kb.copy("act", qTb.ap, pf(2, 4), PB[2:6], [qTb])
            kb.mm([(pf(0, 2)[:, c * 128:(c + 1) * 128], qTv[:, c, :], skTv[:, c, :], True, True) for c in range(8)],
                  [qTb, skT], PB[0:2])
            kb.mm([(pf(6, 2)[:, (c - 8) * 128:(c - 7) * 128], qTv[:, c, :], skTv[:, c, :], True, True) for c in range(8, 16)],
                  [qTb, skT], PB[6:8])
            kb.copy("act", sc[:, 0:1024], pf(0, 2), PB[0:2], [sc])
            kb.copy("act", sc[:, 1024:2048], pf(6, 2), PB[6:8], [sc])
            fw.op("dve", lambda e: [e.max(out=v16v[:, c, 0:8], in_=sc[:, c * 128:(c + 1) * 128]) for c in range(16)][-1], [sc], [v16])
            fw.op("dve", lambda e: [e.match_replace(out=tmp[:, c * 128:(c + 1) * 128], in_to_replace=v16v[:, c, 0:8],
                                                    in_values=sc[:, c * 128:(c + 1) * 128], imm_value=-1e30) for c in range(16)][-1],
                  [sc, v16], [tmp])
            fw.op("dve", lambda e: [e.max(out=v16v[:, c, 8:16], in_=tmp[:, c * 128:(c + 1) * 128]) for c in range(16)][-1], [tmp], [v16])
            candv = cand.ap.rearrange("p (h r c) -> p h r c", h=8, r=16)
            kb.tt("dve", candv, v16q[:, :, 0, :].unsqueeze(3).to_broadcast([128, 8, 16, 16]),
                  v16q[:, :, 1, :].unsqueeze(2).to_broadcast([128, 8, 16, 16]), ALU.add, [v16], [cand])
            fw.op("dve", lambda e: [e.max(out=bestv[:, hh, 0:8], in_=cand[:, hh * 256:(hh + 1) * 256]) for hh in range(8)][-1], [cand], [best])
            fw.op("dve", lambda e: [e.match_replace(out=tmp[:, hh * 256:(hh + 1) * 256], in_to_replace=bestv[:, hh, 0:8],
                                                    in_values=cand[:, hh * 256:(hh + 1) * 256], imm_value=-1e30) for hh in range(8)][-1],
                  [cand, best], [tmp])
            fw.op("dve", lambda e: [e.max(out=bestv[:, hh, 8:16], in_=tmp[:, hh * 256:(hh + 1) * 256]) for hh in range(8)][-1], [tmp], [best])
            kb.tt("dve", v3(exw.ap, 8), bestv, bestv[:, :, 0:1].to_broadcast([128, 8, 16]), ALU.subtract, [best], [exw])
            kb.act(exw.ap, exw.ap, AF.Exp, [exw], [exw])
            kb.reduce("dve", pst[:, 8:16], v3(exw.ap, 8), AX.X, ALU.add, [exw], [pst])
            kb.recip(pst[:, 16:24], pst[:, 8:16], [pst], [pst])
            negav = v3(nega_ap, 8)
            kb.tt("dve", negav, bestv[:, :, 15:16].to_broadcast([128, 8, 128]), scv[:, :, 0, :], ALU.subtract, [best, sc], [nega])
            kb.ts("dve", nega_ap, nega_ap, -1e-5, None, ALU.add, None, [nega], [nega])
            kb.dma("sp", st_nega[blk], nega_ap, "st_st", [nega], ())
            kb.dma("sp", v3(st_s2[blk], 8), scv[:, :, 1, :], "st_st", [sc], ())
            eatv = v3(eat_ap, 8)
            kb.tt("dve", eatv, scv[:, :, 0, :], v16q[:, :, 0, 0:1].to_broadcast([128, 8, 128]), ALU.subtract, [sc, v16], [eat])
            kb.act(eat_ap, eat_ap, AF.Exp, [eat], [eat])
            ea_ap = cand.ap[:, 0:512].bitcast(BF16)
            eb_ap = cand.ap[:, 512:1024].bitcast(BF16)
            kb.tt("dve", v3(ea_ap, 8), eatv, pst[:, 16:24].unsqueeze(2).to_broadcast([128, 8, 128]), ALU.mult, [eat, pst], [cand])
            kb.tt("dve", eatv, scv[:, :, 1, :], v16q[:, :, 1, 0:1].to_broadcast([128, 8, 128]), ALU.subtract, [sc, v16], [eat])
            kb.act(eb_ap, eat_ap, AF.Exp, [eat], [cand])
            kb.dma("sp", st_ea[blk], ea_ap, "st_st", [cand], ())
            kb.dma("sp", st_eb[blk], eb_ap, "st_st", [cand], ())
        fw.barrier()
        ar.ptr = const_end

        ust = [ar.bf16(D, "ust0"), ar.bf16(D, "ust1")]
        ugr = [ar.bf16(KC * 512, "ugr0"), ar.bf16(KC * 512, "ugr1")]
        for et in range(128):
            g, sub = divmod(et, 4)
            us = ust[et % 2]
            ug = ugr[g % 2]
            ugv = v3(ug.ap, KC)
            kb.dma("pool", us.ap, peer_u[et * 128:(et + 1) * 128, :], "ust%d" % (et % 2), (), [us])
            kb.dma("pool", V_d[et * 128:(et + 1) * 128, :], peer_v[et * 128:(et + 1) * 128, :], "vcast", (), ())
            bk = 2 * (et % 2)
            pv = v3(pbf(bk, 2), KC)
            kb.tr([(pv[:, k, :], us[:, k * 128:(k + 1) * 128], ident_b.ap) for k in range(KC)], [us, ident_b], PB[bk:bk + 2])
            if et % 2 == 0:
                kb.copy("act", ugv[:, :, sub * 128:(sub + 1) * 128], pv, PB[bk:bk + 2], [ug])
            else:
                kb.copy("dve", ugv[:, :, sub * 128:(sub + 1) * 128], pv, PB[bk:bk + 2], [ug])
            if sub == 3:
                kb.dma("sp", UT_d[g], ug.ap, "ut_st%d" % (g % 2), [ug], ())
        fw.barrier()
        ar.ptr = const_end

        ar.ptr = const_end_p6
        ut_raw = [ar.f32(KC * 256, "ut0"), ar.f32(KC * 256, "ut1")]
        vt_raw = [ar.f32(2 * D, "vt0"), ar.f32(2 * D, "vt1")]
        for t_ in ut_raw + vt_raw:
            t_.ap, t_.name = t_.ap.bitcast(BF16), t_.ap
        ut, vt = ut_raw, vt_raw
        xq = ar.bf16(TS * D, "xq")
        xqv = xq.ap.rearrange("p (t k m) -> p t k m", t=TS, k=KC)
        sn = ar.f32(TS * 1024, "sn")
        s2t = ar.f32(TS * 1024, "s2t")
        eaT = ar.bf16(TS * 1024, "eaT")
        ebT = ar.bf16(TS * 1024, "ebT")
        snv = sn.ap.rearrange("p (t h k) -> p t h k", t=TS, h=8)
        s2v = s2t.ap.rearrange("p (t h k) -> p t h k", t=TS, h=8)
        eav = eaT.ap.rearrange("p (t h k) -> p t h k", t=TS, h=8)
        ebv = ebT.ap.rearrange("p (t h k) -> p t h k", t=TS, h=8)
        acc = ar.f32(TS * D, "acc")
        accv = v3(acc.ap, TS)
        actg = [ar.bf16(512, "actg%d" % i) for i in range(3)]
        Mk = [ar.bf16(4096, "Mk0"), ar.bf16(4096, "Mk1")]
        Gs = [ar.bf16(512, "G0"), ar.bf16(512, "G1")]
        HmT = [ar.bf16(512, "HmT0"), ar.bf16(512, "HmT1")]
        fst = ar.f32(4, "fst")
        fng = vt[1]
        fng_ap = vt[1].name[:, 0:D]
        h2 = ut[0]
        h2_ap = ut[0].name[:, 0:D]
        junk = Mk[0]
        junk_ap = Mk[0].ap[:, 0:D]
        for stile in range(NB // TS):
            b0 = stile * TS
            for tb in range(TS):
                kb.dma("sp", xq[:, tb * D:(tb + 1) * D], xn2T_d[b0 + tb], "xq", (), [xq])
                kb.dma("sp", sn[:, tb * 1024:(tb + 1) * 1024], st_nega[b0 + tb], "sn", (), [sn])
                kb.dma("sp", s2t[:, tb * 1024:(tb + 1) * 1024], st_s2[b0 + tb], "s2t", (), [s2t])
                kb.dma("sp", eaT[:, tb * 1024:(tb + 1) * 1024], st_ea[b0 + tb], "eaT", (), [eaT])
                kb.dma("sp", ebT[:, tb * 1024:(tb + 1) * 1024], st_eb[b0 + tb], "ebT", (), [ebT])
            kb.memset("pool", acc.ap, 0.0, [acc])
            its = [(g, tb) for g in range(32) for tb in range(TS)]
            N = len(its)
            wts = {}

            def load_u(g):
                u = ut[g % 2]
                kb.dma("sp", u.ap, UT_d[g], "ut%d" % (g % 2), (), [u])

            def load_v(g):
                vv = vt[g % 2]
                kb.dma("sp", v3(vv.ap, 4), V_d[g * 512:(g + 1) * 512, :].rearrange("(i e) n -> e i n", e=128),
                       "vt%d" % (g % 2), (), [vv])

            def stA(s_):
                g, tb = its[s_]
                if tb == 0:
                    load_u(g)
                mkv = Mk[s_ % 2].ap.rearrange("p (h i j) -> p h i j", h=8, i=4)

                kb.tt("dve", mkv, s2v[:, tb].unsqueeze(2).to_broadcast([128, 8, 4, 128]),
                      snv[:, tb, :, 4 * g:4 * g + 4].unsqueeze(3).to_broadcast([128, 8, 4, 128]), ALU.is_ge, [s2t, sn], [Mk[s_ % 2]])
                uv = v3(ut[g % 2].ap, KC)
                p = s_ % 2
                kb.mm([(pf(p), xqv[:, tb, k, :], uv[:, k, :], k == 0, k == KC - 1) for k in range(KC)], [xq, ut[g % 2]], [PB[p]])
                kb.act(actg[s_ % 3].ap, pf(p), AF.Gelu, [PB[p]], [actg[s_ % 3]])

            def stB(s_):
                g, tb = its[s_]
                mkv = Mk[s_ % 2].ap.rearrange("p (h i j) -> p h i j", h=8, i=4)
                kb.tt("pool", mkv, mkv, ebv[:, tb].unsqueeze(2).to_broadcast([128, 8, 4, 128]), ALU.mult, [Mk[s_ % 2], ebT], [Mk[s_ % 2]])
                kb.tt("pool", mkv, mkv, eav[:, tb, :, 4 * g:4 * g + 4].unsqueeze(3).to_broadcast([128, 8, 4, 128]), ALU.mult,
                      [Mk[s_ % 2], eaT], [Mk[s_ % 2]])

            def stC(s_):
                mk = Mk[s_ % 2]
                gs = Gs[s_ % 2]
                kb.tt("dve", mk[:, 0:2048], mk[:, 0:2048], mk[:, 2048:4096], ALU.add, [mk], [mk])
                kb.tt("dve", mk[:, 0:1024], mk[:, 0:1024], mk[:, 1024:2048], ALU.add, [mk], [mk])
                kb.tt("dve", gs.ap, mk[:, 0:512], mk[:, 512:1024], ALU.add, [mk], [gs])
                kb.tt("pool", gs.ap, gs.ap, actg[s_ % 3].ap, ALU.mult, [gs, actg[s_ % 3]], [gs])

            def stD(s_):
                g, tb = its[s_]
                p = s_ % 2
                gs, hT = Gs[p], HmT[p]
                vvv = v3(vt[g % 2].ap, 4)
                kb.tr([(pbf(2 + p)[:, i * 128:(i + 1) * 128], gs[:, i * 128:(i + 1) * 128], ident_b.ap) for i in range(4)],
                      [gs, ident_b], [PB[2 + p]])
                kb.copy("act", hT.ap, pbf(2 + p)[:, 0:512], [PB[2 + p]], [hT])
                kb.mm([(pf(4 + fq), hT[:, i * 128:(i + 1) * 128], vvv[:, i, fq * 512:(fq + 1) * 512], i == 0, i == 3)
                       for fq in range(4) for i in range(4)], [hT, vt[g % 2]], PB[4:8])
                kb.tt("dve", accv[:, tb, :], pf(4, 4), accv[:, tb, :], ALU.add, PB[4:8] + [acc], [acc])

            load_v(0)
            load_v(1)
            for s_ in range(N + 3):
                if 0 <= s_ - 2 < N:
                    stC(s_ - 2)
                if s_ < N:
                    stA(s_)
                if 0 <= s_ - 1 < N:
                    stB(s_ - 1)
                if 0 <= s_ - 3 < N:
                    stD(s_ - 3)
                    g_, tb_ = its[s_ - 3]
                    if tb_ == TS - 1 and g_ + 2 < 32:
                        load_v(g_ + 2)
            kb.dma("sp", fng_ap, final_g.partition_broadcast(128), "fng", (), [fng])
            for tb in range(TS):
                r0 = (b0 + tb) * 128
                kb.dma("sp", h2_ap, h1_d[r0:r0 + 128, :], "h2_ld", (), [h2])
                kb.tt("dve", accv[:, tb, :], accv[:, tb, :], g2_bc.ap, ALU.mult, [acc, g2_bc], [acc])
                kb.tt("dve", h2_ap, h2_ap, accv[:, tb, :], ALU.add, [h2, acc], [h2])
                kb.act(junk_ap, h2_ap, AF.Square, [h2], [junk, fst], accum_out=fst[:, 0:1])
                kb.ts("dve", fst[:, 1:2], fst[:, 0:1], 1.0 / D, EPS, ALU.mult, ALU.add, [fst], [fst])
                kb.act(fst[:, 1:2], fst[:, 1:2], AF.Sqrt, [fst], [fst])
                kb.recip(fst[:, 2:3], fst[:, 1:2], [fst], [fst])
                kb.stt("dve", h2_ap, h2_ap, fst[:, 2:3], fng_ap, ALU.mult, ALU.mult, [h2, fst, fng], [h2])
                kb.dma("sp", y[r0:r0 + 128, :], h2_ap, "y_out", [h2], ())
        fw.barrier()
        fw.emit(st)
    return nc


_PROG_CACHE = {}


def make_in_maps(cfg, inp):
    NB, T = cfg.NB, cfg.TPC
    f32 = lambda a: np.ascontiguousarray(np.asarray(a, dtype=np.float32))
    shared = dict(
        cctx=f32(inp["c_ctx"]), norm1_g=f32(inp["norm1_g"][0]), norm2_g=f32(inp["norm2_g"][0]),
        w_mod=f32(inp["w_mod"][0]), b_mod=f32(inp["b_mod"][0]), w_in=f32(inp["w_in"][0]),
        w_gate_up=f32(inp["w_gate_up"][0]), b_gate=f32(inp["b_gate"][0]), gla_norm_g=f32(inp["gla_norm_g"][0]),
        cmlp_ln_g=f32(inp["cmlp_ln_g"][0]), cmlp_ln_b=f32(inp["cmlp_ln_b"][0]), w_spatial=f32(inp["w_spatial"][0]),
        b_spatial=f32(inp["b_spatial"][0]), w_out=f32(inp["w_out"][0]), peer_wq=f32(inp["peer_wq"][0]),
        peer_sk=f32(inp["peer_sub_keys"][0]), peer_u=f32(inp["peer_u"][0]), peer_v=f32(inp["peer_v"][0]),
        final_g=f32(inp["final_norm_g"]),
    )
    x = np.asarray(inp["x"], dtype=np.float32)
    ctx = np.asarray(inp["ctx"], dtype=np.float32)
    c = np.asarray(inp["c"], dtype=np.float32)
    maps = []
    for core in range(8):
        b, seg = divmod(core, 4)
        xb = x[b]
        slots = [(s, 1.0) for s in range(seg)] + [(s, 0.0) for s in range(3, seg, -1)]
        flags = np.zeros((128, 8), np.float32)
        parts = []
        for j, (s, f) in enumerate(slots):
            blocks = xb[s * T:(s + 1) * T].reshape(NB, 128, D)
            if f == 0.0:
                blocks = blocks[::-1]
            parts.append(blocks.reshape(T, D))
            flags[:, j] = f
        flags[:, 3] = 1.0
        m = dict(shared)
        m.update(xm=f32(xb[seg * T:(seg + 1) * T]), xo=f32(np.concatenate(parts, 0)), ctxb=f32(ctx[b]),
                 cvec=f32(c[b]), flags=flags)
        maps.append(m)
    return maps


def kernel(**inp):
    seq = int(np.asarray(inp["x"]).shape[1])
    cfg = Cfg(seq, int(np.asarray(inp["ctx"]).shape[1]))
    if seq not in _PROG_CACHE:
        _PROG_CACHE[seq] = build_program(cfg)
    nc = _PROG_CACHE[seq]
    maps = make_in_maps(cfg, inp)
    res = run_bass_kernel_spmd(nc, maps, core_ids=list(range(8)))
    out = np.empty((2, seq, D), np.float32)
    for core in range(8):
        b, seg = divmod(core, 4)
        out[b, seg * cfg.TPC:(seg + 1) * cfg.TPC] = res.results[core]["y"]
    return out
```

```python
import numpy as np
import concourse.bass as bass
import concourse.mybir as mybir
from concourse.bass_utils import run_bass_kernel_spmd

F32 = mybir.dt.float32
BF16 = mybir.dt.bfloat16
ALU = mybir.AluOpType
AF = mybir.ActivationFunctionType
AX = mybir.AxisListType


class Tl:
    __slots__ = ("ap", "name", "w", "r")

    def __init__(self, ap, name=""):
        self.ap = ap
        self.name = name
        self.w = None
        self.r = {}

    def __getitem__(self, k):
        return self.ap[k]


class Op:
    __slots__ = ("idx", "eng", "fn", "deps", "dma_key", "val", "signal", "dma_waits")


class FW:
    ENGS = ("pe", "act", "dve", "pool", "sp")

    def __init__(self, nc):
        self.nc = nc
        self.ops = []
        self.dma_cnt = {}

    def op(self, eng, fn, reads=(), writes=(), dma_key=None):
        o = Op()
        o.idx = len(self.ops)
        o.eng = eng
        o.fn = fn
        o.dma_key = dma_key
        o.signal = False
        o.val = 0
        deps = set()
        for t in reads:
            if t.w is not None:
                deps.add(t.w)
        for t in writes:
            if t.w is not None:
                deps.add(t.w)
            deps.update(t.r.values())
        o.deps = deps
        o.dma_waits = {}
        for d in deps:
            od = self.ops[d]
            if od.dma_key is not None:
                o.dma_waits[od.dma_key] = self.dma_cnt[od.dma_key]
        if dma_key is not None:
            self.dma_cnt[dma_key] = self.dma_cnt.get(dma_key, 0) + 16
            o.val = self.dma_cnt[dma_key]
        rkey = ("dma", dma_key) if dma_key is not None else eng
        for t in reads:
            t.r[rkey] = o.idx
        for t in writes:
            t.w = o.idx
            t.r = {}
        self.ops.append(o)
        return o

    def emit(self, stack):
        nc = self.nc
        ops = self.ops
        for o in ops:
            for d in o.deps:
                if ops[d].dma_key is None:
                    ops[d].signal = True
        cnt = {e: 0 for e in self.ENGS}
        for o in ops:
            if o.dma_key is None and o.signal:
                cnt[o.eng] += 1
                o.val = cnt[o.eng]
        esem = {e: stack.enter_context(nc.semaphore("s_" + e)) for e in self.ENGS}
        dsem = {k: stack.enter_context(nc.semaphore("d_%d" % i))
                for i, k in enumerate(self.dma_cnt)}
        streams = {e: [o for o in ops if o.eng == e] for e in self.ENGS}

        def run(eng_name, e):
            waited = {}
            for o in streams[eng_name]:
                waits = {}
                for d in o.deps:
                    od = ops[d]
                    if od.dma_key is not None:
                        key = ("d", od.dma_key)
                        v = o.dma_waits[od.dma_key]
                    else:
                        if od.eng == eng_name and eng_name == "pe":
                            continue
                        key = ("e", od.eng)
                        v = od.val
                    if waits.get(key, 0) < v:
                        waits[key] = v
                for key, v in waits.items():
                    if waited.get(key, 0) >= v:
                        continue
                    s = dsem[key[1]] if key[0] == "d" else esem[key[1]]
                    e.wait_ge(s, v)
                    waited[key] = v
                ins = o.fn(e)
                if o.dma_key is not None:
                    ins.then_inc(dsem[o.dma_key], 16)
                elif o.signal:
                    ins.then_inc(esem[eng_name], 1)

        block = stack.enter_context(nc.Block())

        @block.tensor
        def _(e):
            run("pe", e)

        @block.scalar
        def _(e):
            run("act", e)

        @block.vector
        def _(e):
            run("dve", e)

        @block.gpsimd
        def _(e):
            run("pool", e)

        @block.sync
        def _(e):
            run("sp", e)

    def barrier(self):
        last = {}
        for o in self.ops:
            last[o.eng if o.dma_key is None else ("d", o.dma_key)] = o.idx
        deps = set(last.values())
        for e in self.ENGS:
            o = self.op(e, lambda en: en.nop())
            o.deps |= deps
            for d in deps:
                k = self.ops[d].dma_key
                if k is not None:
                    o.dma_waits[k] = self.dma_cnt[k]


D = 2048
KC = 16
NE = 16384
EPS = 1e-6
IN_W = 5152


class Cfg:
    def __init__(self, seq, ctx=256):
        self.SEQ = seq
        self.CTX = ctx
        self.TPC = seq // 4
        self.NB = self.TPC // 128
        self.TS = min(4, self.NB)


class Arena:
    def __init__(self, ap, words):
        self.ap = ap
        self.ptr = 0
        self.words = words

    def f32(self, n, name=""):
        off = self.ptr
        self.ptr += n
        assert self.ptr <= self.words, (name, self.ptr, self.words)
        return Tl(self.ap[:, off:off + n], name)

    def bf16(self, n, name=""):
        w = (n + 1) // 2
        off = self.ptr
        self.ptr += w
        assert self.ptr <= self.words, (name, self.ptr, self.words)
        return Tl(self.ap[:, off:off + w].bitcast(BF16), name)


def v3(ap, a):
    return ap.rearrange("p (a b) -> p a b", a=a)


class KB:
    def __init__(self, nc, fw):
        self.nc = nc
        self.fw = fw

    def dma(self, q, out_ap, in_ap, key, reads=(), writes=(), nc_ok=False):
        nc = self.nc

        def fn(e):
            if nc_ok:
                with nc.allow_non_contiguous_dma(reason="small strided load"):
                    return e.dma_start(out=out_ap, in_=in_ap)
            return e.dma_start(out=out_ap, in_=in_ap)
        return self.fw.op(q, fn, reads, writes, dma_key=key)

    def act(self, out, in_, func, reads, writes, **kw):
        return self.fw.op("act", lambda e: e.activation(out=out, in_=in_, func=func, **kw), reads, writes)

    def acts(self, specs, reads, writes):
        def fn(e):
            ins = None
            for (out, in_, func, kw) in specs:
                ins = e.activation(out=out, in_=in_, func=func, **kw)
            return ins
        return self.fw.op("act", fn, reads, writes)

    def tt(self, eng, out, in0, in1, op, reads, writes):
        return self.fw.op(eng, lambda e: e.tensor_tensor(out=out, in0=in0, in1=in1, op=op), reads, writes)

    def ts(self, eng, out, in0, s1, s2, op0, op1, reads, writes):
        if s2 is None:
            return self.fw.op(eng, lambda e: e.tensor_scalar(out=out, in0=in0, scalar1=s1, scalar2=None, op0=op0), reads, writes)
        return self.fw.op(eng, lambda e: e.tensor_scalar(out=out, in0=in0, scalar1=s1, scalar2=s2, op0=op0, op1=op1), reads, writes)

    def stt(self, eng, out, in0, scalar, in1, op0, op1, reads, writes):
        return self.fw.op(eng, lambda e: e.scalar_tensor_tensor(out=out, in0=in0, scalar=scalar, in1=in1, op0=op0, op1=op1), reads, writes)

    def stts(self, eng, specs, reads, writes):
        def fn(e):
            ins = None
            for (out, in0, scalar, in1, op0, op1) in specs:
                ins = e.scalar_tensor_tensor(out=out, in0=in0, scalar=scalar, in1=in1, op0=op0, op1=op1)
            return ins
        return self.fw.op(eng, fn, reads, writes)

    def copy(self, eng, out, in_, reads, writes):
        if eng == "act":
            return self.fw.op("act", lambda e: e.activation(out=out, in_=in_, func=AF.Copy), reads, writes)
        return self.fw.op(eng, lambda e: e.tensor_copy(out=out, in_=in_), reads, writes)

    def memset(self, eng, out, val, writes):
        return self.fw.op(eng, lambda e: e.memset(out, val), (), writes)

    def mm(self, specs, reads, writes):
        def fn(e):
            ins = None
            for (out, lhsT, rhs, start, stop) in specs:
                ins = e.matmul(out, lhsT=lhsT, rhs=rhs, start=start, stop=stop)
            return ins
        return self.fw.op("pe", fn, reads, writes)

    def tr(self, specs, reads, writes):
        def fn(e):
            ins = None
            for (out, in_, ident) in specs:
                ins = e.transpose(out=out, in_=in_, identity=ident)
            return ins
        return self.fw.op("pe", fn, reads, writes)

    def reduce(self, eng, out, in_, axis, op, reads, writes):
        nc = self.nc

        def fn(e):
            with nc.allow_low_precision(reason="bf16 gate sum feeds a bf16 matmul operand"):
                return e.tensor_reduce(out=out, in_=in_, axis=axis, op=op)
        return self.fw.op(eng, fn, reads, writes)

    def recip(self, out, in_, reads, writes):
        return self.fw.op("dve", lambda e: e.reciprocal(out=out, in_=in_), reads, writes)

    def rstd(self, out, ss, n, tmp, reads, writes):
        self.ts("dve", tmp, ss, 1.0 / n, EPS, ALU.mult, ALU.add, reads, writes)
        self.act(tmp, tmp, AF.Sqrt, writes, writes)
        self.recip(out, tmp, writes, writes)


AW = 53200


def build_program(cfg, debug=False):
    from contextlib import ExitStack
    NB, TPC, TS = cfg.NB, cfg.TPC, cfg.TS
    nc = bass.Bass("TRN2", target_bir_lowering=False)

    def din(name, shape, dt=F32):
        return nc.dram_tensor(name, list(shape), dt, kind="ExternalInput").ap()

    def dscr(name, shape, dt):
        return nc.dram_tensor(name, list(shape), dt, kind="ExternalOutput" if debug else "Internal").ap()

    xm = din("xm", [TPC, D])
    xo = din("xo", [3 * TPC, D])
    ctxb = din("ctxb", [cfg.CTX, D])
    cvec = din("cvec", [D])
    cctx = din("cctx", [D])
    flags = din("flags", [128, 8])
    norm1_g = din("norm1_g", [D])
    norm2_g = din("norm2_g", [D])
    w_mod = din("w_mod", [D, 6 * D])
    b_mod = din("b_mod", [6 * D])
    w_in = din("w_in", [D, IN_W])
    w_gate_up = din("w_gate_up", [2, 16, 512])
    b_gate = din("b_gate", [2, 512])
    gla_norm_g = din("gla_norm_g", [1024])
    cmlp_ln_g = din("cmlp_ln_g", [1024])
    cmlp_ln_b = din("cmlp_ln_b", [1024])
    w_spatial = din("w_spatial", [8, 128, 128])
    b_spatial = din("b_spatial", [8, 128])
    w_out = din("w_out", [D, D])
    peer_wq = din("peer_wq", [D, D])
    peer_sk = din("peer_sk", [2, 8, 128, 128])
    peer_u = din("peer_u", [NE, D])
    peer_v = din("peer_v", [NE, D])
    final_g = din("final_g", [D])
    y = nc.dram_tensor("y", [TPC, D], F32, kind="ExternalOutput").ap()

    of_d = dscr("of_d", [TPC, 1024], F32)
    mix_d = dscr("mix_d", [TPC, D], BF16)
    h1_d = dscr("h1_d", [TPC, D], F32)
    xn2T_d = dscr("xn2T_d", [NB, 128, D], BF16)
    st_nega = dscr("st_nega", [NB, 128, 1024], F32)
    st_s2 = dscr("st_s2", [NB, 128, 1024], F32)
    st_ea = dscr("st_ea", [NB, 128, 1024], BF16)
    st_eb = dscr("st_eb", [NB, 128, 1024], BF16)
    UT_d = dscr("UT_d", [32, 128, 8192], BF16)
    V_d = dscr("V_d", [NE, D], BF16)

    st = ExitStack()
    with st:
        fw = FW(nc)
        kb = KB(nc, fw)
        arena_t = st.enter_context(nc.sbuf_tensor("arena", [128, AW], F32))
        psum_t = st.enter_context(nc.psum_tensor("psum", [128, 4096], F32))
        ar = Arena(arena_t, AW)
        PB = [Tl(psum_t[:, b * 512:(b + 1) * 512], "B%d" % b) for b in range(8)]

        def pf(b0, nb=1):
            return psum_t[:, b0 * 512:(b0 + nb) * 512]

        def pbf(b0, nb=1):
            return psum_t[:, b0 * 512:(b0 + nb) * 512].bitcast(BF16)

        ident_f = ar.f32(128, "ident_f")
        A_le = ar.f32(128, "A_le")
        A_ge = ar.f32(128, "A_ge")
        A_gt = ar.f32(128, "A_gt")
        A_lt = ar.f32(128, "A_lt")
        ident_b = ar.bf16(128, "ident_b")
        cols = ar.f32(16 * 8, "cols")
        fl = ar.f32(8, "fl")
        fl2 = ar.f32(24, "fl2")
        g2_bc = ar.f32(D, "g2_bc")

        def tri(tl, cmp, sign):
            kb.memset("pool", tl.ap, 0.0, [tl])
            fw.op("pool", lambda e: e.affine_select(out=tl.ap, in_=tl.ap, pattern=[[-sign, 128]], compare_op=cmp,
                                                    fill=1.0, base=0, channel_multiplier=sign), [tl], [tl])
        tri(ident_f, ALU.not_equal, 1)
        tri(A_le, ALU.is_gt, 1)
        tri(A_ge, ALU.is_gt, -1)
        kb.tt("dve", A_gt.ap, A_ge.ap, ident_f.ap, ALU.subtract, [A_ge, ident_f], [A_gt])
        kb.tt("dve", A_lt.ap, A_le.ap, ident_f.ap, ALU.subtract, [A_le, ident_f], [A_lt])
        kb.copy("dve", ident_b.ap, ident_f.ap, [ident_f], [ident_b])
        kb.dma("sp", fl.ap, flags[:, :], "misc", (), [fl])
        kb.ts("dve", fl2[:, 0:8], fl.ap, -1.0 / 16.0, None, ALU.mult, None, [fl], [fl2])
        kb.ts("dve", fl2[:, 16:24], fl.ap, -1.0, 1.0, ALU.mult, ALU.add, [fl], [fl2])
        kb.ts("dve", fl2[:, 8:16], fl2[:, 16:24], -1.0 / 16.0, None, ALU.mult, None, [fl2], [fl2])
        colv = v3(cols.ap, 8)
        C_G1, C_SH1, C_CG1, C_CSH1, C_G2, C_SH2, C_N1, C_N2 = range(8)
        const_end_p6 = ar.ptr
        g1_bc = ar.f32(D, "g1_bc")
        const_end = ar.ptr

        M = ar.f32(6 * D, "M")
        Mc = ar.f32(2 * D, "Mc")
        rep = ar.f32(2 * KC * 128, "rep")
        ccol = ar.f32(32, "ccol")
        wb = [ar.f32(KC * 512, "wb0"), ar.f32(KC * 512, "wb1")]
        repv = rep.ap.rearrange("p (t k m) -> p t k m", t=2, k=KC)
        kb.dma("sp", ccol[:, 0:16], cvec.rearrange("(k p) -> p k", p=128), "misc", (), [ccol], nc_ok=True)
        kb.dma("sp", ccol[:, 16:32], cctx.rearrange("(k p) -> p k", p=128), "misc", (), [ccol], nc_ok=True)
        kb.dma("sp", colv[:, C_N1, :], norm1_g.rearrange("(k p) -> p k", p=128), "misc", (), [cols], nc_ok=True)
        kb.dma("sp", colv[:, C_N2, :], norm2_g.rearrange("(k p) -> p k", p=128), "misc", (), [cols], nc_ok=True)
        kb.dma("sp", M.ap, b_mod.partition_broadcast(128), "misc", (), [M])
        kb.dma("sp", Mc.ap, b_mod[0:2 * D].partition_broadcast(128), "misc", (), [Mc])
        kb.act(ccol.ap, ccol.ap, AF.Silu, [ccol], [ccol])
        kb.copy("dve", repv[:, 0], ccol[:, 0:16].unsqueeze(2).to_broadcast([128, KC, 128]), [ccol], [rep])
        kb.copy("dve", repv[:, 1], ccol[:, 16:32].unsqueeze(2).to_broadcast([128, KC, 128]), [ccol], [rep])
        wmv = w_mod.rearrange("(k p) n -> p k n", p=128)
        for cb in range(24):
            w = wb[cb % 2]
            kb.dma("sp", v3(w.ap, KC), wmv[:, :, cb * 512:(cb + 1) * 512], "wb%d" % (cb % 2), (), [w])
            bk = cb % 2
            kb.mm([(pf(bk), repv[:, 0, k, :], v3(w.ap, KC)[:, k, :], k == 0, k == KC - 1) for k in range(KC)],
                  [rep, w], [PB[bk]])
            kb.tt("dve", M[:, cb * 512:(cb + 1) * 512], pf(bk), M[:, cb * 512:(cb + 1) * 512], ALU.add, [PB[bk], M], [M])
            if cb < 8:
                bk2 = 2 + cb % 2
                kb.mm([(pf(bk2), repv[:, 1, k, :], v3(w.ap, KC)[:, k, :], k == 0, k == KC - 1) for k in range(KC)],
                      [rep, w], [PB[bk2]])
                kb.tt("dve", Mc[:, cb * 512:(cb + 1) * 512], pf(bk2), Mc[:, cb * 512:(cb + 1) * 512], ALU.add, [PB[bk2], Mc], [Mc])
        tmpc = ar.f32(6 * 16, "tmpc")
        tmpcv = v3(tmpc.ap, 6)
        srcs = [(M, 0), (M, 1), (M, 3), (M, 4), (Mc, 0), (Mc, 1)]
        for i, (src, ch) in enumerate(srcs):
            kb.tr([(pf(4, 4)[:, k * 128:(k + 1) * 128], src[:, ch * D + k * 128: ch * D + (k + 1) * 128], ident_f.ap)
                   for k in range(KC)], [src, ident_f], PB[4:8])
            kb.copy("dve", tmpcv[:, i, :], v3(pf(4, 4), KC)[:, :, 0], PB[4:8], [tmpc])
        kb.copy("act", g1_bc.ap, M[:, 2 * D:3 * D], [M], [g1_bc])
        kb.copy("act", g2_bc.ap, M[:, 5 * D:6 * D], [M], [g2_bc])
        for (dst, sc_i, n_i) in ((C_G1, 1, C_N1), (C_G2, 3, C_N2), (C_CG1, 5, C_N1)):
            kb.stt("dve", colv[:, dst, :], tmpcv[:, sc_i, :], 1.0, colv[:, n_i, :], ALU.add, ALU.mult, [tmpc, cols], [cols])
        for (dst, sh_i) in ((C_SH1, 0), (C_SH2, 2), (C_CSH1, 4)):
            kb.copy("dve", colv[:, dst, :], tmpcv[:, sh_i, :], [tmpc], [cols])
        fw.barrier()
        ar.ptr = const_end

        class FE:
            def __init__(self, tb0):
                self.xt = [ar.f32(D, "xt0"), ar.f32(D, "xt1")]
                self.xs = ar.bf16(D, "xs")
                self.xnT = ar.bf16(D, "xnT")
                self.stt_ = ar.f32(4, "fe_st")
                self.n = 0
                self.tb0 = tb0

            def load(self, rows_ap, key="xt"):
                t = self.xt[self.n % 2]
                kb.dma("sp", t.ap, rows_ap, "%s%d" % (key, self.n % 2), (), [t])
                return t

            def norm_T(self, t, gi, si):
                s = self.stt_
                tb0 = self.tb0
                kb.act(self.xs.ap, t.ap, AF.Square, [t], [self.xs, s], accum_out=s[:, 0:1])
                kb.ts("dve", s[:, 1:2], s[:, 0:1], 1.0 / D, EPS, ALU.mult, ALU.add, [s], [s])
                kb.act(s[:, 1:2], s[:, 1:2], AF.Sqrt, [s], [s])
                kb.recip(s[:, 2:3], s[:, 1:2], [s], [s])
                kb.ts("dve", self.xs.ap, t.ap, s[:, 2:3], None, ALU.mult, None, [t, s], [self.xs])
                pv = v3(pbf(tb0, 2), KC)
                kb.tr([(pv[:, k, :], self.xs[:, k * 128:(k + 1) * 128], ident_b.ap) for k in range(KC)],
                      [self.xs, ident_b], PB[tb0:tb0 + 2])
                xv = v3(self.xnT.ap, KC)
                kb.acts([(xv[:, k, :], pv[:, k, :], AF.Identity,
                          dict(scale=colv[:, gi, k:k + 1], bias=colv[:, si, k:k + 1])) for k in range(KC)],
                        PB[tb0:tb0 + 2] + [cols], [self.xnT])
                self.n += 1
                return xv

        GW = 3104
        wg = ar.bf16(KC * GW, "wg")
        wgv = v3(wg.ap, KC)
        kb.dma("pool", wgv, w_in.rearrange("(k p) n -> p k n", p=128)[:, :, 0:GW], "wg", (), [wg])
        Wga = [ar.f32(512, "Wga0"), ar.f32(512, "Wga1")]
        for d in range(2):
            kb.memset("pool", Wga[d].ap, 0.0, [Wga[d]])
            kb.dma("sp", Wga[d][16 * d:16 * d + 16, :], w_gate_up[d], "misc", (), [Wga[d]])
            kb.dma("sp", Wga[d][32:33, :], b_gate[d:d + 1, :], "misc", (), [Wga[d]])
        ngbc = ar.f32(1024, "ngbc")
        kb.dma("sp", ngbc.ap, gla_norm_g.partition_broadcast(128), "misc", (), [ngbc])
        S = [ar.f32(1024, "S_f"), ar.f32(1024, "S_b")]
        Sbf = ar.bf16(1024, "Sbf")
        for d in range(2):
            kb.memset("pool", S[d].ap, 0.0, [S[d]])
        fe = FE(6)
        qT = ar.f32(512, "qT")
        kT = ar.f32(512, "kT")
        k_sb = ar.f32(512, "k_sb")
        v_sb = ar.bf16(1024, "v_sb")
        lrT = ar.f32(128, "lrT")
        e1 = ar.f32(512, "e1")
        la = ar.f32(512, "la")
        ecT = ar.f32(512, "ecT")
        encT = ar.f32(512, "encT")
        ercum = ar.f32(512, "ercum")
        kdec = ar.bf16(512, "kdec")
        qd = ar.bf16(512, "qd")
        ki = ar.bf16(512, "ki")
        scm = ar.bf16(512, "scm")
        o_sb = ar.f32(1024, "o_sb")
        of_sb = ar.f32(1024, "of_sb")
        silug = ar.f32(1024, "silug")
        ybf = ar.bf16(1024, "ybf")
        gst = ar.f32(16, "gst")
        kb.memset("pool", lrT.ap, 1.0, [lrT])
        ONE_NF16 = fl2[:, 3:4]
        ONE_F = fl[:, 3:4]

        TRI = {0: (A_le, A_gt, 127), 1: (A_ge, A_lt, 0)}

        def inproj_state(xv):
            kb.mm([(pf(2), xv[:, k, :], wgv[:, k, 512:1024], k == 0, k == KC - 1) for k in range(KC)],
                  [fe.xnT, wg], [PB[2]])
            kb.copy("act", k_sb.ap, pf(2), [PB[2]], [k_sb])
            for hf in range(2):
                kb.mm([(pf(3 + hf), xv[:, k, :], wgv[:, k, 1024 + hf * 512:1536 + hf * 512], k == 0, k == KC - 1)
                       for k in range(KC)], [fe.xnT, wg], [PB[3 + hf]])
            kb.copy("act", v_sb.ap, pf(3, 2), PB[3:5], [v_sb])
            kb.mm([(pf(5)[0:32, 0:128], wgv[:, k, 3072:3104], xv[:, k, :], k == 0, k == KC - 1) for k in range(KC)],
                  [fe.xnT, wg], [PB[5]])
            kb.copy("dve", lrT[0:32, :], pf(5)[0:32, 0:128], [PB[5]], [lrT])

        def decay_parts(d, nf16_ap):
            cumm, rcm, _ = TRI[d]
            kb.mm([(pf(2), lrT[0:33, :], Wga[d][0:33, :], True, True)], [lrT, Wga[d]], [PB[2]])
            kb.act(e1.ap, pf(2), AF.Exp, [PB[2]], [e1], scale=-1.0)
            kb.act(e1.ap, e1.ap, AF.Ln, [e1], [e1], bias=1.0)
            kb.ts("dve", la.ap, e1.ap, nf16_ap, None, ALU.mult, None, [e1, fl2], [la])
            kb.mm([(pf(5), rcm.ap, la.ap, True, True)], [rcm, la], [PB[5]])
            kb.mm([(pf(1)[:, h * 128:(h + 1) * 128], la[:, h * 128:(h + 1) * 128], cumm.ap, True, True)
                   for h in range(4)], [la, cumm], [PB[1]])
            kb.act(ecT.ap, pf(1), AF.Exp, [PB[1]], [ecT])
            kb.act(ercum.ap, pf(5), AF.Exp, [PB[5]], [ercum])

        def state_update(d, f_ap):
            col = TRI[d][2]
            kb.stt("dve", kdec.ap, ercum.ap, f_ap, k_sb.ap, ALU.mult, ALU.mult, [ercum, k_sb, fl, fl2], [kdec])
            kb.mm([(pf(3, 2)[:, h * 256:(h + 1) * 256], kdec[:, h * 128:(h + 1) * 128],
                    v_sb[:, h * 256:(h + 1) * 256], True, True) for h in range(4)], [kdec, v_sb], PB[3:5])
            kb.stts("dve", [(S[d][:, h * 256:(h + 1) * 256], S[d][:, h * 256:(h + 1) * 256],
                             ecT[:, h * 128 + col:h * 128 + col + 1], pf(3, 2)[:, h * 256:(h + 1) * 256],
                             ALU.mult, ALU.add) for h in range(4)], [S[d], ecT] + PB[3:5], [S[d]])

        def state_block(rows_ap, gi, si, fcol):
            t = fe.load(rows_ap)
            xv = fe.norm_T(t, gi, si)
            inproj_state(xv)
            decay_parts(0, fl2[:, fcol:fcol + 1])
            state_update(0, fl[:, fcol:fcol + 1])
            decay_parts(1, fl2[:, 8 + fcol:9 + fcol])
            state_update(1, fl2[:, 16 + fcol:17 + fcol])

        def main_block(d, blk):
            cumm, rcm, col = TRI[d]
            t = fe.load(xm[blk * 128:(blk + 1) * 128, :])
            xv = fe.norm_T(t, C_G1, C_SH1)
            if d == 1:
                for hf in range(2):
                    kb.mm([(pf(6 + hf), xv[:, k, :], wgv[:, k, 2048 + hf * 512:2560 + hf * 512], k == 0, k == KC - 1)
                           for k in range(KC)], [fe.xnT, wg], [PB[6 + hf]])
                kb.act(silug.ap, pf(6, 2), AF.Silu, PB[6:8], [silug])
                kb.dma("sp", of_sb.ap, of_d[blk * 128:(blk + 1) * 128, :], "of_sb", [ofd_tl[blk]], [of_sb])
            kb.mm([(pf(0)[:, h * 128:(h + 1) * 128], wgv[:, k, h * 128:(h + 1) * 128], xv[:, k, :], k == 0, k == KC - 1)
                   for h in range(4) for k in range(KC)], [fe.xnT, wg], [PB[0]])
            kb.act(qT.ap, pf(0), AF.Identity, [PB[0]], [qT], scale=128.0 ** -0.5)
            kb.mm([(pf(1)[:, h * 128:(h + 1) * 128], wgv[:, k, 512 + h * 128:512 + (h + 1) * 128], xv[:, k, :], k == 0, k == KC - 1)
                   for h in range(4) for k in range(KC)], [fe.xnT, wg], [PB[1]])
            kb.copy("dve", kT.ap, pf(1), [PB[1]], [kT])
            inproj_state(xv)
            decay_parts(d, ONE_NF16)
            kb.act(encT.ap, pf(1), AF.Exp, [PB[1]], [encT], scale=-1.0)
            kb.tt("dve", qd.ap, qT.ap, ecT.ap, ALU.mult, [qT, ecT], [qd])
            kb.tt("dve", ki.ap, kT.ap, encT.ap, ALU.mult, [kT, encT], [ki])
            kb.mm([(pf(0)[:, h * 128:(h + 1) * 128], ki[:, h * 128:(h + 1) * 128], qd[:, h * 128:(h + 1) * 128], True, True)
                   for h in range(4)], [ki, qd], [PB[0]])
            kb.tt("dve", v3(scm.ap, 4), v3(pf(0), 4), cumm.ap.unsqueeze(1).to_broadcast([128, 4, 128]), ALU.mult,
                  [PB[0], cumm], [scm])
            sp = []
            for h in range(4):
                o_ap = pf(6, 2)[:, h * 256:(h + 1) * 256]
                sp.append((o_ap, scm[:, h * 128:(h + 1) * 128], v_sb[:, h * 256:(h + 1) * 256], True, False))
                sp.append((o_ap, qd[:, h * 128:(h + 1) * 128], Sbf[:, h * 256:(h + 1) * 256], False, True))
            kb.mm(sp, [scm, v_sb, qd, Sbf], PB[6:8])
            if d == 0:
                kb.copy("act", o_sb.ap, pf(6, 2), PB[6:8], [o_sb])
                kb.dma("sp", of_d[blk * 128:(blk + 1) * 128, :], o_sb.ap, "of_st", [o_sb], [ofd_tl[blk]])
            else:
                kb.tt("dve", o_sb.ap, pf(6, 2), of_sb.ap, ALU.add, PB[6:8] + [of_sb], [o_sb])
                kb.acts([(of_sb[:, h * 256:(h + 1) * 256], o_sb[:, h * 256:(h + 1) * 256], AF.Square,
                          dict(accum_out=gst[:, h:h + 1])) for h in range(4)], [o_sb], [of_sb, gst])
                kb.ts("dve", gst[:, 4:8], gst[:, 0:4], 1.0 / 256.0, EPS, ALU.mult, ALU.add, [gst], [gst])
                kb.act(gst[:, 4:8], gst[:, 4:8], AF.Sqrt, [gst], [gst])
                kb.recip(gst[:, 8:12], gst[:, 4:8], [gst], [gst])
                kb.stts("dve", [(o_sb[:, h * 256:(h + 1) * 256], o_sb[:, h * 256:(h + 1) * 256], gst[:, 8 + h:9 + h],
                                 ngbc[:, h * 256:(h + 1) * 256], ALU.mult, ALU.mult) for h in range(4)],
                        [o_sb, gst, ngbc], [o_sb])
                kb.tt("dve", ybf.ap, o_sb.ap, silug.ap, ALU.mult, [o_sb, silug], [ybf])
                kb.dma("sp", mix_d[blk * 128:(blk + 1) * 128, 0:1024], ybf.ap, "y_st", [ybf], ())
            state_update(d, ONE_F)
            kb.copy("pool", Sbf.ap, S[d].ap, [S[d]], [Sbf])

        ofd_tl = [Tl(None, 'ofd') for _ in range(NB)]
        nctx = cfg.CTX // 128
        for b in range(nctx):
            state_block(ctxb[b * 128:(b + 1) * 128, :], C_CG1, C_CSH1, 3)
        for b in range(nctx - 1, -1, -1):
            state_block(ctxb[b * 128:(b + 1) * 128, :], C_CG1, C_CSH1, 4)
        for j in range(3):
            for b in range(NB):
                r0 = (j * NB + b) * 128
                state_block(xo[r0:r0 + 128, :], C_G1, C_SH1, j)
        kb.copy("pool", Sbf.ap, S[0].ap, [S[0]], [Sbf])
        for b in range(NB):
            main_block(0, b)
        kb.copy("pool", Sbf.ap, S[1].ap, [S[1]], [Sbf])
        for b in range(NB - 1, -1, -1):
            main_block(1, b)
        fw.barrier()
        ar.ptr = const_end

        wc = ar.bf16(KC * 2048, "wc")
        wcv = v3(wc.ap, KC)
        kb.dma("pool", wcv, w_in.rearrange("(k p) n -> p k n", p=128)[:, :, GW:IN_W], "wc", (), [wc])
        wsT = ar.bf16(1024, "wsT")
        wstg = ar.f32(1024, "wstg")
        bs_col = ar.f32(8, "bs_col")
        lng = ar.f32(1024, "lng")
        lnb = ar.f32(1024, "lnb")
        kb.dma("sp", v3(wstg.ap, 8), w_spatial.rearrange("g p q -> p g q"), "misc", (), [wstg])
        kb.dma("sp", bs_col.ap, b_spatial.rearrange("g p -> p g"), "misc", (), [bs_col], nc_ok=True)
        kb.dma("sp", lng.ap, cmlp_ln_g.partition_broadcast(128), "misc", (), [lng])
        kb.dma("sp", lnb.ap, cmlp_ln_b.partition_broadcast(128), "misc", (), [lnb])
        kb.tr([(pf(0, 2)[:, g * 128:(g + 1) * 128], wstg[:, g * 128:(g + 1) * 128], ident_f.ap) for g in range(8)],
              [wstg, ident_f], PB[0:2])
        kb.copy("dve", wsT.ap, pf(0, 2), PB[0:2], [wsT])
        fe = FE(6)
        gu = ar.f32(1024, "gu")
        gv = ar.f32(1024, "gv")
        vn = ar.bf16(1024, "vn")
        cm = ar.bf16(1024, "cm")
        cst = ar.f32(8, "cst")
        for blk in range(NB):
            t = fe.load(xm[blk * 128:(blk + 1) * 128, :])
            xv = fe.norm_T(t, C_G1, C_SH1)
            for q4 in range(4):
                kb.mm([(pf(q4), xv[:, k, :], wcv[:, k, q4 * 512:(q4 + 1) * 512], k == 0, k == KC - 1) for k in range(KC)],
                      [fe.xnT, wc], [PB[q4]])
            kb.act(gu.ap, pf(0, 2), AF.Gelu, PB[0:2], [gu])
            kb.act(gv.ap, pf(2, 2), AF.Gelu, PB[2:4], [gv, cst], accum_out=cst[:, 0:1])
            kb.ts("dve", cst[:, 1:2], cst[:, 0:1], -1.0 / 1024.0, None, ALU.mult, None, [cst], [cst])
            kb.act(vn.ap, gv.ap, AF.Square, [gv, cst], [vn, cst], bias=cst[:, 1:2], accum_out=cst[:, 2:3])
            kb.ts("dve", cst[:, 3:4], cst[:, 2:3], 1.0 / 1024.0, EPS, ALU.mult, ALU.add, [cst], [cst])
            kb.act(cst[:, 3:4], cst[:, 3:4], AF.Sqrt, [cst], [cst])
            kb.recip(cst[:, 4:5], cst[:, 3:4], [cst], [cst])
            kb.ts("dve", gv.ap, gv.ap, cst[:, 1:2], cst[:, 4:5], ALU.add, ALU.mult, [gv, cst], [gv])
            kb.tt("dve", gv.ap, gv.ap, lng.ap, ALU.mult, [gv, lng], [gv])
            kb.tt("dve", vn.ap, gv.ap, lnb.ap, ALU.add, [gv, lnb], [vn])
            kb.mm([(pf(4, 2)[:, g * 128:(g + 1) * 128], wsT[:, g * 128:(g + 1) * 128], vn[:, g * 128:(g + 1) * 128], True, True)
                   for g in range(8)], [wsT, vn], PB[4:6])
            kb.stts("dve", [(cm[:, g * 128:(g + 1) * 128], pf(4, 2)[:, g * 128:(g + 1) * 128], bs_col[:, g:g + 1],
                             gu[:, g * 128:(g + 1) * 128], ALU.add, ALU.mult) for g in range(8)],
                    PB[4:6] + [bs_col, gu], [cm])
            kb.dma("sp", mix_d[blk * 128:(blk + 1) * 128, 1024:2048], cm.ap, "cm_st", [cm], ())
        fw.barrier()
        ar.ptr = const_end

        wo = ar.bf16(KC * D, "wo")
        wov = v3(wo.ap, KC)
        wq = ar.bf16(KC * D, "wq")
        wqv = v3(wq.ap, KC)
        skT = ar.bf16(D, "skT")
        skTv = v3(skT.ap, 16)
        kb.dma("pool", wqv, peer_wq.rearrange("(k p) n -> p k n", p=128), "wq", (), [wq])
        h1 = ar.f32(D, "h1")
        sc = ar.f32(D, "sc")
        tmp = ar.f32(D, "tmp")
        stg = [h1, sc]
        for k in range(KC):
            sg = stg[k % 2]
            kb.dma("sp", sg.ap, w_out[k * 128:(k + 1) * 128, :], "wo_st%d" % (k % 2), (), [sg])
            kb.tt("dve", wov[:, k, :], sg.ap, g1_bc.ap, ALU.mult, [sg, g1_bc], [wo])
        kb.dma("sp", v3(tmp.ap, 16), peer_sk.rearrange("t h k d -> k (t h) d"), "misc", (), [tmp])
        specs = []
        for half in range(2):
            for hh in range(8):
                c = hh * 2 + half
                specs.append((pf(0, 4)[:, c * 128:(c + 1) * 128], tmp[:, (half * 8 + hh) * 128:(half * 8 + hh + 1) * 128], ident_f.ap))
        kb.tr(specs, [tmp, ident_f], PB[0:4])
        kb.copy("dve", skT.ap, pf(0, 4), PB[0:4], [skT])
        mixT = ar.bf16(D, "mixT")
        mixTv = v3(mixT.ap, KC)
        xt5 = sc
        xn2T = ar.bf16(D, "xn2T")
        xn2v = v3(xn2T.ap, KC)
        qTb = ar.bf16(D, "qTb")
        qTv = v3(qTb.ap, 16)
        v16 = ar.f32(256, "v16")
        v16v = v3(v16.ap, 16)
        cand = ar.f32(D, "cand")
        mix_sb = cand
        mix_ap = cand.ap[:, 0:1024].bitcast(BF16)
        xs2 = mixT
        best = ar.f32(128, "best")
        bestv = v3(best.ap, 8)
        nega = tmp
        nega_ap = tmp.ap[:, 1024:2048]
        eat = tmp
        eat_ap = tmp.ap[:, 0:1024]
        pst = ar.f32(64, "pst")
        exw = ar.f32(128, "exw")
        scv = sc.ap.rearrange("p (h t k) -> p h t k", h=8, t=2)
        v16q = v16.ap.rearrange("p (h t r) -> p h t r", h=8, t=2)
        for blk in range(NB):
            r0 = blk * 128
            kb.dma("sp", mix_ap, mix_d[r0:r0 + 128, :], "mix_ld", (), [mix_sb])
            kb.dma("sp", xt5.ap, xm[r0:r0 + 128, :], "xt5", (), [xt5])
            pv = v3(pbf(0, 2), KC)
            kb.tr([(pv[:, k, :], mix_ap[:, k * 128:(k + 1) * 128], ident_b.ap) for k in range(KC)], [mix_sb, ident_b], PB[0:2])
            kb.copy("act", mixT.ap, pbf(0, 2), PB[0:2], [mixT])
            for q4 in range(4):
                kb.mm([(pf(2 + q4), mixTv[:, k, :], wov[:, k, q4 * 512:(q4 + 1) * 512], k == 0, k == KC - 1) for k in range(KC)],
                      [mixT, wo], [PB[2 + q4]])
            kb.tt("dve", h1.ap, pf(2, 4), xt5.ap, ALU.add, PB[2:6] + [xt5], [h1])
            kb.dma("sp", h1_d[r0:r0 + 128, :], h1.ap, "h1_st", [h1], ())
            kb.act(xs2.ap, h1.ap, AF.Square, [h1], [xs2, pst], accum_out=pst[:, 0:1])
            kb.ts("dve", pst[:, 1:2], pst[:, 0:1], 1.0 / D, EPS, ALU.mult, ALU.add, [pst], [pst])
            kb.act(pst[:, 1:2], pst[:, 1:2], AF.Sqrt, [pst], [pst])
            kb.recip(pst[:, 2:3], pst[:, 1:2], [pst], [pst])
            kb.ts("dve", xs2.ap, h1.ap, pst[:, 2:3], None, ALU.mult, None, [h1, pst], [xs2])
            pv2 = v3(pbf(6, 2), KC)
            kb.tr([(pv2[:, k, :], xs2[:, k * 128:(k + 1) * 128], ident_b.ap) for k in range(KC)], [xs2, ident_b], PB[6:8])
            kb.acts([(xn2v[:, k, :], pv2[:, k, :], AF.Identity,
                      dict(scale=colv[:, C_G2, k:k + 1], bias=colv[:, C_SH2, k:k + 1])) for k in range(KC)],
                    PB[6:8] + [cols], [xn2T])
            kb.dma("sp", xn2T_d[blk], xn2T.ap, "xn2_st", [xn2T], ())
            kb.mm([(pf(2, 4)[:, c * 128:(c + 1) * 128], wqv[:, k, c * 128:(c + 1) * 128], xn2v[:, k, :], k == 0, k == KC - 1)
                   for c in range(16) for k in range(KC)], [wq, xn2T], PB[2:6])
            kb.copy("act", qTb.ap, pf(2, 4), PB[2:6], [qTb])
            kb.mm([(pf(0, 2)[:, c * 128:(c + 1) * 128], qTv[:, c, :], skTv[:, c, :], True, True) for c in range(8)],
                  [qTb, skT], PB[0:2])
            kb.mm([(pf(6, 2)[:, (c - 8) * 128:(c - 7) * 128], qTv[:, c, :], skTv[:, c, :], True, True) for c in range(8, 16)],
                  [qTb, skT], PB[6:8])
            kb.copy("act", sc[:, 0:1024], pf(0, 2), PB[0:2], [sc])
            kb.copy("act", sc[:, 1024:2048], pf(6, 2), PB[6:8], [sc])
            fw.op("dve", lambda e: [e.max(out=v16v[:, c, 0:8], in_=sc[:, c * 128:(c + 1) * 128]) for c in range(16)][-1], [sc], [v16])
            fw.op("dve", lambda e: [e.match_replace(out=tmp[:, c * 128:(c + 1) * 128], in_to_replace=v16v[:, c, 0:8],
                                                    in_values=sc[:, c * 128:(c + 1) * 128], imm_value=-1e30) for c in range(16)][-1],
                  [sc, v16], [tmp])
            fw.op("dve", lambda e: [e.max(out=v16v[:, c, 8:16], in_=tmp[:, c * 128:(c + 1) * 128]) for c in range(16)][-1], [tmp], [v16])
            candv = cand.ap.rearrange("p (h r c) -> p h r c", h=8, r=16)
            kb.tt("dve", candv, v16q[:, :, 0, :].unsqueeze(3).to_broadcast([128, 8, 16, 16]),
                  v16q[:, :, 1, :].unsqueeze(2).to_broadcast([128, 8, 16, 16]), ALU.add, [v16], [cand])
            fw.op("dve", lambda e: [e.max(out=bestv[:, hh, 0:8], in_=cand[:, hh * 256:(hh + 1) * 256]) for hh in range(8)][-1], [cand], [best])
            fw.op("dve", lambda e: [e.match_replace(out=tmp[:, hh * 256:(hh + 1) * 256], in_to_replace=bestv[:, hh, 0:8],
                                                    in_values=cand[:, hh * 256:(hh + 1) * 256], imm_value=-1e30) for hh in range(8)][-1],
                  [cand, best], [tmp])
            fw.op("dve", lambda e: [e.max(out=bestv[:, hh, 8:16], in_=tmp[:, hh * 256:(hh + 1) * 256]) for hh in range(8)][-1], [tmp], [best])
            kb.tt("dve", v3(exw.ap, 8), bestv, bestv[:, :, 0:1].to_broadcast([128, 8, 16]), ALU.subtract, [best], [exw])
            kb.act(exw.ap, exw.ap, AF.Exp, [exw], [exw])
            kb.reduce("dve", pst[:, 8:16], v3(exw.ap, 8), AX.X, ALU.add, [exw], [pst])
            kb.recip(pst[:, 16:24], pst[:, 8:16], [pst], [pst])
            negav = v3(nega_ap, 8)
            kb.tt("dve", negav, bestv[:, :, 15:16].to_broadcast([128, 8, 128]), scv[:, :, 0, :], ALU.subtract, [best, sc], [nega])
            kb.ts("dve", nega_ap, nega_ap, -1e-5, None, ALU.add, None, [nega], [nega])
            kb.dma("sp", st_nega[blk], nega_ap, "st_st", [nega], ())
            kb.dma("sp", v3(st_s2[blk], 8), scv[:, :, 1, :], "st_st", [sc], ())
            eatv = v3(eat_ap, 8)
            kb.tt("dve", eatv, scv[:, :, 0, :], v16q[:, :, 0, 0:1].to_broadcast([128, 8, 128]), ALU.subtract, [sc, v16], [eat])
            kb.act(eat_ap, eat_ap, AF.Exp, [eat], [eat])
            ea_ap = cand.ap[:, 0:512].bitcast(BF16)
            eb_ap = cand.ap[:, 512:1024].bitcast(BF16)
            kb.tt("dve", v3(ea_ap, 8), eatv, pst[:, 16:24].unsqueeze(2).to_broadcast([128, 8, 128]), ALU.mult, [eat, pst], [cand])
            kb.tt("dve", eatv, scv[:, :, 1, :], v16q[:, :, 1, 0:1].to_broadcast([128, 8, 128]), ALU.subtract, [sc, v16], [eat])
            kb.act(eb_ap, eat_ap, AF.Exp, [eat], [cand])
            kb.dma("sp", st_ea[blk], ea_ap, "st_st", [cand], ())
            kb.dma("sp", st_eb[blk], eb_ap, "st_st", [cand], ())
        fw.barrier()
        ar.ptr = const_end

        ust = [ar.bf16(D, "ust0"), ar.bf16(D, "ust1")]
        ugr = [ar.bf16(KC * 512, "ugr0"), ar.bf16(KC * 512, "ugr1")]
        for et in range(128):
            g, sub = divmod(et, 4)
            us = ust[et % 2]
            ug = ugr[g % 2]
            ugv = v3(ug.ap, KC)
            kb.dma("pool", us.ap, peer_u[et * 128:(et + 1) * 128, :], "ust%d" % (et % 2), (), [us])
            kb.dma("pool", V_d[et * 128:(et + 1) * 128, :], peer_v[et * 128:(et + 1) * 128, :], "vcast", (), ())
            bk = 2 * (et % 2)
            pv = v3(pbf(bk, 2), KC)
            kb.tr([(pv[:, k, :], us[:, k * 128:(k + 1) * 128], ident_b.ap) for k in range(KC)], [us, ident_b], PB[bk:bk + 2])
            if et % 2 == 0:
                kb.copy("act", ugv[:, :, sub * 128:(sub + 1) * 128], pv, PB[bk:bk + 2], [ug])
            else:
                kb.copy("dve", ugv[:, :, sub * 128:(sub + 1) * 128], pv, PB[bk:bk + 2], [ug])
            if sub == 3:
                kb.dma("sp", UT_d[g], ug.ap, "ut_st%d" % (g % 2), [ug], ())
        fw.barrier()
        ar.ptr = const_end

        ar.ptr = const_end_p6
        ut_raw = [ar.f32(KC * 256, "ut0"), ar.f32(KC * 256, "ut1")]
        vt_raw = [ar.f32(2 * D, "vt0"), ar.f32(2 * D, "vt1")]
        for t_ in ut_raw + vt_raw:
            t_.ap, t_.name = t_.ap.bitcast(BF16), t_.ap
        ut, vt = ut_raw, vt_raw
        xq = ar.bf16(TS * D, "xq")
        xqv = xq.ap.rearrange("p (t k m) -> p t k m", t=TS, k=KC)
        sn = ar.f32(TS * 1024, "sn")
        s2t = ar.f32(TS * 1024, "s2t")
        eaT = ar.bf16(TS * 1024, "eaT")
        ebT = ar.bf16(TS * 1024, "ebT")
        snv = sn.ap.rearrange("p (t h k) -> p t h k", t=TS, h=8)
        s2v = s2t.ap.rearrange("p (t h k) -> p t h k", t=TS, h=8)
        eav = eaT.ap.rearrange("p (t h k) -> p t h k", t=TS, h=8)
        ebv = ebT.ap.rearrange("p (t h k) -> p t h k", t=TS, h=8)
        acc = ar.f32(TS * D, "acc")
        accv = v3(acc.ap, TS)
        actg = [ar.bf16(512, "actg%d" % i) for i in range(2)]
        Mk = [ar.bf16(4096, "Mk%d" % i) for i in range(3)]
        Gs = [ar.bf16(512, "G0"), ar.bf16(512, "G1")]
        HmT = [ar.bf16(512, "HmT0"), ar.bf16(512, "HmT1")]
        fst = ar.f32(4, "fst")
        fng = vt[1]
        fng_ap = vt[1].name[:, 0:D]
        h2 = ut[0]
        h2_ap = ut[0].name[:, 0:D]
        junk = Mk[0]
        junk_ap = Mk[0].ap[:, 0:D]
        for stile in range(NB // TS):
            b0 = stile * TS
            for tb in range(TS):
                kb.dma("sp", xq[:, tb * D:(tb + 1) * D], xn2T_d[b0 + tb], "xq", (), [xq])
                kb.dma("sp", sn[:, tb * 1024:(tb + 1) * 1024], st_nega[b0 + tb], "sn", (), [sn])
                kb.dma("sp", s2t[:, tb * 1024:(tb + 1) * 1024], st_s2[b0 + tb], "s2t", (), [s2t])
                kb.dma("sp", eaT[:, tb * 1024:(tb + 1) * 1024], st_ea[b0 + tb], "eaT", (), [eaT])
                kb.dma("sp", ebT[:, tb * 1024:(tb + 1) * 1024], st_eb[b0 + tb], "ebT", (), [ebT])
            kb.memset("pool", acc.ap, 0.0, [acc])
            its = [(g, tb) for g in range(32) for tb in range(TS)]
            N = len(its)
            wts = {}

            def load_u(g):
                u = ut[g % 2]
                kb.dma("sp", u.ap, UT_d[g], "ut%d" % (g % 2), (), [u])

            def load_v(g):
                vv = vt[g % 2]
                kb.dma("sp", v3(vv.ap, 4), V_d[g * 512:(g + 1) * 512, :].rearrange("(i e) n -> e i n", e=128),
                       "vt%d" % (g % 2), (), [vv])

            def stA(s_):
                g, tb = its[s_]
                if tb == 0:
                    load_u(g)
                mkv = Mk[s_ % 3].ap.rearrange("p (h i j) -> p h i j", h=8, i=4)
                kb.tt("dve", mkv, s2v[:, tb].unsqueeze(2).to_broadcast([128, 8, 4, 128]),
                      snv[:, tb, :, 4 * g:4 * g + 4].unsqueeze(3).to_broadcast([128, 8, 4, 128]), ALU.is_ge, [s2t, sn], [Mk[s_ % 3]])

            def stB(s_):
                g, tb = its[s_]
                mk = Mk[s_ % 3]
                mkv = mk.ap.rearrange("p (h i j) -> p h i j", h=8, i=4)
                kb.tt("dve", mkv, mkv, ebv[:, tb].unsqueeze(2).to_broadcast([128, 8, 4, 128]), ALU.mult, [mk, ebT], [mk])
                kb.tt("pool", mkv, mkv, eav[:, tb, :, 4 * g:4 * g + 4].unsqueeze(3).to_broadcast([128, 8, 4, 128]), ALU.mult,
                      [mk, eaT], [mk])

            def stM(s_):
                g, tb = its[s_]
                uv = v3(ut[g % 2].ap, KC)
                p = s_ % 2
                kb.mm([(pf(p), xqv[:, tb, k, :], uv[:, k, :], k == 0, k == KC - 1) for k in range(KC)], [xq, ut[g % 2]], [PB[p]])
                kb.act(actg[p].ap, pf(p), AF.Gelu, [PB[p]], [actg[p]])

            def stC(s_):
                mk = Mk[s_ % 3]
                gs = Gs[s_ % 2]
                kb.tt("dve", mk[:, 0:2048], mk[:, 0:2048], mk[:, 2048:4096], ALU.add, [mk], [mk])
                kb.tt("dve", mk[:, 0:1024], mk[:, 0:1024], mk[:, 1024:2048], ALU.add, [mk], [mk])
                kb.tt("dve", gs.ap, mk[:, 0:512], mk[:, 512:1024], ALU.add, [mk], [gs])
                kb.tt("pool", gs.ap, gs.ap, actg[s_ % 2].ap, ALU.mult, [gs, actg[s_ % 2]], [gs])

            def stD(s_):
                g, tb = its[s_]
                p = s_ % 2
                gs, hT = Gs[p], HmT[p]
                vvv = v3(vt[g % 2].ap, 4)
                kb.tr([(pbf(2 + p)[:, i * 128:(i + 1) * 128], gs[:, i * 128:(i + 1) * 128], ident_b.ap) for i in range(4)],
                      [gs, ident_b], [PB[2 + p]])
                kb.copy("act", hT.ap, pbf(2 + p)[:, 0:512], [PB[2 + p]], [hT])
                kb.mm([(pf(4 + fq), hT[:, i * 128:(i + 1) * 128], vvv[:, i, fq * 512:(fq + 1) * 512], i == 0, i == 3)
                       for fq in range(4) for i in range(4)], [hT, vt[g % 2]], PB[4:8])
                kb.tt("dve", accv[:, tb, :], pf(4, 4), accv[:, tb, :], ALU.add, PB[4:8] + [acc], [acc])

            load_v(0)
            load_v(1)
            for s_ in range(N + 4):
                if 0 <= s_ - 3 < N:
                    stC(s_ - 3)
                if s_ < N:
                    stA(s_)
                if 0 <= s_ - 1 < N:
                    stB(s_ - 1)
                if 0 <= s_ - 4 < N:
                    stD(s_ - 4)
                    g_, tb_ = its[s_ - 4]
                    if tb_ == TS - 1 and g_ + 2 < 32:
                        load_v(g_ + 2)
                if 0 <= s_ - 2 < N:
                    stM(s_ - 2)
            kb.dma("sp", fng_ap, final_g.partition_broadcast(128), "fng", (), [fng])
            for tb in range(TS):
                r0 = (b0 + tb) * 128
                kb.dma("sp", h2_ap, h1_d[r0:r0 + 128, :], "h2_ld", (), [h2])
                kb.tt("dve", accv[:, tb, :], accv[:, tb, :], g2_bc.ap, ALU.mult, [acc, g2_bc], [acc])
                kb.tt("dve", h2_ap, h2_ap, accv[:, tb, :], ALU.add, [h2, acc], [h2])
                kb.act(junk_ap, h2_ap, AF.Square, [h2], [junk, fst], accum_out=fst[:, 0:1])
                kb.ts("dve", fst[:, 1:2], fst[:, 0:1], 1.0 / D, EPS, ALU.mult, ALU.add, [fst], [fst])
                kb.act(fst[:, 1:2], fst[:, 1:2], AF.Sqrt, [fst], [fst])
                kb.recip(fst[:, 2:3], fst[:, 1:2], [fst], [fst])
                kb.stt("dve", h2_ap, h2_ap, fst[:, 2:3], fng_ap, ALU.mult, ALU.mult, [h2, fst, fng], [h2])
                kb.dma("sp", y[r0:r0 + 128, :], h2_ap, "y_out", [h2], ())
        fw.barrier()
        fw.emit(st)
    return nc


_PROG_CACHE = {}


def make_in_maps(cfg, inp):
    NB, T = cfg.NB, cfg.TPC
    f32 = lambda a: np.ascontiguousarray(np.asarray(a, dtype=np.float32))
    shared = dict(
        cctx=f32(inp["c_ctx"]), norm1_g=f32(inp["norm1_g"][0]), norm2_g=f32(inp["norm2_g"][0]),
        w_mod=f32(inp["w_mod"][0]), b_mod=f32(inp["b_mod"][0]), w_in=f32(inp["w_in"][0]),
        w_gate_up=f32(inp["w_gate_up"][0]), b_gate=f32(inp["b_gate"][0]), gla_norm_g=f32(inp["gla_norm_g"][0]),
        cmlp_ln_g=f32(inp["cmlp_ln_g"][0]), cmlp_ln_b=f32(inp["cmlp_ln_b"][0]), w_spatial=f32(inp["w_spatial"][0]),
        b_spatial=f32(inp["b_spatial"][0]), w_out=f32(inp["w_out"][0]), peer_wq=f32(inp["peer_wq"][0]),
        peer_sk=f32(inp["peer_sub_keys"][0]), peer_u=f32(inp["peer_u"][0]), peer_v=f32(inp["peer_v"][0]),
        final_g=f32(inp["final_norm_g"]),
    )
    x = np.asarray(inp["x"], dtype=np.float32)
    ctx = np.asarray(inp["ctx"], dtype=np.float32)
    c = np.asarray(inp["c"], dtype=np.float32)
    maps = []
    for core in range(8):
        b, seg = divmod(core, 4)
        xb = x[b]
        slots = [(s, 1.0) for s in range(seg)] + [(s, 0.0) for s in range(3, seg, -1)]
        flags = np.zeros((128, 8), np.float32)
        parts = []
        for j, (s, f) in enumerate(slots):
            blocks = xb[s * T:(s + 1) * T].reshape(NB, 128, D)
            if f == 0.0:
                blocks = blocks[::-1]
            parts.append(blocks.reshape(T, D))
            flags[:, j] = f
        flags[:, 3] = 1.0
        m = dict(shared)
        m.update(xm=f32(xb[seg * T:(seg + 1) * T]), xo=f32(np.concatenate(parts, 0)), ctxb=f32(ctx[b]),
                 cvec=f32(c[b]), flags=flags)
        maps.append(m)
    return maps


def kernel(**inp):
    seq = int(np.asarray(inp["x"]).shape[1])
    cfg = Cfg(seq, int(np.asarray(inp["ctx"]).shape[1]))
    if seq not in _PROG_CACHE:
        _PROG_CACHE[seq] = build_program(cfg)
    nc = _PROG_CACHE[seq]
    maps = make_in_maps(cfg, inp)
    res = run_bass_kernel_spmd(nc, maps, core_ids=list(range(8)))
    out = np.empty((2, seq, D), np.float32)
    for core in range(8):
        b, seg = divmod(core, 4)
        out[b, seg * cfg.TPC:(seg + 1) * cfg.TPC] = res.results[core]["y"]
    return out
```

```python
import numpy as np
import concourse.bass as bass
import concourse.mybir as mybir
from concourse.bass_utils import run_bass_kernel_spmd

F32 = mybir.dt.float32
BF16 = mybir.dt.bfloat16
ALU = mybir.AluOpType
AF = mybir.ActivationFunctionType
AX = mybir.AxisListType


class Tl:
    __slots__ = ("ap", "name", "w", "r")

    def __init__(self, ap, name=""):
        self.ap = ap
        self.name = name
        self.w = None
        self.r = {}

    def __getitem__(self, k):
        return self.ap[k]


class Op:
    __slots__ = ("idx", "eng", "fn", "deps", "dma_key", "val", "signal", "dma_waits")


class FW:
    ENGS = ("pe", "act", "dve", "pool", "sp")

    def __init__(self, nc):
        self.nc = nc
        self.ops = []
        self.dma_cnt = {}

    def op(self, eng, fn, reads=(), writes=(), dma_key=None):
        o = Op()
        o.idx = len(self.ops)
        o.eng = eng
        o.fn = fn
        o.dma_key = dma_key
        o.signal = False
        o.val = 0
        deps = set()
        for t in reads:
            if t.w is not None:
                deps.add(t.w)
        for t in writes:
            if t.w is not None:
                deps.add(t.w)
            deps.update(t.r.values())
        o.deps = deps
        o.dma_waits = {}
        for d in deps:
            od = self.ops[d]
            if od.dma_key is not None:
                o.dma_waits[od.dma_key] = self.dma_cnt[od.dma_key]
        if dma_key is not None:
            self.dma_cnt[dma_key] = self.dma_cnt.get(dma_key, 0) + 16
            o.val = self.dma_cnt[dma_key]
        rkey = ("dma", dma_key) if dma_key is not None else eng
        for t in reads:
            t.r[rkey] = o.idx
        for t in writes:
            t.w = o.idx
            t.r = {}
        self.ops.append(o)
        return o

    def emit(self, stack):
        nc = self.nc
        ops = self.ops
        for o in ops:
            for d in o.deps:
                if ops[d].dma_key is None:
                    ops[d].signal = True
        cnt = {e: 0 for e in self.ENGS}
        for o in ops:
            if o.dma_key is None and o.signal:
                cnt[o.eng] += 1
                o.val = cnt[o.eng]
        esem = {e: stack.enter_context(nc.semaphore("s_" + e)) for e in self.ENGS}
        dsem = {k: stack.enter_context(nc.semaphore("d_%d" % i))
                for i, k in enumerate(self.dma_cnt)}
        streams = {e: [o for o in ops if o.eng == e] for e in self.ENGS}

        def run(eng_name, e):
            waited = {}
            for o in streams[eng_name]:
                waits = {}
                for d in o.deps:
                    od = ops[d]
                    if od.dma_key is not None:
                        key = ("d", od.dma_key)
                        v = o.dma_waits[od.dma_key]
                    else:
                        if od.eng == eng_name and eng_name == "pe":
                            continue
                        key = ("e", od.eng)
                        v = od.val
                    if waits.get(key, 0) < v:
                        waits[key] = v
                for key, v in waits.items():
                    if waited.get(key, 0) >= v:
                        continue
                    s = dsem[key[1]] if key[0] == "d" else esem[key[1]]
                    e.wait_ge(s, v)
                    waited[key] = v
                ins = o.fn(e)
                if o.dma_key is not None:
                    ins.then_inc(dsem[o.dma_key], 16)
                elif o.signal:
                    ins.then_inc(esem[eng_name], 1)

        block = stack.enter_context(nc.Block())

        @block.tensor
        def _(e):
            run("pe", e)

        @block.scalar
        def _(e):
            run("act", e)

        @block.vector
        def _(e):
            run("dve", e)

        @block.gpsimd
        def _(e):
            run("pool", e)

        @block.sync
        def _(e):
            run("sp", e)

    def barrier(self):
        last = {}
        for o in self.ops:
            last[o.eng if o.dma_key is None else ("d", o.dma_key)] = o.idx
        deps = set(last.values())
        for e in self.ENGS:
            o = self.op(e, lambda en: en.nop())
            o.deps |= deps
            for d in deps:
                k = self.ops[d].dma_key
                if k is not None:
                    o.dma_waits[k] = self.dma_cnt[k]


D = 2048
KC = 16
NE = 16384
EPS = 1e-6
IN_W = 5152


class Cfg:
    def __init__(self, seq, ctx=256):
        self.SEQ = seq
        self.CTX = ctx
        self.TPC = seq // 4
        self.NB = self.TPC // 128
        self.TS = min(4, self.NB)


class Arena:
    def __init__(self, ap, words):
        self.ap = ap
        self.ptr = 0
        self.words = words

    def f32(self, n, name=""):
        off = self.ptr
        self.ptr += n
        assert self.ptr <= self.words, (name, self.ptr, self.words)
        return Tl(self.ap[:, off:off + n], name)

    def bf16(self, n, name=""):
        w = (n + 1) // 2
        off = self.ptr
        self.ptr += w
        assert self.ptr <= self.words, (name, self.ptr, self.words)
        return Tl(self.ap[:, off:off + w].bitcast(BF16), name)


def v3(ap, a):
    return ap.rearrange("p (a b) -> p a b", a=a)


class KB:
    def __init__(self, nc, fw):
        self.nc = nc
        self.fw = fw

    def dma(self, q, out_ap, in_ap, key, reads=(), writes=(), nc_ok=False):
        nc = self.nc

        def fn(e):
            if nc_ok:
                with nc.allow_non_contiguous_dma(reason="small strided load"):
                    return e.dma_start(out=out_ap, in_=in_ap)
            return e.dma_start(out=out_ap, in_=in_ap)
        return self.fw.op(q, fn, reads, writes, dma_key=key)

    def act(self, out, in_, func, reads, writes, **kw):
        return self.fw.op("act", lambda e: e.activation(out=out, in_=in_, func=func, **kw), reads, writes)

    def acts(self, specs, reads, writes):
        def fn(e):
            ins = None
            for (out, in_, func, kw) in specs:
                ins = e.activation(out=out, in_=in_, func=func, **kw)
            return ins
        return self.fw.op("act", fn, reads, writes)

    def tt(self, eng, out, in0, in1, op, reads, writes):
        return self.fw.op(eng, lambda e: e.tensor_tensor(out=out, in0=in0, in1=in1, op=op), reads, writes)

    def ts(self, eng, out, in0, s1, s2, op0, op1, reads, writes):
        if s2 is None:
            return self.fw.op(eng, lambda e: e.tensor_scalar(out=out, in0=in0, scalar1=s1, scalar2=None, op0=op0), reads, writes)
        return self.fw.op(eng, lambda e: e.tensor_scalar(out=out, in0=in0, scalar1=s1, scalar2=s2, op0=op0, op1=op1), reads, writes)

    def stt(self, eng, out, in0, scalar, in1, op0, op1, reads, writes):
        return self.fw.op(eng, lambda e: e.scalar_tensor_tensor(out=out, in0=in0, scalar=scalar, in1=in1, op0=op0, op1=op1), reads, writes)

    def stts(self, eng, specs, reads, writes):
        def fn(e):
            ins = None
            for (out, in0, scalar, in1, op0, op1) in specs:
                ins = e.scalar_tensor_tensor(out=out, in0=in0, scalar=scalar, in1=in1, op0=op0, op1=op1)
            return ins
        return self.fw.op(eng, fn, reads, writes)

    def copy(self, eng, out, in_, reads, writes):
        if eng == "act":
            return self.fw.op("act", lambda e: e.activation(out=out, in_=in_, func=AF.Copy), reads, writes)
        return self.fw.op(eng, lambda e: e.tensor_copy(out=out, in_=in_), reads, writes)

    def memset(self, eng, out, val, writes):
        return self.fw.op(eng, lambda e: e.memset(out, val), (), writes)

    def mm(self, specs, reads, writes):
        def fn(e):
            ins = None
            for (out, lhsT, rhs, start, stop) in specs:
                ins = e.matmul(out, lhsT=lhsT, rhs=rhs, start=start, stop=stop)
            return ins
        return self.fw.op("pe", fn, reads, writes)

    def tr(self, specs, reads, writes):
        def fn(e):
            ins = None
            for (out, in_, ident) in specs:
                ins = e.transpose(out=out, in_=in_, identity=ident)
            return ins
        return self.fw.op("pe", fn, reads, writes)

    def reduce(self, eng, out, in_, axis, op, reads, writes):
        nc = self.nc

        def fn(e):
            with nc.allow_low_precision(reason="bf16 gate sum feeds a bf16 matmul operand"):
                return e.tensor_reduce(out=out, in_=in_, axis=axis, op=op)
        return self.fw.op(eng, fn, reads, writes)

    def recip(self, out, in_, reads, writes):
        return self.fw.op("dve", lambda e: e.reciprocal(out=out, in_=in_), reads, writes)

    def rstd(self, out, ss, n, tmp, reads, writes):
        self.ts("dve", tmp, ss, 1.0 / n, EPS, ALU.mult, ALU.add, reads, writes)
        self.act(tmp, tmp, AF.Sqrt, writes, writes)
        self.recip(out, tmp, writes, writes)


AW = 53200


def build_program(cfg, debug=False):
    from contextlib import ExitStack
    NB, TPC, TS = cfg.NB, cfg.TPC, cfg.TS
    nc = bass.Bass("TRN2", target_bir_lowering=False)

    def din(name, shape, dt=F32):
        return nc.dram_tensor(name, list(shape), dt, kind="ExternalInput").ap()

    def dscr(name, shape, dt):
        return nc.dram_tensor(name, list(shape), dt, kind="ExternalOutput" if debug else "Internal").ap()

    xm = din("xm", [TPC, D])
    xo = din("xo", [3 * TPC, D])
    ctxb = din("ctxb", [cfg.CTX, D])
    cvec = din("cvec", [D])
    cctx = din("cctx", [D])
    flags = din("flags", [128, 8])
    norm1_g = din("norm1_g", [D])
    norm2_g = din("norm2_g", [D])
    w_mod = din("w_mod", [D, 6 * D])
    b_mod = din("b_mod", [6 * D])
    w_in = din("w_in", [D, IN_W])
    w_gate_up = din("w_gate_up", [2, 16, 512])
    b_gate = din("b_gate", [2, 512])
    gla_norm_g = din("gla_norm_g", [1024])
    cmlp_ln_g = din("cmlp_ln_g", [1024])
    cmlp_ln_b = din("cmlp_ln_b", [1024])
    w_spatial = din("w_spatial", [8, 128, 128])
    b_spatial = din("b_spatial", [8, 128])
    w_out = din("w_out", [D, D])
    peer_wq = din("peer_wq", [D, D])
    peer_sk = din("peer_sk", [2, 8, 128, 128])
    peer_u = din("peer_u", [NE, D])
    peer_v = din("peer_v", [NE, D])
    final_g = din("final_g", [D])
    y = nc.dram_tensor("y", [TPC, D], F32, kind="ExternalOutput").ap()

    of_d = dscr("of_d", [TPC, 1024], F32)
    mix_d = dscr("mix_d", [TPC, D], BF16)
    h1_d = dscr("h1_d", [TPC, D], F32)
    xn2T_d = dscr("xn2T_d", [NB, 128, D], BF16)
    st_nega = dscr("st_nega", [NB, 128, 1024], F32)
    st_s2 = dscr("st_s2", [NB, 128, 1024], F32)
    st_ea = dscr("st_ea", [NB, 128, 1024], BF16)
    st_eb = dscr("st_eb", [NB, 128, 1024], BF16)
    UT_d = dscr("UT_d", [32, 128, 8192], BF16)
    V_d = dscr("V_d", [NE, D], BF16)

    st = ExitStack()
    with st:
        fw = FW(nc)
        kb = KB(nc, fw)
        arena_t = st.enter_context(nc.sbuf_tensor("arena", [128, AW], F32))
        psum_t = st.enter_context(nc.psum_tensor("psum", [128, 4096], F32))
        ar = Arena(arena_t, AW)
        PB = [Tl(psum_t[:, b * 512:(b + 1) * 512], "B%d" % b) for b in range(8)]

        def pf(b0, nb=1):
            return psum_t[:, b0 * 512:(b0 + nb) * 512]

        def pbf(b0, nb=1):
            return psum_t[:, b0 * 512:(b0 + nb) * 512].bitcast(BF16)

        ident_f = ar.f32(128, "ident_f")
        A_le = ar.f32(128, "A_le")
        A_ge = ar.f32(128, "A_ge")
        A_gt = ar.f32(128, "A_gt")
        A_lt = ar.f32(128, "A_lt")
        ident_b = ar.bf16(128, "ident_b")
        cols = ar.f32(16 * 8, "cols")
        fl = ar.f32(8, "fl")
        fl2 = ar.f32(24, "fl2")
        g2_bc = ar.f32(D, "g2_bc")

        def tri(tl, cmp, sign):
            kb.memset("pool", tl.ap, 0.0, [tl])
            fw.op("pool", lambda e: e.affine_select(out=tl.ap, in_=tl.ap, pattern=[[-sign, 128]], compare_op=cmp,
                                                    fill=1.0, base=0, channel_multiplier=sign), [tl], [tl])
        tri(ident_f, ALU.not_equal, 1)
        tri(A_le, ALU.is_gt, 1)
        tri(A_ge, ALU.is_gt, -1)
        kb.tt("dve", A_gt.ap, A_ge.ap, ident_f.ap, ALU.subtract, [A_ge, ident_f], [A_gt])
        kb.tt("dve", A_lt.ap, A_le.ap, ident_f.ap, ALU.subtract, [A_le, ident_f], [A_lt])
        kb.copy("dve", ident_b.ap, ident_f.ap, [ident_f], [ident_b])
        kb.dma("sp", fl.ap, flags[:, :], "misc", (), [fl])
        kb.ts("dve", fl2[:, 0:8], fl.ap, -1.0 / 16.0, None, ALU.mult, None, [fl], [fl2])
        kb.ts("dve", fl2[:, 16:24], fl.ap, -1.0, 1.0, ALU.mult, ALU.add, [fl], [fl2])
        kb.ts("dve", fl2[:, 8:16], fl2[:, 16:24], -1.0 / 16.0, None, ALU.mult, None, [fl2], [fl2])
        colv = v3(cols.ap, 8)
        C_G1, C_SH1, C_CG1, C_CSH1, C_G2, C_SH2, C_N1, C_N2 = range(8)
        const_end_p6 = ar.ptr
        g1_bc = ar.f32(D, "g1_bc")
        const_end = ar.ptr

        M = ar.f32(6 * D, "M")
        Mc = ar.f32(2 * D, "Mc")
        rep = ar.f32(2 * KC * 128, "rep")
        ccol = ar.f32(32, "ccol")
        wb = [ar.f32(KC * 512, "wb0"), ar.f32(KC * 512, "wb1")]
        repv = rep.ap.rearrange("p (t k m) -> p t k m", t=2, k=KC)
        kb.dma("sp", ccol[:, 0:16], cvec.rearrange("(k p) -> p k", p=128), "misc", (), [ccol], nc_ok=True)
        kb.dma("sp", ccol[:, 16:32], cctx.rearrange("(k p) -> p k", p=128), "misc", (), [ccol], nc_ok=True)
        kb.dma("sp", colv[:, C_N1, :], norm1_g.rearrange("(k p) -> p k", p=128), "misc", (), [cols], nc_ok=True)
        kb.dma("sp", colv[:, C_N2, :], norm2_g.rearrange("(k p) -> p k", p=128), "misc", (), [cols], nc_ok=True)
        kb.dma("sp", M.ap, b_mod.partition_broadcast(128), "misc", (), [M])
        kb.dma("sp", Mc.ap, b_mod[0:2 * D].partition_broadcast(128), "misc", (), [Mc])
        kb.act(ccol.ap, ccol.ap, AF.Silu, [ccol], [ccol])
        kb.copy("dve", repv[:, 0], ccol[:, 0:16].unsqueeze(2).to_broadcast([128, KC, 128]), [ccol], [rep])
        kb.copy("dve", repv[:, 1], ccol[:, 16:32].unsqueeze(2).to_broadcast([128, KC, 128]), [ccol], [rep])
        wmv = w_mod.rearrange("(k p) n -> p k n", p=128)
        for cb in range(24):
            w = wb[cb % 2]
            kb.dma("sp", v3(w.ap, KC), wmv[:, :, cb * 512:(cb + 1) * 512], "wb%d" % (cb % 2), (), [w])
            bk = cb % 2
            kb.mm([(pf(bk), repv[:, 0, k, :], v3(w.ap, KC)[:, k, :], k == 0, k == KC - 1) for k in range(KC)],
                  [rep, w], [PB[bk]])
            kb.tt("dve", M[:, cb * 512:(cb + 1) * 512], pf(bk), M[:, cb * 512:(cb + 1) * 512], ALU.add, [PB[bk], M], [M])
            if cb < 8:
                bk2 = 2 + cb % 2
                kb.mm([(pf(bk2), repv[:, 1, k, :], v3(w.ap, KC)[:, k, :], k == 0, k == KC - 1) for k in range(KC)],
                      [rep, w], [PB[bk2]])
                kb.tt("dve", Mc[:, cb * 512:(cb + 1) * 512], pf(bk2), Mc[:, cb * 512:(cb + 1) * 512], ALU.add, [PB[bk2], Mc], [Mc])
        tmpc = ar.f32(6 * 16, "tmpc")
        tmpcv = v3(tmpc.ap, 6)
        srcs = [(M, 0), (M, 1), (M, 3), (M, 4), (Mc, 0), (Mc, 1)]
        for i, (src, ch) in enumerate(srcs):
            kb.tr([(pf(4, 4)[:, k * 128:(k + 1) * 128], src[:, ch * D + k * 128: ch * D + (k + 1) * 128], ident_f.ap)
                   for k in range(KC)], [src, ident_f], PB[4:8])
            kb.copy("dve", tmpcv[:, i, :], v3(pf(4, 4), KC)[:, :, 0], PB[4:8], [tmpc])
        kb.copy("act", g1_bc.ap, M[:, 2 * D:3 * D], [M], [g1_bc])
        kb.copy("act", g2_bc.ap, M[:, 5 * D:6 * D], [M], [g2_bc])
        for (dst, sc_i, n_i) in ((C_G1, 1, C_N1), (C_G2, 3, C_N2), (C_CG1, 5, C_N1)):
            kb.stt("dve", colv[:, dst, :], tmpcv[:, sc_i, :], 1.0, colv[:, n_i, :], ALU.add, ALU.mult, [tmpc, cols], [cols])
        for (dst, sh_i) in ((C_SH1, 0), (C_SH2, 2), (C_CSH1, 4)):
            kb.copy("dve", colv[:, dst, :], tmpcv[:, sh_i, :], [tmpc], [cols])
        fw.barrier()
        ar.ptr = const_end

        class FE:
            def __init__(self, tb0):
                self.xt = [ar.f32(D, "xt0"), ar.f32(D, "xt1")]
                self.xs = ar.bf16(D, "xs")
                self.xnT = ar.bf16(D, "xnT")
                self.stt_ = ar.f32(4, "fe_st")
                self.n = 0
                self.tb0 = tb0

            def load(self, rows_ap, key="xt"):
                t = self.xt[self.n % 2]
                kb.dma("sp", t.ap, rows_ap, "%s%d" % (key, self.n % 2), (), [t])
                return t

            def norm_T(self, t, gi, si):
                s = self.stt_
                tb0 = self.tb0
                kb.act(self.xs.ap, t.ap, AF.Square, [t], [self.xs, s], accum_out=s[:, 0:1])
                kb.ts("dve", s[:, 1:2], s[:, 0:1], 1.0 / D, EPS, ALU.mult, ALU.add, [s], [s])
                kb.act(s[:, 1:2], s[:, 1:2], AF.Sqrt, [s], [s])
                kb.recip(s[:, 2:3], s[:, 1:2], [s], [s])
                kb.ts("dve", self.xs.ap, t.ap, s[:, 2:3], None, ALU.mult, None, [t, s], [self.xs])
                pv = v3(pbf(tb0, 2), KC)
                kb.tr([(pv[:, k, :], self.xs[:, k * 128:(k + 1) * 128], ident_b.ap) for k in range(KC)],
                      [self.xs, ident_b], PB[tb0:tb0 + 2])
                xv = v3(self.xnT.ap, KC)
                kb.acts([(xv[:, k, :], pv[:, k, :], AF.Identity,
                          dict(scale=colv[:, gi, k:k + 1], bias=colv[:, si, k:k + 1])) for k in range(KC)],
                        PB[tb0:tb0 + 2] + [cols], [self.xnT])
                self.n += 1
                return xv

        GW = 3104
        wg = ar.bf16(KC * GW, "wg")
        wgv = v3(wg.ap, KC)
        kb.dma("pool", wgv, w_in.rearrange("(k p) n -> p k n", p=128)[:, :, 0:GW], "wg", (), [wg])
        Wga = [ar.f32(512, "Wga0"), ar.f32(512, "Wga1")]
        for d in range(2):
            kb.memset("pool", Wga[d].ap, 0.0, [Wga[d]])
            kb.dma("sp", Wga[d][16 * d:16 * d + 16, :], w_gate_up[d], "misc", (), [Wga[d]])
            kb.dma("sp", Wga[d][32:33, :], b_gate[d:d + 1, :], "misc", (), [Wga[d]])
        ngbc = ar.f32(1024, "ngbc")
        kb.dma("sp", ngbc.ap, gla_norm_g.partition_broadcast(128), "misc", (), [ngbc])
        S = [ar.f32(1024, "S_f"), ar.f32(1024, "S_b")]
        Sbf = ar.bf16(1024, "Sbf")
        for d in range(2):
            kb.memset("pool", S[d].ap, 0.0, [S[d]])
        fe = FE(6)
        qT = ar.f32(512, "qT")
        kT = ar.f32(512, "kT")
        k_sb = ar.f32(512, "k_sb")
        v_sb = ar.bf16(1024, "v_sb")
        lrT = ar.f32(128, "lrT")
        e1 = ar.f32(512, "e1")
        la = ar.f32(512, "la")
        ecT = ar.f32(512, "ecT")
        encT = ar.f32(512, "encT")
        ercum = ar.f32(512, "ercum")
        kdec = ar.bf16(512, "kdec")
        qd = ar.bf16(512, "qd")
        ki = ar.bf16(512, "ki")
        scm = ar.bf16(512, "scm")
        o_sb = ar.f32(1024, "o_sb")
        of_sb = ar.f32(1024, "of_sb")
        silug = ar.f32(1024, "silug")
        ybf = ar.bf16(1024, "ybf")
        gst = ar.f32(16, "gst")
        kb.memset("pool", lrT.ap, 1.0, [lrT])
        ONE_NF16 = fl2[:, 3:4]
        ONE_F = fl[:, 3:4]

        TRI = {0: (A_le, A_gt, 127), 1: (A_ge, A_lt, 0)}

        def inproj_state(xv):
            kb.mm([(pf(2), xv[:, k, :], wgv[:, k, 512:1024], k == 0, k == KC - 1) for k in range(KC)],
                  [fe.xnT, wg], [PB[2]])
            kb.copy("act", k_sb.ap, pf(2), [PB[2]], [k_sb])
            for hf in range(2):
                kb.mm([(pf(3 + hf), xv[:, k, :], wgv[:, k, 1024 + hf * 512:1536 + hf * 512], k == 0, k == KC - 1)
                       for k in range(KC)], [fe.xnT, wg], [PB[3 + hf]])
            kb.copy("act", v_sb.ap, pf(3, 2), PB[3:5], [v_sb])
            kb.mm([(pf(5)[0:32, 0:128], wgv[:, k, 3072:3104], xv[:, k, :], k == 0, k == KC - 1) for k in range(KC)],
                  [fe.xnT, wg], [PB[5]])
            kb.copy("dve", lrT[0:32, :], pf(5)[0:32, 0:128], [PB[5]], [lrT])

        def decay_parts(d, nf16_ap):
            cumm, rcm, _ = TRI[d]
            kb.mm([(pf(2), lrT[0:33, :], Wga[d][0:33, :], True, True)], [lrT, Wga[d]], [PB[2]])
            kb.act(e1.ap, pf(2), AF.Exp, [PB[2]], [e1], scale=-1.0)
            kb.act(e1.ap, e1.ap, AF.Ln, [e1], [e1], bias=1.0)
            kb.ts("dve", la.ap, e1.ap, nf16_ap, None, ALU.mult, None, [e1, fl2], [la])
            kb.mm([(pf(5), rcm.ap, la.ap, True, True)], [rcm, la], [PB[5]])
            kb.mm([(pf(1)[:, h * 128:(h + 1) * 128], la[:, h * 128:(h + 1) * 128], cumm.ap, True, True)
                   for h in range(4)], [la, cumm], [PB[1]])
            kb.act(ecT.ap, pf(1), AF.Exp, [PB[1]], [ecT])
            kb.act(ercum.ap, pf(5), AF.Exp, [PB[5]], [ercum])

        def state_update(d, f_ap):
            col = TRI[d][2]
            kb.stt("dve", kdec.ap, ercum.ap, f_ap, k_sb.ap, ALU.mult, ALU.mult, [ercum, k_sb, fl, fl2], [kdec])
            kb.mm([(pf(3, 2)[:, h * 256:(h + 1) * 256], kdec[:, h * 128:(h + 1) * 128],
                    v_sb[:, h * 256:(h + 1) * 256], True, True) for h in range(4)], [kdec, v_sb], PB[3:5])
            kb.stts("dve", [(S[d][:, h * 256:(h + 1) * 256], S[d][:, h * 256:(h + 1) * 256],
                             ecT[:, h * 128 + col:h * 128 + col + 1], pf(3, 2)[:, h * 256:(h + 1) * 256],
                             ALU.mult, ALU.add) for h in range(4)], [S[d], ecT] + PB[3:5], [S[d]])

        def state_block(rows_ap, gi, si, fcol):
            t = fe.load(rows_ap)
            xv = fe.norm_T(t, gi, si)
            inproj_state(xv)
            decay_parts(0, fl2[:, fcol:fcol + 1])
            state_update(0, fl[:, fcol:fcol + 1])
            decay_parts(1, fl2[:, 8 + fcol:9 + fcol])
            state_update(1, fl2[:, 16 + fcol:17 + fcol])

        def main_block(d, blk):
            cumm, rcm, col = TRI[d]
            t = fe.load(xm[blk * 128:(blk + 1) * 128, :])
            xv = fe.norm_T(t, C_G1, C_SH1)
            if d == 1:
                for hf in range(2):
                    kb.mm([(pf(6 + hf), xv[:, k, :], wgv[:, k, 2048 + hf * 512:2560 + hf * 512], k == 0, k == KC - 1)
                           for k in range(KC)], [fe.xnT, wg], [PB[6 + hf]])
                kb.act(silug.ap, pf(6, 2), AF.Silu, PB[6:8], [silug])
                kb.dma("sp", of_sb.ap, of_d[blk * 128:(blk + 1) * 128, :], "of_sb", [ofd_tl[blk]], [of_sb])
            kb.mm([(pf(0)[:, h * 128:(h + 1) * 128], wgv[:, k, h * 128:(h + 1) * 128], xv[:, k, :], k == 0, k == KC - 1)
                   for h in range(4) for k in range(KC)], [fe.xnT, wg], [PB[0]])
            kb.act(qT.ap, pf(0), AF.Identity, [PB[0]], [qT], scale=128.0 ** -0.5)
            kb.mm([(pf(1)[:, h * 128:(h + 1) * 128], wgv[:, k, 512 + h * 128:512 + (h + 1) * 128], xv[:, k, :], k == 0, k == KC - 1)
                   for h in range(4) for k in range(KC)], [fe.xnT, wg], [PB[1]])
            kb.copy("dve", kT.ap, pf(1), [PB[1]], [kT])
            inproj_state(xv)
            decay_parts(d, ONE_NF16)
            kb.act(encT.ap, pf(1), AF.Exp, [PB[1]], [encT], scale=-1.0)
            kb.tt("dve", qd.ap, qT.ap, ecT.ap, ALU.mult, [qT, ecT], [qd])
            kb.tt("dve", ki.ap, kT.ap, encT.ap, ALU.mult, [kT, encT], [ki])
            kb.mm([(pf(0)[:, h * 128:(h + 1) * 128], ki[:, h * 128:(h + 1) * 128], qd[:, h * 128:(h + 1) * 128], True, True)
                   for h in range(4)], [ki, qd], [PB[0]])
            kb.tt("dve", v3(scm.ap, 4), v3(pf(0), 4), cumm.ap.unsqueeze(1).to_broadcast([128, 4, 128]), ALU.mult,
                  [PB[0], cumm], [scm])
            sp = []
            for h in range(4):
                o_ap = pf(6, 2)[:, h * 256:(h + 1) * 256]
                sp.append((o_ap, scm[:, h * 128:(h + 1) * 128], v_sb[:, h * 256:(h + 1) * 256], True, False))
                sp.append((o_ap, qd[:, h * 128:(h + 1) * 128], Sbf[:, h * 256:(h + 1) * 256], False, True))
            kb.mm(sp, [scm, v_sb, qd, Sbf], PB[6:8])
            if d == 0:
                kb.copy("act", o_sb.ap, pf(6, 2), PB[6:8], [o_sb])
                kb.dma("sp", of_d[blk * 128:(blk + 1) * 128, :], o_sb.ap, "of_st", [o_sb], [ofd_tl[blk]])
            else:
                kb.tt("dve", o_sb.ap, pf(6, 2), of_sb.ap, ALU.add, PB[6:8] + [of_sb], [o_sb])
                kb.acts([(of_sb[:, h * 256:(h + 1) * 256], o_sb[:, h * 256:(h + 1) * 256], AF.Square,
                          dict(accum_out=gst[:, h:h + 1])) for h in range(4)], [o_sb], [of_sb, gst])
                kb.ts("dve", gst[:, 4:8], gst[:, 0:4], 1.0 / 256.0, EPS, ALU.mult, ALU.add, [gst], [gst])
                kb.act(gst[:, 4:8], gst[:, 4:8], AF.Sqrt, [gst], [gst])
                kb.recip(gst[:, 8:12], gst[:, 4:8], [gst], [gst])
                kb.stts("dve", [(o_sb[:, h * 256:(h + 1) * 256], o_sb[:, h * 256:(h + 1) * 256], gst[:, 8 + h:9 + h],
                                 ngbc[:, h * 256:(h + 1) * 256], ALU.mult, ALU.mult) for h in range(4)],
                        [o_sb, gst, ngbc], [o_sb])
                kb.tt("dve", ybf.ap, o_sb.ap, silug.ap, ALU.mult, [o_sb, silug], [ybf])
                kb.dma("sp", mix_d[blk * 128:(blk + 1) * 128, 0:1024], ybf.ap, "y_st", [ybf], ())
            state_update(d, ONE_F)
            kb.copy("pool", Sbf.ap, S[d].ap, [S[d]], [Sbf])

        ofd_tl = [Tl(None, 'ofd') for _ in range(NB)]
        nctx = cfg.CTX // 128
        for b in range(nctx):
            state_block(ctxb[b * 128:(b + 1) * 128, :], C_CG1, C_CSH1, 3)
        for b in range(nctx - 1, -1, -1):
            state_block(ctxb[b * 128:(b + 1) * 128, :], C_CG1, C_CSH1, 4)
        for j in range(3):
            for b in range(NB):
                r0 = (j * NB + b) * 128
                state_block(xo[r0:r0 + 128, :], C_G1, C_SH1, j)
        kb.copy("pool", Sbf.ap, S[0].ap, [S[0]], [Sbf])
        for b in range(NB):
            main_block(0, b)
        kb.copy("pool", Sbf.ap, S[1].ap, [S[1]], [Sbf])
        for b in range(NB - 1, -1, -1):
            main_block(1, b)
        fw.barrier()
        ar.ptr = const_end

        wc = ar.bf16(KC * 2048, "wc")
        wcv = v3(wc.ap, KC)
        kb.dma("pool", wcv, w_in.rearrange("(k p) n -> p k n", p=128)[:, :, GW:IN_W], "wc", (), [wc])
        wsT = ar.bf16(1024, "wsT")
        wstg = ar.f32(1024, "wstg")
        bs_col = ar.f32(8, "bs_col")
        lng = ar.f32(1024, "lng")
        lnb = ar.f32(1024, "lnb")
        kb.dma("sp", v3(wstg.ap, 8), w_spatial.rearrange("g p q -> p g q"), "misc", (), [wstg])
        kb.dma("sp", bs_col.ap, b_spatial.rearrange("g p -> p g"), "misc", (), [bs_col], nc_ok=True)
        kb.dma("sp", lng.ap, cmlp_ln_g.partition_broadcast(128), "misc", (), [lng])
        kb.dma("sp", lnb.ap, cmlp_ln_b.partition_broadcast(128), "misc", (), [lnb])
        kb.tr([(pf(0, 2)[:, g * 128:(g + 1) * 128], wstg[:, g * 128:(g + 1) * 128], ident_f.ap) for g in range(8)],
              [wstg, ident_f], PB[0:2])
        kb.copy("dve", wsT.ap, pf(0, 2), PB[0:2], [wsT])
        fe = FE(6)
        gu = ar.f32(1024, "gu")
        gv = ar.f32(1024, "gv")
        vn = ar.bf16(1024, "vn")
        cm = ar.bf16(1024, "cm")
        cst = ar.f32(8, "cst")
        for blk in range(NB):
            t = fe.load(xm[blk * 128:(blk + 1) * 128, :])
            xv = fe.norm_T(t, C_G1, C_SH1)
            for q4 in range(4):
                kb.mm([(pf(q4), xv[:, k, :], wcv[:, k, q4 * 512:(q4 + 1) * 512], k == 0, k == KC - 1) for k in range(KC)],
                      [fe.xnT, wc], [PB[q4]])
            kb.act(gu.ap, pf(0, 2), AF.Gelu, PB[0:2], [gu])
            kb.act(gv.ap, pf(2, 2), AF.Gelu, PB[2:4], [gv, cst], accum_out=cst[:, 0:1])
            kb.ts("dve", cst[:, 1:2], cst[:, 0:1], -1.0 / 1024.0, None, ALU.mult, None, [cst], [cst])
            kb.act(vn.ap, gv.ap, AF.Square, [gv, cst], [vn, cst], bias=cst[:, 1:2], accum_out=cst[:, 2:3])
            kb.ts("dve", cst[:, 3:4], cst[:, 2:3], 1.0 / 1024.0, EPS, ALU.mult, ALU.add, [cst], [cst])
            kb.act(cst[:, 3:4], cst[:, 3:4], AF.Sqrt, [cst], [cst])
            kb.recip(cst[:, 4:5], cst[:, 3:4], [cst], [cst])
            kb.ts("dve", gv.ap, gv.ap, cst[:, 1:2], cst[:, 4:5], ALU.add, ALU.mult, [gv, cst], [gv])
            kb.tt("dve", gv.ap, gv.ap, lng.ap, ALU.mult, [gv, lng], [gv])
            kb.tt("dve", vn.ap, gv.ap, lnb.ap, ALU.add, [gv, lnb], [vn])
            kb.mm([(pf(4, 2)[:, g * 128:(g + 1) * 128], wsT[:, g * 128:(g + 1) * 128], vn[:, g * 128:(g + 1) * 128], True, True)
                   for g in range(8)], [wsT, vn], PB[4:6])
            kb.stts("dve", [(cm[:, g * 128:(g + 1) * 128], pf(4, 2)[:, g * 128:(g + 1) * 128], bs_col[:, g:g + 1],
                             gu[:, g * 128:(g + 1) * 128], ALU.add, ALU.mult) for g in range(8)],
                    PB[4:6] + [bs_col, gu], [cm])
            kb.dma("sp", mix_d[blk * 128:(blk + 1) * 128, 1024:2048], cm.ap, "cm_st", [cm], ())
        fw.barrier()
        ar.ptr = const_end

        wo = ar.bf16(KC * D, "wo")
        wov = v3(wo.ap, KC)
        wq = ar.bf16(KC * D, "wq")
        wqv = v3(wq.ap, KC)
        skT = ar.bf16(D, "skT")
        skTv = v3(skT.ap, 16)
        kb.dma("pool", wqv, peer_wq.rearrange("(k p) n -> p k n", p=128), "wq", (), [wq])
        h1 = ar.f32(D, "h1")
        sc = ar.f32(D, "sc")
        tmp = ar.f32(D, "tmp")
        stg = [h1, sc]
        for k in range(KC):
            sg = stg[k % 2]
            kb.dma("sp", sg.ap, w_out[k * 128:(k + 1) * 128, :], "wo_st%d" % (k % 2), (), [sg])
            kb.tt("dve", wov[:, k, :], sg.ap, g1_bc.ap, ALU.mult, [sg, g1_bc], [wo])
        kb.dma("sp", v3(tmp.ap, 16), peer_sk.rearrange("t h k d -> k (t h) d"), "misc", (), [tmp])
        specs = []
        for half in range(2):
            for hh in range(8):
                c = hh * 2 + half
                specs.append((pf(0, 4)[:, c * 128:(c + 1) * 128], tmp[:, (half * 8 + hh) * 128:(half * 8 + hh + 1) * 128], ident_f.ap))
        kb.tr(specs, [tmp, ident_f], PB[0:4])
        kb.copy("dve", skT.ap, pf(0, 4), PB[0:4], [skT])
        mixT = ar.bf16(D, "mixT")
        mixTv = v3(mixT.ap, KC)
        xt5 = sc
        xn2T = ar.bf16(D, "xn2T")
        xn2v = v3(xn2T.ap, KC)
        qTb = ar.bf16(D, "qTb")
        qTv = v3(qTb.ap, 16)
        v16 = ar.f32(256, "v16")
        v16v = v3(v16.ap, 16)
        cand = ar.f32(D, "cand")
        mix_sb = cand
        mix_ap = cand.ap[:, 0:1024].bitcast(BF16)
        xs2 = mixT
        best = ar.f32(128, "best")
        bestv = v3(best.ap, 8)
        nega = tmp
        nega_ap = tmp.ap[:, 1024:2048]
        eat = tmp
        eat_ap = tmp.ap[:, 0:1024]
        pst = ar.f32(64, "pst")
        exw = ar.f32(128, "exw")
        scv = sc.ap.rearrange("p (h t k) -> p h t k", h=8, t=2)
        v16q = v16.ap.rearrange("p (h t r) -> p h t r", h=8, t=2)
        for blk in range(NB):
            r0 = blk * 128
            kb.dma("sp", mix_ap, mix_d[r0:r0 + 128, :], "mix_ld", (), [mix_sb])
            kb.dma("sp", xt5.ap, xm[r0:r0 + 128, :], "xt5", (), [xt5])
            pv = v3(pbf(0, 2), KC)
            kb.tr([(pv[:, k, :], mix_ap[:, k * 128:(k + 1) * 128], ident_b.ap) for k in range(KC)], [mix_sb, ident_b], PB[0:2])
            kb.copy("act", mixT.ap, pbf(0, 2), PB[0:2], [mixT])
            for q4 in range(4):
                kb.mm([(pf(2 + q4), mixTv[:, k, :], wov[:, k, q4 * 512:(q4 + 1) * 512], k == 0, k == KC - 1) for k in range(KC)],
                      [mixT, wo], [PB[2 + q4]])
            kb.tt("dve", h1.ap, pf(2, 4), xt5.ap, ALU.add, PB[2:6] + [xt5], [h1])
            kb.dma("sp", h1_d[r0:r0 + 128, :], h1.ap, "h1_st", [h1], ())
            kb.act(xs2.ap, h1.ap, AF.Square, [h1], [xs2, pst], accum_out=pst[:, 0:1])
            kb.ts("dve", pst[:, 1:2], pst[:, 0:1], 1.0 / D, EPS, ALU.mult, ALU.add, [pst], [pst])
            kb.act(pst[:, 1:2], pst[:, 1:2], AF.Sqrt, [pst], [pst])
            kb.recip(pst[:, 2:3], pst[:, 1:2], [pst], [pst])
            kb.ts("dve", xs2.ap, h1.ap, pst[:, 2:3], None, ALU.mult, None, [h1, pst], [xs2])
            pv2 = v3(pbf(6, 2), KC)
            kb.tr([(pv2[:, k, :], xs2[:, k * 128:(k + 1) * 128], ident_b.ap) for k in range(KC)], [xs2, ident_b], PB[6:8])
            kb.acts([(xn2v[:, k, :], pv2[:, k, :], AF.Identity,
                      dict(scale=colv[:, C_G2, k:k + 1], bias=colv[:, C_SH2, k:k + 1])) for k in range(KC)],
                    PB[6:8] + [cols], [xn2T])
            kb.dma("sp", xn2T_d[blk], xn2T.ap, "xn2_st", [xn2T], ())
            kb.mm([(pf(2, 4)[:, c * 128:(c + 1) * 128], wqv[:, k, c * 128:(c + 1) * 128], xn2v[:, k, :], k == 0, k == KC - 1)
                   for c in range(16) for k in range(KC)], [wq, xn2T], PB[2:6])
            kb.copy("act", qTb.ap, pf(2, 4), PB[2:6], [qTb])
            kb.mm([(pf(0, 2)[:, c * 128:(c + 1) * 128], qTv[:, c, :], skTv[:, c, :], True, True) for c in range(8)],
                  [qTb, skT], PB[0:2])
            kb.mm([(pf(6, 2)[:, (c - 8) * 128:(c - 7) * 128], qTv[:, c, :], skTv[:, c, :], True, True) for c in range(8, 16)],
                  [qTb, skT], PB[6:8])
            kb.copy("act", sc[:, 0:1024], pf(0, 2), PB[0:2], [sc])
            kb.copy("act", sc[:, 1024:2048], pf(6, 2), PB[6:8], [sc])
            fw.op("dve", lambda e: [e.max(out=v16v[:, c, 0:8], in_=sc[:, c * 128:(c + 1) * 128]) for c in range(16)][-1], [sc], [v16])
            fw.op("dve", lambda e: [e.match_replace(out=tmp[:, c * 128:(c + 1) * 128], in_to_replace=v16v[:, c, 0:8],
                                                    in_values=sc[:, c * 128:(c + 1) * 128], imm_value=-1e30) for c in range(16)][-1],
                  [sc, v16], [tmp])
            fw.op("dve", lambda e: [e.max(out=v16v[:, c, 8:16], in_=tmp[:, c * 128:(c + 1) * 128]) for c in range(16)][-1], [tmp], [v16])
            candv = cand.ap.rearrange("p (h r c) -> p h r c", h=8, r=16)
            kb.tt("dve", candv, v16q[:, :, 0, :].unsqueeze(3).to_broadcast([128, 8, 16, 16]),
                  v16q[:, :, 1, :].unsqueeze(2).to_broadcast([128, 8, 16, 16]), ALU.add, [v16], [cand])
            fw.op("dve", lambda e: [e.max(out=bestv[:, hh, 0:8], in_=cand[:, hh * 256:(hh + 1) * 256]) for hh in range(8)][-1], [cand], [best])
            fw.op("dve", lambda e: [e.match_replace(out=tmp[:, hh * 256:(hh + 1) * 256], in_to_replace=bestv[:, hh, 0:8],
                                                    in_values=cand[:, hh * 256:(hh + 1) * 256], imm_value=-1e30) for hh in range(8)][-1],
                  [cand, best], [tmp])
            fw.op("dve", lambda e: [e.max(out=bestv[:, hh, 8:16], in_=tmp[:, hh * 256:(hh + 1) * 256]) for hh in range(8)][-1], [tmp], [best])
            kb.tt("dve", v3(exw.ap, 8), bestv, bestv[:, :, 0:1].to_broadcast([128, 8, 16]), ALU.subtract, [best], [exw])
            kb.act(exw.ap, exw.ap, AF.Exp, [exw], [exw])
            kb.reduce("dve", pst[:, 8:16], v3(exw.ap, 8), AX.X, ALU.add, [exw], [pst])
            kb.recip(pst[:, 16:24], pst[:, 8:16], [pst], [pst])
            negav = v3(nega_ap, 8)
            kb.tt("dve", negav, bestv[:, :, 15:16].to_broadcast([128, 8, 128]), scv[:, :, 0, :], ALU.subtract, [best, sc], [nega])
            kb.ts("dve", nega_ap, nega_ap, -1e-5, None, ALU.add, None, [nega], [nega])
            kb.dma("sp", st_nega[blk], nega_ap, "st_st", [nega], ())
            kb.dma("sp", v3(st_s2[blk], 8), scv[:, :, 1, :], "st_st", [sc], ())
            eatv = v3(eat_ap, 8)
            kb.tt("dve", eatv, scv[:, :, 0, :], v16q[:, :, 0, 0:1].to_broadcast([128, 8, 128]), ALU.subtract, [sc, v16], [eat])
            kb.act(eat_ap, eat_ap, AF.Exp, [eat], [eat])
            ea_ap = cand.ap[:, 0:512].bitcast(BF16)
            eb_ap = cand.ap[:, 512:1024].bitcast(BF16)
            kb.tt("dve", v3(ea_ap, 8), eatv, pst[:, 16:24].unsqueeze(2).to_broadcast([128, 8, 128]), ALU.mult, [eat, pst], [cand])
            kb.tt("dve", eatv, scv[:, :, 1, :], v16q[:, :, 1, 0:1].to_broadcast([128, 8, 128]), ALU.subtract, [sc, v16], [eat])
            kb.act(eb_ap, eat_ap, AF.Exp, [eat], [cand])
            kb.dma("sp", st_ea[blk], ea_ap, "st_st", [cand], ())
            kb.dma("sp", st_eb[blk], eb_ap, "st_st", [cand], ())
        fw.barrier()
        ar.ptr = const_end

        ust = [ar.bf16(D, "ust0"), ar.bf16(D, "ust1")]
        ugr = [ar.bf16(KC * 512, "ugr0"), ar.bf16(KC * 512, "ugr1")]
        for et in range(128):
            g, sub = divmod(et, 4)
            us = ust[et % 2]
            ug = ugr[g % 2]
            ugv = v3(ug.ap, KC)
            kb.dma("pool", us.ap, peer_u[et * 128:(et + 1) * 128, :], "ust%d" % (et % 2), (), [us])
            kb.dma("pool", V_d[et * 128:(et + 1) * 128, :], peer_v[et * 128:(et + 1) * 128, :], "vcast", (), ())
            bk = 2 * (et % 2)
            pv = v3(pbf(bk, 2), KC)
            kb.tr([(pv[:, k, :], us[:, k * 128:(k + 1) * 128], ident_b.ap) for k in range(KC)], [us, ident_b], PB[bk:bk + 2])
            if et % 2 == 0:
                kb.copy("act", ugv[:, :, sub * 128:(sub + 1) * 128], pv, PB[bk:bk + 2], [ug])
            else:
                kb.copy("dve", ugv[:, :, sub * 128:(sub + 1) * 128], pv, PB[bk:bk + 2], [ug])
            if sub == 3:
                kb.dma("sp", UT_d[g], ug.ap, "ut_st%d" % (g % 2), [ug], ())
        fw.barrier()
        ar.ptr = const_end

        ar.ptr = const_end_p6
        ut_raw = [ar.f32(KC * 256, "ut0"), ar.f32(KC * 256, "ut1")]
        vt_raw = [ar.f32(2 * D, "vt0"), ar.f32(2 * D, "vt1")]
        for t_ in ut_raw + vt_raw:
            t_.ap, t_.name = t_.ap.bitcast(BF16), t_.ap
        ut, vt = ut_raw, vt_raw
        xq = ar.bf16(TS * D, "xq")
        xqv = xq.ap.rearrange("p (t k m) -> p t k m", t=TS, k=KC)
        sn = ar.f32(TS * 1024, "sn")
        s2t = ar.f32(TS * 1024, "s2t")
        eaT = ar.bf16(TS * 1024, "eaT")
        ebT = ar.bf16(TS * 1024, "ebT")
        snv = sn.ap.rearrange("p (t h k) -> p t h k", t=TS, h=8)
        s2v = s2t.ap.rearrange("p (t h k) -> p t h k", t=TS, h=8)
        eav = eaT.ap.rearrange("p (t h k) -> p t h k", t=TS, h=8)
        ebv = ebT.ap.rearrange("p (t h k) -> p t h k", t=TS, h=8)
        acc = ar.f32(TS * D, "acc")
        accv = v3(acc.ap, TS)
        actg = [ar.bf16(512, "actg%d" % i) for i in range(2)]
        Mk = [ar.bf16(4096, "Mk%d" % i) for i in range(3)]
        Gs = [ar.bf16(512, "G0"), ar.bf16(512, "G1")]
        HmT = [ar.bf16(512, "HmT0"), ar.bf16(512, "HmT1")]
        fst = ar.f32(4, "fst")
        fng = vt[1]
        fng_ap = vt[1].name[:, 0:D]
        h2 = ut[0]
        h2_ap = ut[0].name[:, 0:D]
        junk = Mk[0]
        junk_ap = Mk[0].ap[:, 0:D]
        for stile in range(NB // TS):
            b0 = stile * TS
            for tb in range(TS):
                kb.dma("sp", xq[:, tb * D:(tb + 1) * D], xn2T_d[b0 + tb], "xq", (), [xq])
                kb.dma("sp", sn[:, tb * 1024:(tb + 1) * 1024], st_nega[b0 + tb], "sn", (), [sn])
                kb.dma("sp", s2t[:, tb * 1024:(tb + 1) * 1024], st_s2[b0 + tb], "s2t", (), [s2t])
                kb.dma("sp", eaT[:, tb * 1024:(tb + 1) * 1024], st_ea[b0 + tb], "eaT", (), [eaT])
                kb.dma("sp", ebT[:, tb * 1024:(tb + 1) * 1024], st_eb[b0 + tb], "ebT", (), [ebT])
            kb.memset("pool", acc.ap, 0.0, [acc])
            its = [(g, tb) for g in range(32) for tb in range(TS)]
            N = len(its)
            wts = {}

            def load_u(g):
                u = ut[g % 2]
                kb.dma("sp", u.ap, UT_d[g], "ut%d" % (g % 2), (), [u])

            def load_v(g):
                vv = vt[g % 2]
                kb.dma("sp", v3(vv.ap, 4), V_d[g * 512:(g + 1) * 512, :].rearrange("(i e) n -> e i n", e=128),
                       "vt%d" % (g % 2), (), [vv])

            def stA(s_):
                g, tb = its[s_]
                if tb == 0:
                    load_u(g)
                mkv = Mk[s_ % 3].ap.rearrange("p (h i j) -> p h i j", h=8, i=4)
                kb.tt("dve", mkv, s2v[:, tb].unsqueeze(2).to_broadcast([128, 8, 4, 128]),
                      snv[:, tb, :, 4 * g:4 * g + 4].unsqueeze(3).to_broadcast([128, 8, 4, 128]), ALU.is_ge, [s2t, sn], [Mk[s_ % 3]])

            def stB(s_):
                g, tb = its[s_]
                mk = Mk[s_ % 3]
                mkv = mk.ap.rearrange("p (h i j) -> p h i j", h=8, i=4)
                kb.tt("dve", mkv, mkv, ebv[:, tb].unsqueeze(2).to_broadcast([128, 8, 4, 128]), ALU.mult, [mk, ebT], [mk])
                kb.tt("pool", mkv, mkv, eav[:, tb, :, 4 * g:4 * g + 4].unsqueeze(3).to_broadcast([128, 8, 4, 128]), ALU.mult,
                      [mk, eaT], [mk])

            def stM(s_):
                g, tb = its[s_]
                uv = v3(ut[g % 2].ap, KC)
                p = s_ % 2
                kb.mm([(pf(p), xqv[:, tb, k, :], uv[:, k, :], k == 0, k == KC - 1) for k in range(KC)], [xq, ut[g % 2]], [PB[p]])
                kb.act(actg[p].ap, pf(p), AF.Gelu, [PB[p]], [actg[p]])

            def stC(s_):
                mk = Mk[s_ % 3]
                gs = Gs[s_ % 2]
                kb.tt("dve", mk[:, 0:2048], mk[:, 0:2048], mk[:, 2048:4096], ALU.add, [mk], [mk])
                kb.tt("dve", mk[:, 0:1024], mk[:, 0:1024], mk[:, 1024:2048], ALU.add, [mk], [mk])
                kb.tt("dve", gs.ap, mk[:, 0:512], mk[:, 512:1024], ALU.add, [mk], [gs])
                kb.tt("pool", gs.ap, gs.ap, actg[s_ % 2].ap, ALU.mult, [gs, actg[s_ % 2]], [gs])

            def stD(s_):
                g, tb = its[s_]
                p = s_ % 2
                gs, hT = Gs[p], HmT[p]
                vvv = v3(vt[g % 2].ap, 4)
                kb.tr([(pbf(2 + p)[:, i * 128:(i + 1) * 128], gs[:, i * 128:(i + 1) * 128], ident_b.ap) for i in range(4)],
                      [gs, ident_b], [PB[2 + p]])
                kb.copy("act", hT.ap, pbf(2 + p)[:, 0:512], [PB[2 + p]], [hT])
                sp_ = []
                for fq in range(4):
                    sp_.append((pf(4 + fq), ident_f.ap, accv[:, tb, fq * 512:(fq + 1) * 512], True, False))
                    for i in range(4):
                        sp_.append((pf(4 + fq), hT[:, i * 128:(i + 1) * 128], vvv[:, i, fq * 512:(fq + 1) * 512], False, i == 3))
                kb.mm(sp_, [hT, vt[g % 2], acc, ident_f], PB[4:8])
                kb.copy("act", accv[:, tb, :], pf(4, 4), PB[4:8], [acc])

            load_v(0)
            load_v(1)
            for s_ in range(N + 4):
                if 0 <= s_ - 3 < N:
                    stC(s_ - 3)
                if s_ < N:
                    stA(s_)
                if 0 <= s_ - 1 < N:
                    stB(s_ - 1)
                if 0 <= s_ - 4 < N:
                    stD(s_ - 4)
                    g_, tb_ = its[s_ - 4]
                    if tb_ == TS - 1 and g_ + 2 < 32:
                        load_v(g_ + 2)
                if 0 <= s_ - 2 < N:
                    stM(s_ - 2)
            kb.dma("sp", fng_ap, final_g.partition_broadcast(128), "fng", (), [fng])
            for tb in range(TS):
                r0 = (b0 + tb) * 128
                kb.dma("sp", h2_ap, h1_d[r0:r0 + 128, :], "h2_ld", (), [h2])
                kb.tt("dve", accv[:, tb, :], accv[:, tb, :], g2_bc.ap, ALU.mult, [acc, g2_bc], [acc])
                kb.tt("dve", h2_ap, h2_ap, accv[:, tb, :], ALU.add, [h2, acc], [h2])
                kb.act(junk_ap, h2_ap, AF.Square, [h2], [junk, fst], accum_out=fst[:, 0:1])
                kb.ts("dve", fst[:, 1:2], fst[:, 0:1], 1.0 / D, EPS, ALU.mult, ALU.add, [fst], [fst])
                kb.act(fst[:, 1:2], fst[:, 1:2], AF.Sqrt, [fst], [fst])
                kb.recip(fst[:, 2:3], fst[:, 1:2], [fst], [fst])
                kb.stt("dve", h2_ap, h2_ap, fst[:, 2:3], fng_ap, ALU.mult, ALU.mult, [h2, fst, fng], [h2])
                kb.dma("sp", y[r0:r0 + 128, :], h2_ap, "y_out", [h2], ())
        fw.barrier()
        fw.emit(st)
    return nc


_PROG_CACHE = {}


def make_in_maps(cfg, inp):
    NB, T = cfg.NB, cfg.TPC
    f32 = lambda a: np.ascontiguousarray(np.asarray(a, dtype=np.float32))
    shared = dict(
        cctx=f32(inp["c_ctx"]), norm1_g=f32(inp["norm1_g"][0]), norm2_g=f32(inp["norm2_g"][0]),
        w_mod=f32(inp["w_mod"][0]), b_mod=f32(inp["b_mod"][0]), w_in=f32(inp["w_in"][0]),
        w_gate_up=f32(inp["w_gate_up"][0]), b_gate=f32(inp["b_gate"][0]), gla_norm_g=f32(inp["gla_norm_g"][0]),
        cmlp_ln_g=f32(inp["cmlp_ln_g"][0]), cmlp_ln_b=f32(inp["cmlp_ln_b"][0]), w_spatial=f32(inp["w_spatial"][0]),
        b_spatial=f32(inp["b_spatial"][0]), w_out=f32(inp["w_out"][0]), peer_wq=f32(inp["peer_wq"][0]),
        peer_sk=f32(inp["peer_sub_keys"][0]), peer_u=f32(inp["peer_u"][0]), peer_v=f32(inp["peer_v"][0]),
        final_g=f32(inp["final_norm_g"]),
    )
    x = np.asarray(inp["x"], dtype=np.float32)
    ctx = np.asarray(inp["ctx"], dtype=np.float32)
    c = np.asarray(inp["c"], dtype=np.float32)
    maps = []
    for core in range(8):
        b, seg = divmod(core, 4)
        xb = x[b]
        slots = [(s, 1.0) for s in range(seg)] + [(s, 0.0) for s in range(3, seg, -1)]
        flags = np.zeros((128, 8), np.float32)
        parts = []
        for j, (s, f) in enumerate(slots):
            blocks = xb[s * T:(s + 1) * T].reshape(NB, 128, D)
            if f == 0.0:
                blocks = blocks[::-1]
            parts.append(blocks.reshape(T, D))
            flags[:, j] = f
        flags[:, 3] = 1.0
        m = dict(shared)
        m.update(xm=f32(xb[seg * T:(seg + 1) * T]), xo=f32(np.concatenate(parts, 0)), ctxb=f32(ctx[b]),
                 cvec=f32(c[b]), flags=flags)
        maps.append(m)
    return maps


def kernel(**inp):
    seq = int(np.asarray(inp["x"]).shape[1])
    cfg = Cfg(seq, int(np.asarray(inp["ctx"]).shape[1]))
    if seq not in _PROG_CACHE:
        _PROG_CACHE[seq] = build_program(cfg)
    nc = _PROG_CACHE[seq]
    maps = make_in_maps(cfg, inp)
    res = run_bass_kernel_spmd(nc, maps, core_ids=list(range(8)))
    out = np.empty((2, seq, D), np.float32)
    for core in range(8):
        b, seg = divmod(core, 4)
        out[b, seg * cfg.TPC:(seg + 1) * cfg.TPC] = res.results[core]["y"]
    return out
```

```python
import numpy as np
import concourse.bass as bass
import concourse.mybir as mybir
from concourse.bass_utils import run_bass_kernel_spmd

F32 = mybir.dt.float32
BF16 = mybir.dt.bfloat16
ALU = mybir.AluOpType
AF = mybir.ActivationFunctionType
AX = mybir.AxisListType


class Tl:
    __slots__ = ("ap", "name", "w", "r")

    def __init__(self, ap, name=""):
        self.ap = ap
        self.name = name
        self.w = None
        self.r = {}

    def __getitem__(self, k):
        return self.ap[k]


class Op:
    __slots__ = ("idx", "eng", "fn", "deps", "dma_key", "val", "signal", "dma_waits")


class FW:
    ENGS = ("pe", "act", "dve", "pool", "sp")

    def __init__(self, nc):
        self.nc = nc
        self.ops = []
        self.dma_cnt = {}

    def op(self, eng, fn, reads=(), writes=(), dma_key=None):
        o = Op()
        o.idx = len(self.ops)
        o.eng = eng
        o.fn = fn
        o.dma_key = dma_key
        o.signal = False
        o.val = 0
        deps = set()
        for t in reads:
            if t.w is not None:
                deps.add(t.w)
        for t in writes:
            if t.w is not None:
                deps.add(t.w)
            deps.update(t.r.values())
        o.deps = deps
        o.dma_waits = {}
        for d in deps:
            od = self.ops[d]
            if od.dma_key is not None:
                o.dma_waits[od.dma_key] = self.dma_cnt[od.dma_key]
        if dma_key is not None:
            self.dma_cnt[dma_key] = self.dma_cnt.get(dma_key, 0) + 16
            o.val = self.dma_cnt[dma_key]
        rkey = ("dma", dma_key) if dma_key is not None else eng
        for t in reads:
            t.r[rkey] = o.idx
        for t in writes:
            t.w = o.idx
            t.r = {}
        self.ops.append(o)
        return o

    def emit(self, stack):
        nc = self.nc
        ops = self.ops
        for o in ops:
            for d in o.deps:
                if ops[d].dma_key is None:
                    ops[d].signal = True
        cnt = {e: 0 for e in self.ENGS}
        for o in ops:
            if o.dma_key is None and o.signal:
                cnt[o.eng] += 1
                o.val = cnt[o.eng]
        esem = {e: stack.enter_context(nc.semaphore("s_" + e)) for e in self.ENGS}
        dsem = {k: stack.enter_context(nc.semaphore("d_%d" % i))
                for i, k in enumerate(self.dma_cnt)}
        streams = {e: [o for o in ops if o.eng == e] for e in self.ENGS}

        def run(eng_name, e):
            waited = {}
            for o in streams[eng_name]:
                waits = {}
                for d in o.deps:
                    od = ops[d]
                    if od.dma_key is not None:
                        key = ("d", od.dma_key)
                        v = o.dma_waits[od.dma_key]
                    else:
                        if od.eng == eng_name and eng_name == "pe":
                            continue
                        key = ("e", od.eng)
                        v = od.val
                    if waits.get(key, 0) < v:
                        waits[key] = v
                for key, v in waits.items():
                    if waited.get(key, 0) >= v:
                        continue
                    s = dsem[key[1]] if key[0] == "d" else esem[key[1]]
                    e.wait_ge(s, v)
                    waited[key] = v
                ins = o.fn(e)
                if o.dma_key is not None:
                    ins.then_inc(dsem[o.dma_key], 16)
                elif o.signal:
                    ins.then_inc(esem[eng_name], 1)

        block = stack.enter_context(nc.Block())

        @block.tensor
        def _(e):
            run("pe", e)

        @block.scalar
        def _(e):
            run("act", e)

        @block.vector
        def _(e):
            run("dve", e)

        @block.gpsimd
        def _(e):
            run("pool", e)

        @block.sync
        def _(e):
            run("sp", e)

    def barrier(self):
        last = {}
        for o in self.ops:
            last[o.eng if o.dma_key is None else ("d", o.dma_key)] = o.idx
        deps = set(last.values())
        for e in self.ENGS:
            o = self.op(e, lambda en: en.nop())
            o.deps |= deps
            for d in deps:
                k = self.ops[d].dma_key
                if k is not None:
                    o.dma_waits[k] = self.dma_cnt[k]


D = 2048
KC = 16
NE = 16384
EPS = 1e-6
IN_W = 5152


class Cfg:
    def __init__(self, seq, ctx=256):
        self.SEQ = seq
        self.CTX = ctx
        self.TPC = seq // 4
        self.NB = self.TPC // 128
        self.TS = min(4, self.NB)


class Arena:
    def __init__(self, ap, words):
        self.ap = ap
        self.ptr = 0
        self.words = words

    def f32(self, n, name=""):
        off = self.ptr
        self.ptr += n
        assert self.ptr <= self.words, (name, self.ptr, self.words)
        return Tl(self.ap[:, off:off + n], name)

    def bf16(self, n, name=""):
        w = (n + 1) // 2
        off = self.ptr
        self.ptr += w
        assert self.ptr <= self.words, (name, self.ptr, self.words)
        return Tl(self.ap[:, off:off + w].bitcast(BF16), name)


def v3(ap, a):
    return ap.rearrange("p (a b) -> p a b", a=a)


class KB:
    def __init__(self, nc, fw):
        self.nc = nc
        self.fw = fw

    def dma(self, q, out_ap, in_ap, key, reads=(), writes=(), nc_ok=False):
        nc = self.nc

        def fn(e):
            if nc_ok:
                with nc.allow_non_contiguous_dma(reason="small strided load"):
                    return e.dma_start(out=out_ap, in_=in_ap)
            return e.dma_start(out=out_ap, in_=in_ap)
        return self.fw.op(q, fn, reads, writes, dma_key=key)

    def act(self, out, in_, func, reads, writes, **kw):
        return self.fw.op("act", lambda e: e.activation(out=out, in_=in_, func=func, **kw), reads, writes)

    def acts(self, specs, reads, writes):
        def fn(e):
            ins = None
            for (out, in_, func, kw) in specs:
                ins = e.activation(out=out, in_=in_, func=func, **kw)
            return ins
        return self.fw.op("act", fn, reads, writes)

    def tt(self, eng, out, in0, in1, op, reads, writes):
        return self.fw.op(eng, lambda e: e.tensor_tensor(out=out, in0=in0, in1=in1, op=op), reads, writes)

    def ts(self, eng, out, in0, s1, s2, op0, op1, reads, writes):
        if s2 is None:
            return self.fw.op(eng, lambda e: e.tensor_scalar(out=out, in0=in0, scalar1=s1, scalar2=None, op0=op0), reads, writes)
        return self.fw.op(eng, lambda e: e.tensor_scalar(out=out, in0=in0, scalar1=s1, scalar2=s2, op0=op0, op1=op1), reads, writes)

    def stt(self, eng, out, in0, scalar, in1, op0, op1, reads, writes):
        return self.fw.op(eng, lambda e: e.scalar_tensor_tensor(out=out, in0=in0, scalar=scalar, in1=in1, op0=op0, op1=op1), reads, writes)

    def stts(self, eng, specs, reads, writes):
        def fn(e):
            ins = None
            for (out, in0, scalar, in1, op0, op1) in specs:
                ins = e.scalar_tensor_tensor(out=out, in0=in0, scalar=scalar, in1=in1, op0=op0, op1=op1)
            return ins
        return self.fw.op(eng, fn, reads, writes)

    def copy(self, eng, out, in_, reads, writes):
        if eng == "act":
            return self.fw.op("act", lambda e: e.activation(out=out, in_=in_, func=AF.Copy), reads, writes)
        return self.fw.op(eng, lambda e: e.tensor_copy(out=out, in_=in_), reads, writes)

    def memset(self, eng, out, val, writes):
        return self.fw.op(eng, lambda e: e.memset(out, val), (), writes)

    def mm(self, specs, reads, writes):
        def fn(e):
            ins = None
            for (out, lhsT, rhs, start, stop) in specs:
                ins = e.matmul(out, lhsT=lhsT, rhs=rhs, start=start, stop=stop)
            return ins
        return self.fw.op("pe", fn, reads, writes)

    def tr(self, specs, reads, writes):
        def fn(e):
            ins = None
            for (out, in_, ident) in specs:
                ins = e.transpose(out=out, in_=in_, identity=ident)
            return ins
        return self.fw.op("pe", fn, reads, writes)

    def reduce(self, eng, out, in_, axis, op, reads, writes):
        nc = self.nc

        def fn(e):
            with nc.allow_low_precision(reason="bf16 gate sum feeds a bf16 matmul operand"):
                return e.tensor_reduce(out=out, in_=in_, axis=axis, op=op)
        return self.fw.op(eng, fn, reads, writes)

    def recip(self, out, in_, reads, writes):
        return self.fw.op("dve", lambda e: e.reciprocal(out=out, in_=in_), reads, writes)

    def rstd(self, out, ss, n, tmp, reads, writes):
        self.ts("dve", tmp, ss, 1.0 / n, EPS, ALU.mult, ALU.add, reads, writes)
        self.act(tmp, tmp, AF.Sqrt, writes, writes)
        self.recip(out, tmp, writes, writes)


AW = 53200


def build_program(cfg, debug=False):
    from contextlib import ExitStack
    NB, TPC, TS = cfg.NB, cfg.TPC, cfg.TS
    nc = bass.Bass("TRN2", target_bir_lowering=False)

    def din(name, shape, dt=F32):
        return nc.dram_tensor(name, list(shape), dt, kind="ExternalInput").ap()

    def dscr(name, shape, dt):
        return nc.dram_tensor(name, list(shape), dt, kind="ExternalOutput" if debug else "Internal").ap()

    xm = din("xm", [TPC, D])
    xo = din("xo", [3 * TPC, D])
    ctxb = din("ctxb", [cfg.CTX, D])
    cvec = din("cvec", [D])
    cctx = din("cctx", [D])
    flags = din("flags", [128, 8])
    norm1_g = din("norm1_g", [D])
    norm2_g = din("norm2_g", [D])
    w_mod = din("w_mod", [D, 6 * D])
    b_mod = din("b_mod", [6 * D])
    w_in = din("w_in", [D, IN_W])
    w_gate_up = din("w_gate_up", [2, 16, 512])
    b_gate = din("b_gate", [2, 512])
    gla_norm_g = din("gla_norm_g", [1024])
    cmlp_ln_g = din("cmlp_ln_g", [1024])
    cmlp_ln_b = din("cmlp_ln_b", [1024])
    w_spatial = din("w_spatial", [8, 128, 128])
    b_spatial = din("b_spatial", [8, 128])
    w_out = din("w_out", [D, D])
    peer_wq = din("peer_wq", [D, D])
    peer_sk = din("peer_sk", [2, 8, 128, 128])
    peer_u = din("peer_u", [NE, D])
    peer_v = din("peer_v", [NE, D])
    final_g = din("final_g", [D])
    y = nc.dram_tensor("y", [TPC, D], F32, kind="ExternalOutput").ap()

    of_d = dscr("of_d", [TPC, 1024], F32)
    mix_d = dscr("mix_d", [TPC, D], BF16)
    h1_d = dscr("h1_d", [TPC, D], F32)
    xn2T_d = dscr("xn2T_d", [NB, 128, D], BF16)
    st_nega = dscr("st_nega", [NB, 128, 1024], F32)
    st_s2 = dscr("st_s2", [NB, 128, 1024], F32)
    st_ea = dscr("st_ea", [NB, 128, 1024], BF16)
    st_eb = dscr("st_eb", [NB, 128, 1024], BF16)
    UT_d = dscr("UT_d", [32, 128, 8192], BF16)
    V_d = dscr("V_d", [NE, D], BF16)

    st = ExitStack()
    with st:
        fw = FW(nc)
        kb = KB(nc, fw)
        arena_t = st.enter_context(nc.sbuf_tensor("arena", [128, AW], F32))
        psum_t = st.enter_context(nc.psum_tensor("psum", [128, 4096], F32))
        ar = Arena(arena_t, AW)
        PB = [Tl(psum_t[:, b * 512:(b + 1) * 512], "B%d" % b) for b in range(8)]

        def pf(b0, nb=1):
            return psum_t[:, b0 * 512:(b0 + nb) * 512]

        def pbf(b0, nb=1):
            return psum_t[:, b0 * 512:(b0 + nb) * 512].bitcast(BF16)

        ident_f = ar.f32(128, "ident_f")
        A_le = ar.f32(128, "A_le")
        A_ge = ar.f32(128, "A_ge")
        A_gt = ar.f32(128, "A_gt")
        A_lt = ar.f32(128, "A_lt")
        ident_b = ar.bf16(128, "ident_b")
        cols = ar.f32(16 * 8, "cols")
        fl = ar.f32(8, "fl")
        fl2 = ar.f32(24, "fl2")
        g2_bc = ar.f32(D, "g2_bc")

        def tri(tl, cmp, sign):
            kb.memset("pool", tl.ap, 0.0, [tl])
            fw.op("pool", lambda e: e.affine_select(out=tl.ap, in_=tl.ap, pattern=[[-sign, 128]], compare_op=cmp,
                                                    fill=1.0, base=0, channel_multiplier=sign), [tl], [tl])
        tri(ident_f, ALU.not_equal, 1)
        tri(A_le, ALU.is_gt, 1)
        tri(A_ge, ALU.is_gt, -1)
        kb.tt("dve", A_gt.ap, A_ge.ap, ident_f.ap, ALU.subtract, [A_ge, ident_f], [A_gt])
        kb.tt("dve", A_lt.ap, A_le.ap, ident_f.ap, ALU.subtract, [A_le, ident_f], [A_lt])
        kb.copy("dve", ident_b.ap, ident_f.ap, [ident_f], [ident_b])
        kb.dma("sp", fl.ap, flags[:, :], "misc", (), [fl])
        kb.ts("dve", fl2[:, 0:8], fl.ap, -1.0 / 16.0, None, ALU.mult, None, [fl], [fl2])
        kb.ts("dve", fl2[:, 16:24], fl.ap, -1.0, 1.0, ALU.mult, ALU.add, [fl], [fl2])
        kb.ts("dve", fl2[:, 8:16], fl2[:, 16:24], -1.0 / 16.0, None, ALU.mult, None, [fl2], [fl2])
        colv = v3(cols.ap, 8)
        C_G1, C_SH1, C_CG1, C_CSH1, C_G2, C_SH2, C_N1, C_N2 = range(8)
        const_end_p6 = ar.ptr
        g1_bc = ar.f32(D, "g1_bc")
        const_end = ar.ptr

        M = ar.f32(6 * D, "M")
        Mc = ar.f32(2 * D, "Mc")
        rep = ar.f32(2 * KC * 128, "rep")
        ccol = ar.f32(32, "ccol")
        wb = [ar.f32(KC * 512, "wb0"), ar.f32(KC * 512, "wb1")]
        repv = rep.ap.rearrange("p (t k m) -> p t k m", t=2, k=KC)
        kb.dma("sp", ccol[:, 0:16], cvec.rearrange("(k p) -> p k", p=128), "misc", (), [ccol], nc_ok=True)
        kb.dma("sp", ccol[:, 16:32], cctx.rearrange("(k p) -> p k", p=128), "misc", (), [ccol], nc_ok=True)
        kb.dma("sp", colv[:, C_N1, :], norm1_g.rearrange("(k p) -> p k", p=128), "misc", (), [cols], nc_ok=True)
        kb.dma("sp", colv[:, C_N2, :], norm2_g.rearrange("(k p) -> p k", p=128), "misc", (), [cols], nc_ok=True)
        kb.dma("sp", M.ap, b_mod.partition_broadcast(128), "misc", (), [M])
        kb.dma("sp", Mc.ap, b_mod[0:2 * D].partition_broadcast(128), "misc", (), [Mc])
        kb.act(ccol.ap, ccol.ap, AF.Silu, [ccol], [ccol])
        kb.copy("dve", repv[:, 0], ccol[:, 0:16].unsqueeze(2).to_broadcast([128, KC, 128]), [ccol], [rep])
        kb.copy("dve", repv[:, 1], ccol[:, 16:32].unsqueeze(2).to_broadcast([128, KC, 128]), [ccol], [rep])
        wmv = w_mod.rearrange("(k p) n -> p k n", p=128)
        for cb in range(24):
            w = wb[cb % 2]
            kb.dma("sp", v3(w.ap, KC), wmv[:, :, cb * 512:(cb + 1) * 512], "wb%d" % (cb % 2), (), [w])
            bk = cb % 2
            kb.mm([(pf(bk), repv[:, 0, k, :], v3(w.ap, KC)[:, k, :], k == 0, k == KC - 1) for k in range(KC)],
                  [rep, w], [PB[bk]])
            kb.tt("dve", M[:, cb * 512:(cb + 1) * 512], pf(bk), M[:, cb * 512:(cb + 1) * 512], ALU.add, [PB[bk], M], [M])
            if cb < 8:
                bk2 = 2 + cb % 2
                kb.mm([(pf(bk2), repv[:, 1, k, :], v3(w.ap, KC)[:, k, :], k == 0, k == KC - 1) for k in range(KC)],
                      [rep, w], [PB[bk2]])
                kb.tt("dve", Mc[:, cb * 512:(cb + 1) * 512], pf(bk2), Mc[:, cb * 512:(cb + 1) * 512], ALU.add, [PB[bk2], Mc], [Mc])
        tmpc = ar.f32(6 * 16, "tmpc")
        tmpcv = v3(tmpc.ap, 6)
        srcs = [(M, 0), (M, 1), (M, 3), (M, 4), (Mc, 0), (Mc, 1)]
        for i, (src, ch) in enumerate(srcs):
            kb.tr([(pf(4, 4)[:, k * 128:(k + 1) * 128], src[:, ch * D + k * 128: ch * D + (k + 1) * 128], ident_f.ap)
                   for k in range(KC)], [src, ident_f], PB[4:8])
            kb.copy("dve", tmpcv[:, i, :], v3(pf(4, 4), KC)[:, :, 0], PB[4:8], [tmpc])
        kb.copy("act", g1_bc.ap, M[:, 2 * D:3 * D], [M], [g1_bc])
        kb.copy("act", g2_bc.ap, M[:, 5 * D:6 * D], [M], [g2_bc])
        for (dst, sc_i, n_i) in ((C_G1, 1, C_N1), (C_G2, 3, C_N2), (C_CG1, 5, C_N1)):
            kb.stt("dve", colv[:, dst, :], tmpcv[:, sc_i, :], 1.0, colv[:, n_i, :], ALU.add, ALU.mult, [tmpc, cols], [cols])
        for (dst, sh_i) in ((C_SH1, 0), (C_SH2, 2), (C_CSH1, 4)):
            kb.copy("dve", colv[:, dst, :], tmpcv[:, sh_i, :], [tmpc], [cols])
        fw.barrier()
        ar.ptr = const_end

        class FE:
            def __init__(self, tb0):
                self.xt = [ar.f32(D, "xt0"), ar.f32(D, "xt1")]
                self.xs = ar.bf16(D, "xs")
                self.xnT = ar.bf16(D, "xnT")
                self.stt_ = ar.f32(4, "fe_st")
                self.n = 0
                self.tb0 = tb0

            def load(self, rows_ap, key="xt"):
                t = self.xt[self.n % 2]
                kb.dma("sp", t.ap, rows_ap, "%s%d" % (key, self.n % 2), (), [t])
                return t

            def norm_T(self, t, gi, si):
                s = self.stt_
                tb0 = self.tb0
                kb.act(self.xs.ap, t.ap, AF.Square, [t], [self.xs, s], accum_out=s[:, 0:1])
                kb.ts("dve", s[:, 1:2], s[:, 0:1], 1.0 / D, EPS, ALU.mult, ALU.add, [s], [s])
                kb.act(s[:, 1:2], s[:, 1:2], AF.Sqrt, [s], [s])
                kb.recip(s[:, 2:3], s[:, 1:2], [s], [s])
                kb.ts("dve", self.xs.ap, t.ap, s[:, 2:3], None, ALU.mult, None, [t, s], [self.xs])
                pv = v3(pbf(tb0, 2), KC)
                kb.tr([(pv[:, k, :], self.xs[:, k * 128:(k + 1) * 128], ident_b.ap) for k in range(KC)],
                      [self.xs, ident_b], PB[tb0:tb0 + 2])
                xv = v3(self.xnT.ap, KC)
                kb.acts([(xv[:, k, :], pv[:, k, :], AF.Identity,
                          dict(scale=colv[:, gi, k:k + 1], bias=colv[:, si, k:k + 1])) for k in range(KC)],
                        PB[tb0:tb0 + 2] + [cols], [self.xnT])
                self.n += 1
                return xv

        GW = 3104
        wg = ar.bf16(KC * GW, "wg")
        wgv = v3(wg.ap, KC)
        kb.dma("pool", wgv, w_in.rearrange("(k p) n -> p k n", p=128)[:, :, 0:GW], "wg", (), [wg])
        for et in range(64):
            kb.dma("pool", V_d[et * 256:(et + 1) * 256, :], peer_v[et * 256:(et + 1) * 256, :], "vcast", (), ())
        Wga = [ar.f32(512, "Wga0"), ar.f32(512, "Wga1")]
        for d in range(2):
            kb.memset("pool", Wga[d].ap, 0.0, [Wga[d]])
            kb.dma("sp", Wga[d][16 * d:16 * d + 16, :], w_gate_up[d], "misc", (), [Wga[d]])
            kb.dma("sp", Wga[d][32:33, :], b_gate[d:d + 1, :], "misc", (), [Wga[d]])
        ngbc = ar.f32(1024, "ngbc")
        kb.dma("sp", ngbc.ap, gla_norm_g.partition_broadcast(128), "misc", (), [ngbc])
        S = [ar.f32(1024, "S_f"), ar.f32(1024, "S_b")]
        Sbf = ar.bf16(1024, "Sbf")
        for d in range(2):
            kb.memset("pool", S[d].ap, 0.0, [S[d]])
        fe = FE(6)
        qT = ar.f32(512, "qT")
        kT = ar.f32(512, "kT")
        k_sb = ar.f32(512, "k_sb")
        v_sb = ar.bf16(1024, "v_sb")
        lrT = ar.f32(128, "lrT")
        e1 = ar.f32(512, "e1")
        la = ar.f32(512, "la")
        ecT = ar.f32(512, "ecT")
        encT = ar.f32(512, "encT")
        ercum = ar.f32(512, "ercum")
        kdec = ar.bf16(512, "kdec")
        qd = ar.bf16(512, "qd")
        ki = ar.bf16(512, "ki")
        scm = ar.bf16(512, "scm")
        o_sb = ar.f32(1024, "o_sb")
        of_sb = ar.f32(1024, "of_sb")
        silug = ar.f32(1024, "silug")
        ybf = ar.bf16(1024, "ybf")
        gst = ar.f32(16, "gst")
        kb.memset("pool", lrT.ap, 1.0, [lrT])
        ONE_NF16 = fl2[:, 3:4]
        ONE_F = fl[:, 3:4]

        TRI = {0: (A_le, A_gt, 127), 1: (A_ge, A_lt, 0)}

        def inproj_state(xv):
            kb.mm([(pf(2), xv[:, k, :], wgv[:, k, 512:1024], k == 0, k == KC - 1) for k in range(KC)],
                  [fe.xnT, wg], [PB[2]])
            kb.copy("act", k_sb.ap, pf(2), [PB[2]], [k_sb])
            for hf in range(2):
                kb.mm([(pf(3 + hf), xv[:, k, :], wgv[:, k, 1024 + hf * 512:1536 + hf * 512], k == 0, k == KC - 1)
                       for k in range(KC)], [fe.xnT, wg], [PB[3 + hf]])
            kb.copy("act", v_sb.ap, pf(3, 2), PB[3:5], [v_sb])
            kb.mm([(pf(5)[0:32, 0:128], wgv[:, k, 3072:3104], xv[:, k, :], k == 0, k == KC - 1) for k in range(KC)],
                  [fe.xnT, wg], [PB[5]])
            kb.copy("dve", lrT[0:32, :], pf(5)[0:32, 0:128], [PB[5]], [lrT])

        def decay_parts(d, nf16_ap):
            cumm, rcm, _ = TRI[d]
            kb.mm([(pf(2), lrT[0:33, :], Wga[d][0:33, :], True, True)], [lrT, Wga[d]], [PB[2]])
            kb.act(e1.ap, pf(2), AF.Exp, [PB[2]], [e1], scale=-1.0)
            kb.act(e1.ap, e1.ap, AF.Ln, [e1], [e1], bias=1.0)
            kb.ts("dve", la.ap, e1.ap, nf16_ap, None, ALU.mult, None, [e1, fl2], [la])
            kb.mm([(pf(5), rcm.ap, la.ap, True, True)], [rcm, la], [PB[5]])
            kb.mm([(pf(1)[:, h * 128:(h + 1) * 128], la[:, h * 128:(h + 1) * 128], cumm.ap, True, True)
                   for h in range(4)], [la, cumm], [PB[1]])
            kb.act(ecT.ap, pf(1), AF.Exp, [PB[1]], [ecT])
            kb.act(ercum.ap, pf(5), AF.Exp, [PB[5]], [ercum])

        def state_update(d, f_ap):
            col = TRI[d][2]
            kb.stt("dve", kdec.ap, ercum.ap, f_ap, k_sb.ap, ALU.mult, ALU.mult, [ercum, k_sb, fl, fl2], [kdec])
            kb.mm([(pf(3, 2)[:, h * 256:(h + 1) * 256], kdec[:, h * 128:(h + 1) * 128],
                    v_sb[:, h * 256:(h + 1) * 256], True, True) for h in range(4)], [kdec, v_sb], PB[3:5])
            kb.stts("dve", [(S[d][:, h * 256:(h + 1) * 256], S[d][:, h * 256:(h + 1) * 256],
                             ecT[:, h * 128 + col:h * 128 + col + 1], pf(3, 2)[:, h * 256:(h + 1) * 256],
                             ALU.mult, ALU.add) for h in range(4)], [S[d], ecT] + PB[3:5], [S[d]])

        def state_block(rows_ap, gi, si, fcol):
            t = fe.load(rows_ap)
            xv = fe.norm_T(t, gi, si)
            inproj_state(xv)
            decay_parts(0, fl2[:, fcol:fcol + 1])
            state_update(0, fl[:, fcol:fcol + 1])
            decay_parts(1, fl2[:, 8 + fcol:9 + fcol])
            state_update(1, fl2[:, 16 + fcol:17 + fcol])

        def main_block(d, blk):
            cumm, rcm, col = TRI[d]
            t = fe.load(xm[blk * 128:(blk + 1) * 128, :])
            xv = fe.norm_T(t, C_G1, C_SH1)
            if d == 1:
                for hf in range(2):
                    kb.mm([(pf(6 + hf), xv[:, k, :], wgv[:, k, 2048 + hf * 512:2560 + hf * 512], k == 0, k == KC - 1)
                           for k in range(KC)], [fe.xnT, wg], [PB[6 + hf]])
                kb.act(silug.ap, pf(6, 2), AF.Silu, PB[6:8], [silug])
                kb.dma("sp", of_sb.ap, of_d[blk * 128:(blk + 1) * 128, :], "of_sb", [ofd_tl[blk]], [of_sb])
            kb.mm([(pf(0)[:, h * 128:(h + 1) * 128], wgv[:, k, h * 128:(h + 1) * 128], xv[:, k, :], k == 0, k == KC - 1)
                   for h in range(4) for k in range(KC)], [fe.xnT, wg], [PB[0]])
            kb.act(qT.ap, pf(0), AF.Identity, [PB[0]], [qT], scale=128.0 ** -0.5)
            kb.mm([(pf(1)[:, h * 128:(h + 1) * 128], wgv[:, k, 512 + h * 128:512 + (h + 1) * 128], xv[:, k, :], k == 0, k == KC - 1)
                   for h in range(4) for k in range(KC)], [fe.xnT, wg], [PB[1]])
            kb.copy("dve", kT.ap, pf(1), [PB[1]], [kT])
            inproj_state(xv)
            decay_parts(d, ONE_NF16)
            kb.act(encT.ap, pf(1), AF.Exp, [PB[1]], [encT], scale=-1.0)
            kb.tt("dve", qd.ap, qT.ap, ecT.ap, ALU.mult, [qT, ecT], [qd])
            kb.tt("dve", ki.ap, kT.ap, encT.ap, ALU.mult, [kT, encT], [ki])
            kb.mm([(pf(0)[:, h * 128:(h + 1) * 128], ki[:, h * 128:(h + 1) * 128], qd[:, h * 128:(h + 1) * 128], True, True)
                   for h in range(4)], [ki, qd], [PB[0]])
            kb.tt("dve", v3(scm.ap, 4), v3(pf(0), 4), cumm.ap.unsqueeze(1).to_broadcast([128, 4, 128]), ALU.mult,
                  [PB[0], cumm], [scm])
            sp = []
            for h in range(4):
                o_ap = pf(6, 2)[:, h * 256:(h + 1) * 256]
                sp.append((o_ap, scm[:, h * 128:(h + 1) * 128], v_sb[:, h * 256:(h + 1) * 256], True, False))
                sp.append((o_ap, qd[:, h * 128:(h + 1) * 128], Sbf[:, h * 256:(h + 1) * 256], False, True))
            kb.mm(sp, [scm, v_sb, qd, Sbf], PB[6:8])
            if d == 0:
                kb.copy("act", o_sb.ap, pf(6, 2), PB[6:8], [o_sb])
                kb.dma("sp", of_d[blk * 128:(blk + 1) * 128, :], o_sb.ap, "of_st", [o_sb], [ofd_tl[blk]])
            else:
                kb.tt("dve", o_sb.ap, pf(6, 2), of_sb.ap, ALU.add, PB[6:8] + [of_sb], [o_sb])
                kb.acts([(of_sb[:, h * 256:(h + 1) * 256], o_sb[:, h * 256:(h + 1) * 256], AF.Square,
                          dict(accum_out=gst[:, h:h + 1])) for h in range(4)], [o_sb], [of_sb, gst])
                kb.ts("dve", gst[:, 4:8], gst[:, 0:4], 1.0 / 256.0, EPS, ALU.mult, ALU.add, [gst], [gst])
                kb.act(gst[:, 4:8], gst[:, 4:8], AF.Sqrt, [gst], [gst])
                kb.recip(gst[:, 8:12], gst[:, 4:8], [gst], [gst])
                kb.stts("dve", [(o_sb[:, h * 256:(h + 1) * 256], o_sb[:, h * 256:(h + 1) * 256], gst[:, 8 + h:9 + h],
                                 ngbc[:, h * 256:(h + 1) * 256], ALU.mult, ALU.mult) for h in range(4)],
                        [o_sb, gst, ngbc], [o_sb])
                kb.tt("dve", ybf.ap, o_sb.ap, silug.ap, ALU.mult, [o_sb, silug], [ybf])
                kb.dma("sp", mix_d[blk * 128:(blk + 1) * 128, 0:1024], ybf.ap, "y_st", [ybf], ())
            state_update(d, ONE_F)
            kb.copy("pool", Sbf.ap, S[d].ap, [S[d]], [Sbf])

        ofd_tl = [Tl(None, 'ofd') for _ in range(NB)]
        nctx = cfg.CTX // 128
        for b in range(nctx):
            state_block(ctxb[b * 128:(b + 1) * 128, :], C_CG1, C_CSH1, 3)
        for b in range(nctx - 1, -1, -1):
            state_block(ctxb[b * 128:(b + 1) * 128, :], C_CG1, C_CSH1, 4)
        for j in range(3):
            for b in range(NB):
                r0 = (j * NB + b) * 128
                state_block(xo[r0:r0 + 128, :], C_G1, C_SH1, j)
        kb.copy("pool", Sbf.ap, S[0].ap, [S[0]], [Sbf])
        for b in range(NB):
            main_block(0, b)
        kb.copy("pool", Sbf.ap, S[1].ap, [S[1]], [Sbf])
        for b in range(NB - 1, -1, -1):
            main_block(1, b)
        fw.barrier()
        ar.ptr = const_end

        wc = ar.bf16(KC * 2048, "wc")
        wcv = v3(wc.ap, KC)
        kb.dma("pool", wcv, w_in.rearrange("(k p) n -> p k n", p=128)[:, :, GW:IN_W], "wc", (), [wc])
        wsT = ar.bf16(1024, "wsT")
        wstg = ar.f32(1024, "wstg")
        bs_col = ar.f32(8, "bs_col")
        lng = ar.f32(1024, "lng")
        lnb = ar.f32(1024, "lnb")
        kb.dma("sp", v3(wstg.ap, 8), w_spatial.rearrange("g p q -> p g q"), "misc", (), [wstg])
        kb.dma("sp", bs_col.ap, b_spatial.rearrange("g p -> p g"), "misc", (), [bs_col], nc_ok=True)
        kb.dma("sp", lng.ap, cmlp_ln_g.partition_broadcast(128), "misc", (), [lng])
        kb.dma("sp", lnb.ap, cmlp_ln_b.partition_broadcast(128), "misc", (), [lnb])
        kb.tr([(pf(0, 2)[:, g * 128:(g + 1) * 128], wstg[:, g * 128:(g + 1) * 128], ident_f.ap) for g in range(8)],
              [wstg, ident_f], PB[0:2])
        kb.copy("dve", wsT.ap, pf(0, 2), PB[0:2], [wsT])
        fe = FE(6)
        gu = ar.f32(1024, "gu")
        gv = ar.f32(1024, "gv")
        vn = ar.bf16(1024, "vn")
        cm = ar.bf16(1024, "cm")
        cst = ar.f32(8, "cst")
        for blk in range(NB):
            t = fe.load(xm[blk * 128:(blk + 1) * 128, :])
            xv = fe.norm_T(t, C_G1, C_SH1)
            for q4 in range(4):
                kb.mm([(pf(q4), xv[:, k, :], wcv[:, k, q4 * 512:(q4 + 1) * 512], k == 0, k == KC - 1) for k in range(KC)],
                      [fe.xnT, wc], [PB[q4]])
            kb.act(gu.ap, pf(0, 2), AF.Gelu, PB[0:2], [gu])
            kb.act(gv.ap, pf(2, 2), AF.Gelu, PB[2:4], [gv, cst], accum_out=cst[:, 0:1])
            kb.ts("dve", cst[:, 1:2], cst[:, 0:1], -1.0 / 1024.0, None, ALU.mult, None, [cst], [cst])
            kb.act(vn.ap, gv.ap, AF.Square, [gv, cst], [vn, cst], bias=cst[:, 1:2], accum_out=cst[:, 2:3])
            kb.ts("dve", cst[:, 3:4], cst[:, 2:3], 1.0 / 1024.0, EPS, ALU.mult, ALU.add, [cst], [cst])
            kb.act(cst[:, 3:4], cst[:, 3:4], AF.Sqrt, [cst], [cst])
            kb.recip(cst[:, 4:5], cst[:, 3:4], [cst], [cst])
            kb.ts("dve", gv.ap, gv.ap, cst[:, 1:2], cst[:, 4:5], ALU.add, ALU.mult, [gv, cst], [gv])
            kb.tt("dve", gv.ap, gv.ap, lng.ap, ALU.mult, [gv, lng], [gv])
            kb.tt("dve", vn.ap, gv.ap, lnb.ap, ALU.add, [gv, lnb], [vn])
            kb.mm([(pf(4, 2)[:, g * 128:(g + 1) * 128], wsT[:, g * 128:(g + 1) * 128], vn[:, g * 128:(g + 1) * 128], True, True)
                   for g in range(8)], [wsT, vn], PB[4:6])
            kb.stts("dve", [(cm[:, g * 128:(g + 1) * 128], pf(4, 2)[:, g * 128:(g + 1) * 128], bs_col[:, g:g + 1],
                             gu[:, g * 128:(g + 1) * 128], ALU.add, ALU.mult) for g in range(8)],
                    PB[4:6] + [bs_col, gu], [cm])
            kb.dma("sp", mix_d[blk * 128:(blk + 1) * 128, 1024:2048], cm.ap, "cm_st", [cm], ())
        fw.barrier()
        ar.ptr = const_end

        wo = ar.bf16(KC * D, "wo")
        wov = v3(wo.ap, KC)
        wq = ar.bf16(KC * D, "wq")
        wqv = v3(wq.ap, KC)
        skT = ar.bf16(D, "skT")
        skTv = v3(skT.ap, 16)
        kb.dma("pool", wqv, peer_wq.rearrange("(k p) n -> p k n", p=128), "wq", (), [wq])
        h1 = ar.f32(D, "h1")
        sc = ar.f32(D, "sc")
        tmp = ar.f32(D, "tmp")
        stg = [h1, sc]
        for k in range(KC):
            sg = stg[k % 2]
            kb.dma("sp", sg.ap, w_out[k * 128:(k + 1) * 128, :], "wo_st%d" % (k % 2), (), [sg])
            kb.tt("dve", wov[:, k, :], sg.ap, g1_bc.ap, ALU.mult, [sg, g1_bc], [wo])
        kb.dma("sp", v3(tmp.ap, 16), peer_sk.rearrange("t h k d -> k (t h) d"), "misc", (), [tmp])
        specs = []
        for half in range(2):
            for hh in range(8):
                c = hh * 2 + half
                specs.append((pf(0, 4)[:, c * 128:(c + 1) * 128], tmp[:, (half * 8 + hh) * 128:(half * 8 + hh + 1) * 128], ident_f.ap))
        kb.tr(specs, [tmp, ident_f], PB[0:4])
        kb.copy("dve", skT.ap, pf(0, 4), PB[0:4], [skT])
        mixT = ar.bf16(D, "mixT")
        mixTv = v3(mixT.ap, KC)
        xt5 = sc
        xn2T = ar.bf16(D, "xn2T")
        xn2v = v3(xn2T.ap, KC)
        qTb = ar.bf16(D, "qTb")
        qTv = v3(qTb.ap, 16)
        v16 = ar.f32(256, "v16")
        v16v = v3(v16.ap, 16)
        cand = ar.f32(D, "cand")
        mix_sb = cand
        mix_ap = cand.ap[:, 0:1024].bitcast(BF16)
        xs2 = mixT
        best = ar.f32(128, "best")
        bestv = v3(best.ap, 8)
        nega = tmp
        nega_ap = tmp.ap[:, 1024:2048]
        eat = tmp
        eat_ap = tmp.ap[:, 0:1024]
        pst = ar.f32(64, "pst")
        exw = ar.f32(128, "exw")
        scv = sc.ap.rearrange("p (h t k) -> p h t k", h=8, t=2)
        v16q = v16.ap.rearrange("p (h t r) -> p h t r", h=8, t=2)
        for blk in range(NB):
            r0 = blk * 128
            kb.dma("sp", mix_ap, mix_d[r0:r0 + 128, :], "mix_ld", (), [mix_sb])
            kb.dma("sp", xt5.ap, xm[r0:r0 + 128, :], "xt5", (), [xt5])
            pv = v3(pbf(0, 2), KC)
            kb.tr([(pv[:, k, :], mix_ap[:, k * 128:(k + 1) * 128], ident_b.ap) for k in range(KC)], [mix_sb, ident_b], PB[0:2])
            kb.copy("act", mixT.ap, pbf(0, 2), PB[0:2], [mixT])
            for q4 in range(4):
                kb.mm([(pf(2 + q4), mixTv[:, k, :], wov[:, k, q4 * 512:(q4 + 1) * 512], k == 0, k == KC - 1) for k in range(KC)],
                      [mixT, wo], [PB[2 + q4]])
            kb.tt("dve", h1.ap, pf(2, 4), xt5.ap, ALU.add, PB[2:6] + [xt5], [h1])
            kb.dma("sp", h1_d[r0:r0 + 128, :], h1.ap, "h1_st", [h1], ())
            kb.act(xs2.ap, h1.ap, AF.Square, [h1], [xs2, pst], accum_out=pst[:, 0:1])
            kb.ts("dve", pst[:, 1:2], pst[:, 0:1], 1.0 / D, EPS, ALU.mult, ALU.add, [pst], [pst])
            kb.act(pst[:, 1:2], pst[:, 1:2], AF.Sqrt, [pst], [pst])
            kb.recip(pst[:, 2:3], pst[:, 1:2], [pst], [pst])
            kb.ts("dve", xs2.ap, h1.ap, pst[:, 2:3], None, ALU.mult, None, [h1, pst], [xs2])
            pv2 = v3(pbf(6, 2), KC)
            kb.tr([(pv2[:, k, :], xs2[:, k * 128:(k + 1) * 128], ident_b.ap) for k in range(KC)], [xs2, ident_b], PB[6:8])
            kb.acts([(xn2v[:, k, :], pv2[:, k, :], AF.Identity,
                      dict(scale=colv[:, C_G2, k:k + 1], bias=colv[:, C_SH2, k:k + 1])) for k in range(KC)],
                    PB[6:8] + [cols], [xn2T])
            kb.dma("sp", xn2T_d[blk], xn2T.ap, "xn2_st", [xn2T], ())
            kb.mm([(pf(2, 4)[:, c * 128:(c + 1) * 128], wqv[:, k, c * 128:(c + 1) * 128], xn2v[:, k, :], k == 0, k == KC - 1)
                   for c in range(16) for k in range(KC)], [wq, xn2T], PB[2:6])
            kb.copy("act", qTb.ap, pf(2, 4), PB[2:6], [qTb])
            kb.mm([(pf(0, 2)[:, c * 128:(c + 1) * 128], qTv[:, c, :], skTv[:, c, :], True, True) for c in range(8)],
                  [qTb, skT], PB[0:2])
            kb.mm([(pf(6, 2)[:, (c - 8) * 128:(c - 7) * 128], qTv[:, c, :], skTv[:, c, :], True, True) for c in range(8, 16)],
                  [qTb, skT], PB[6:8])
            kb.copy("act", sc[:, 0:1024], pf(0, 2), PB[0:2], [sc])
            kb.copy("act", sc[:, 1024:2048], pf(6, 2), PB[6:8], [sc])
            fw.op("dve", lambda e: [e.max(out=v16v[:, c, 0:8], in_=sc[:, c * 128:(c + 1) * 128]) for c in range(16)][-1], [sc], [v16])
            fw.op("dve", lambda e: [e.match_replace(out=tmp[:, c * 128:(c + 1) * 128], in_to_replace=v16v[:, c, 0:8],
                                                    in_values=sc[:, c * 128:(c + 1) * 128], imm_value=-1e30) for c in range(16)][-1],
                  [sc, v16], [tmp])
            fw.op("dve", lambda e: [e.max(out=v16v[:, c, 8:16], in_=tmp[:, c * 128:(c + 1) * 128]) for c in range(16)][-1], [tmp], [v16])
            candv = cand.ap.rearrange("p (h r c) -> p h r c", h=8, r=16)
            kb.tt("dve", candv, v16q[:, :, 0, :].unsqueeze(3).to_broadcast([128, 8, 16, 16]),
                  v16q[:, :, 1, :].unsqueeze(2).to_broadcast([128, 8, 16, 16]), ALU.add, [v16], [cand])
            fw.op("dve", lambda e: [e.max(out=bestv[:, hh, 0:8], in_=cand[:, hh * 256:(hh + 1) * 256]) for hh in range(8)][-1], [cand], [best])
            fw.op("dve", lambda e: [e.match_replace(out=tmp[:, hh * 256:(hh + 1) * 256], in_to_replace=bestv[:, hh, 0:8],
                                                    in_values=cand[:, hh * 256:(hh + 1) * 256], imm_value=-1e30) for hh in range(8)][-1],
                  [cand, best], [tmp])
            fw.op("dve", lambda e: [e.max(out=bestv[:, hh, 8:16], in_=tmp[:, hh * 256:(hh + 1) * 256]) for hh in range(8)][-1], [tmp], [best])
            kb.tt("dve", v3(exw.ap, 8), bestv, bestv[:, :, 0:1].to_broadcast([128, 8, 16]), ALU.subtract, [best], [exw])
            kb.act(exw.ap, exw.ap, AF.Exp, [exw], [exw])
            kb.reduce("dve", pst[:, 8:16], v3(exw.ap, 8), AX.X, ALU.add, [exw], [pst])
            kb.recip(pst[:, 16:24], pst[:, 8:16], [pst], [pst])
            negav = v3(nega_ap, 8)
            kb.tt("dve", negav, bestv[:, :, 15:16].to_broadcast([128, 8, 128]), scv[:, :, 0, :], ALU.subtract, [best, sc], [nega])
            kb.ts("dve", nega_ap, nega_ap, -1e-5, None, ALU.add, None, [nega], [nega])
            kb.dma("sp", st_nega[blk], nega_ap, "st_st", [nega], ())
            kb.dma("sp", v3(st_s2[blk], 8), scv[:, :, 1, :], "st_st", [sc], ())
            eatv = v3(eat_ap, 8)
            kb.tt("dve", eatv, scv[:, :, 0, :], v16q[:, :, 0, 0:1].to_broadcast([128, 8, 128]), ALU.subtract, [sc, v16], [eat])
            kb.act(eat_ap, eat_ap, AF.Exp, [eat], [eat])
            ea_ap = cand.ap[:, 0:512].bitcast(BF16)
            eb_ap = cand.ap[:, 512:1024].bitcast(BF16)
            kb.tt("dve", v3(ea_ap, 8), eatv, pst[:, 16:24].unsqueeze(2).to_broadcast([128, 8, 128]), ALU.mult, [eat, pst], [cand])
            kb.tt("dve", eatv, scv[:, :, 1, :], v16q[:, :, 1, 0:1].to_broadcast([128, 8, 128]), ALU.subtract, [sc, v16], [eat])
            kb.act(eb_ap, eat_ap, AF.Exp, [eat], [cand])
            kb.dma("sp", st_ea[blk], ea_ap, "st_st", [cand], ())
            kb.dma("sp", st_eb[blk], eb_ap, "st_st", [cand], ())
        fw.barrier()
        ar.ptr = const_end

        ust = [ar.bf16(D, "ust0"), ar.bf16(D, "ust1")]
        ugr = [ar.bf16(KC * 512, "ugr0"), ar.bf16(KC * 512, "ugr1")]
        for et in range(128):
            g, sub = divmod(et, 4)
            us = ust[et % 2]
            ug = ugr[g % 2]
            ugv = v3(ug.ap, KC)
            kb.dma("pool", us.ap, peer_u[et * 128:(et + 1) * 128, :], "ust%d" % (et % 2), (), [us])
            bk = 2 * (et % 2)
            pv = v3(pbf(bk, 2), KC)
            kb.tr([(pv[:, k, :], us[:, k * 128:(k + 1) * 128], ident_b.ap) for k in range(KC)], [us, ident_b], PB[bk:bk + 2])
            if et % 2 == 0:
                kb.copy("act", ugv[:, :, sub * 128:(sub + 1) * 128], pv, PB[bk:bk + 2], [ug])
            else:
                kb.copy("dve", ugv[:, :, sub * 128:(sub + 1) * 128], pv, PB[bk:bk + 2], [ug])
            if sub == 3:
                kb.dma("sp", UT_d[g], ug.ap, "ut_st%d" % (g % 2), [ug], ())
        fw.barrier()
        ar.ptr = const_end

        ar.ptr = const_end_p6
        ut_raw = [ar.f32(KC * 256, "ut0"), ar.f32(KC * 256, "ut1")]
        vt_raw = [ar.f32(2 * D, "vt0"), ar.f32(2 * D, "vt1")]
        for t_ in ut_raw + vt_raw:
            t_.ap, t_.name = t_.ap.bitcast(BF16), t_.ap
        ut, vt = ut_raw, vt_raw
        xq = ar.bf16(TS * D, "xq")
        xqv = xq.ap.rearrange("p (t k m) -> p t k m", t=TS, k=KC)
        sn = ar.f32(TS * 1024, "sn")
        s2t = ar.f32(TS * 1024, "s2t")
        eaT = ar.bf16(TS * 1024, "eaT")
        ebT = ar.bf16(TS * 1024, "ebT")
        snv = sn.ap.rearrange("p (t h k) -> p t h k", t=TS, h=8)
        s2v = s2t.ap.rearrange("p (t h k) -> p t h k", t=TS, h=8)
        eav = eaT.ap.rearrange("p (t h k) -> p t h k", t=TS, h=8)
        ebv = ebT.ap.rearrange("p (t h k) -> p t h k", t=TS, h=8)
        acc = ar.f32(TS * D, "acc")
        accv = v3(acc.ap, TS)
        actg = [ar.bf16(512, "actg%d" % i) for i in range(2)]
        Mk = [ar.bf16(4096, "Mk%d" % i) for i in range(3)]
        Gs = [ar.bf16(512, "G0"), ar.bf16(512, "G1")]
        HmT = [ar.bf16(512, "HmT0"), ar.bf16(512, "HmT1")]
        fst = ar.f32(4, "fst")
        fng = vt[1]
        fng_ap = vt[1].name[:, 0:D]
        h2 = ut[0]
        h2_ap = ut[0].name[:, 0:D]
        junk = Mk[0]
        junk_ap = Mk[0].ap[:, 0:D]
        for stile in range(NB // TS):
            b0 = stile * TS
            for tb in range(TS):
                kb.dma("sp", xq[:, tb * D:(tb + 1) * D], xn2T_d[b0 + tb], "xq", (), [xq])
                kb.dma("sp", sn[:, tb * 1024:(tb + 1) * 1024], st_nega[b0 + tb], "sn", (), [sn])
                kb.dma("sp", s2t[:, tb * 1024:(tb + 1) * 1024], st_s2[b0 + tb], "s2t", (), [s2t])
                kb.dma("sp", eaT[:, tb * 1024:(tb + 1) * 1024], st_ea[b0 + tb], "eaT", (), [eaT])
                kb.dma("sp", ebT[:, tb * 1024:(tb + 1) * 1024], st_eb[b0 + tb], "ebT", (), [ebT])
            kb.memset("pool", acc.ap, 0.0, [acc])
            its = [(g, tb) for g in range(32) for tb in range(TS)]
            N = len(its)
            wts = {}

            def load_u(g):
                u = ut[g % 2]
                kb.dma("sp", u.ap, UT_d[g], "ut%d" % (g % 2), (), [u])

            def load_v(g):
                vv = vt[g % 2]
                kb.dma("sp", v3(vv.ap, 4), V_d[g * 512:(g + 1) * 512, :].rearrange("(i e) n -> e i n", e=128),
                       "vt%d" % (g % 2), (), [vv])

            def stA(s_):
                g, tb = its[s_]
                if tb == 0:
                    load_u(g)
                mkv = Mk[s_ % 3].ap.rearrange("p (h i j) -> p h i j", h=8, i=4)
                kb.tt("dve", mkv, s2v[:, tb].unsqueeze(2).to_broadcast([128, 8, 4, 128]),
                      snv[:, tb, :, 4 * g:4 * g + 4].unsqueeze(3).to_broadcast([128, 8, 4, 128]), ALU.is_ge, [s2t, sn], [Mk[s_ % 3]])

            def stB(s_):
                g, tb = its[s_]
                mk = Mk[s_ % 3]
                mkv = mk.ap.rearrange("p (h i j) -> p h i j", h=8, i=4)
                kb.tt("dve", mkv, mkv, ebv[:, tb].unsqueeze(2).to_broadcast([128, 8, 4, 128]), ALU.mult, [mk, ebT], [mk])
                kb.tt("dve", mkv, mkv, eav[:, tb, :, 4 * g:4 * g + 4].unsqueeze(3).to_broadcast([128, 8, 4, 128]), ALU.mult,
                      [mk, eaT], [mk])

            def stM(s_):
                g, tb = its[s_]
                uv = v3(ut[g % 2].ap, KC)
                p = s_ % 2
                kb.mm([(pf(p), xqv[:, tb, k, :], uv[:, k, :], k == 0, k == KC - 1) for k in range(KC)], [xq, ut[g % 2]], [PB[p]])
                kb.act(actg[p].ap, pf(p), AF.Gelu, [PB[p]], [actg[p]])

            def stC(s_):
                mk = Mk[s_ % 3]
                gs = Gs[s_ % 2]
                kb.tt("dve", mk[:, 0:2048], mk[:, 0:2048], mk[:, 2048:4096], ALU.add, [mk], [mk])
                kb.tt("dve", mk[:, 0:1024], mk[:, 0:1024], mk[:, 1024:2048], ALU.add, [mk], [mk])
                kb.tt("dve", gs.ap, mk[:, 0:512], mk[:, 512:1024], ALU.add, [mk], [gs])
                kb.tt("dve", gs.ap, gs.ap, actg[s_ % 2].ap, ALU.mult, [gs, actg[s_ % 2]], [gs])

            def stD(s_):
                g, tb = its[s_]
                p = s_ % 2
                gs, hT = Gs[p], HmT[p]
                vvv = v3(vt[g % 2].ap, 4)
                kb.tr([(pbf(2 + p)[:, i * 128:(i + 1) * 128], gs[:, i * 128:(i + 1) * 128], ident_b.ap) for i in range(4)],
                      [gs, ident_b], [PB[2 + p]])
                kb.copy("act", hT.ap, pbf(2 + p)[:, 0:512], [PB[2 + p]], [hT])
                sp_ = []
                for fq in range(4):
                    sp_.append((pf(4 + fq), ident_f.ap, accv[:, tb, fq * 512:(fq + 1) * 512], True, False))
                    for i in range(4):
                        sp_.append((pf(4 + fq), hT[:, i * 128:(i + 1) * 128], vvv[:, i, fq * 512:(fq + 1) * 512], False, i == 3))
                kb.mm(sp_, [hT, vt[g % 2], acc, ident_f], PB[4:8])
                kb.copy("act", accv[:, tb, :], pf(4, 4), PB[4:8], [acc])

            load_v(0)
            load_v(1)
            for s_ in range(N + 4):
                if 0 <= s_ - 3 < N:
                    stC(s_ - 3)
                if s_ < N:
                    stA(s_)
                if 0 <= s_ - 1 < N:
                    stB(s_ - 1)
                if 0 <= s_ - 4 < N:
                    stD(s_ - 4)
                    g_, tb_ = its[s_ - 4]
                    if tb_ == TS - 1 and g_ + 2 < 32:
                        load_v(g_ + 2)
                if 0 <= s_ - 2 < N:
                    stM(s_ - 2)
            kb.dma("sp", fng_ap, final_g.partition_broadcast(128), "fng", (), [fng])
            for tb in range(TS):
                r0 = (b0 + tb) * 128
                kb.dma("sp", h2_ap, h1_d[r0:r0 + 128, :], "h2_ld", (), [h2])
                kb.tt("dve", accv[:, tb, :], accv[:, tb, :], g2_bc.ap, ALU.mult, [acc, g2_bc], [acc])
                kb.tt("dve", h2_ap, h2_ap, accv[:, tb, :], ALU.add, [h2, acc], [h2])
                kb.act(junk_ap, h2_ap, AF.Square, [h2], [junk, fst], accum_out=fst[:, 0:1])
                kb.ts("dve", fst[:, 1:2], fst[:, 0:1], 1.0 / D, EPS, ALU.mult, ALU.add, [fst], [fst])
                kb.act(fst[:, 1:2], fst[:, 1:2], AF.Sqrt, [fst], [fst])
                kb.recip(fst[:, 2:3], fst[:, 1:2], [fst], [fst])
                kb.stt("dve", h2_ap, h2_ap, fst[:, 2:3], fng_ap, ALU.mult, ALU.mult, [h2, fst, fng], [h2])
                kb.dma("sp", y[r0:r0 + 128, :], h2_ap, "y_out", [h2], ())
        fw.barrier()
        fw.emit(st)
    return nc


_PROG_CACHE = {}


def make_in_maps(cfg, inp):
    NB, T = cfg.NB, cfg.TPC
    f32 = lambda a: np.ascontiguousarray(np.asarray(a, dtype=np.float32))
    shared = dict(
        cctx=f32(inp["c_ctx"]), norm1_g=f32(inp["norm1_g"][0]), norm2_g=f32(inp["norm2_g"][0]),
        w_mod=f32(inp["w_mod"][0]), b_mod=f32(inp["b_mod"][0]), w_in=f32(inp["w_in"][0]),
        w_gate_up=f32(inp["w_gate_up"][0]), b_gate=f32(inp["b_gate"][0]), gla_norm_g=f32(inp["gla_norm_g"][0]),
        cmlp_ln_g=f32(inp["cmlp_ln_g"][0]), cmlp_ln_b=f32(inp["cmlp_ln_b"][0]), w_spatial=f32(inp["w_spatial"][0]),
        b_spatial=f32(inp["b_spatial"][0]), w_out=f32(inp["w_out"][0]), peer_wq=f32(inp["peer_wq"][0]),
        peer_sk=f32(inp["peer_sub_keys"][0]), peer_u=f32(inp["peer_u"][0]), peer_v=f32(inp["peer_v"][0]),
        final_g=f32(inp["final_norm_g"]),
    )
    x = np.asarray(inp["x"], dtype=np.float32)
    ctx = np.asarray(inp["ctx"], dtype=np.float32)
    c = np.asarray(inp["c"], dtype=np.float32)
    maps = []
    for core in range(8):
        b, seg = divmod(core, 4)
        xb = x[b]
        slots = [(s, 1.0) for s in range(seg)] + [(s, 0.0) for s in range(3, seg, -1)]
        flags = np.zeros((128, 8), np.float32)
        parts = []
        for j, (s, f) in enumerate(slots):
            blocks = xb[s * T:(s + 1) * T].reshape(NB, 128, D)
            if f == 0.0:
                blocks = blocks[::-1]
            parts.append(blocks.reshape(T, D))
            flags[:, j] = f
        flags[:, 3] = 1.0
        m = dict(shared)
        m.update(xm=f32(xb[seg * T:(seg + 1) * T]), xo=f32(np.concatenate(parts, 0)), ctxb=f32(ctx[b]),
                 cvec=f32(c[b]), flags=flags)
        maps.append(m)
    return maps


def kernel(**inp):
    seq = int(np.asarray(inp["x"]).shape[1])
    cfg = Cfg(seq, int(np.asarray(inp["ctx"]).shape[1]))
    if seq not in _PROG_CACHE:
        _PROG_CACHE[seq] = build_program(cfg)
    nc = _PROG_CACHE[seq]
    maps = make_in_maps(cfg, inp)
    res = run_bass_kernel_spmd(nc, maps, core_ids=list(range(8)))
    out = np.empty((2, seq, D), np.float32)
    for core in range(8):
        b, seg = divmod(core, 4)
        out[b, seg * cfg.TPC:(seg + 1) * cfg.TPC] = res.results[core]["y"]
    return out
```

```python
import numpy as np
import concourse.bass as bass
import concourse.mybir as mybir
from concourse.bass_utils import run_bass_kernel_spmd

F32 = mybir.dt.float32
BF16 = mybir.dt.bfloat16
ALU = mybir.AluOpType
AF = mybir.ActivationFunctionType
AX = mybir.AxisListType


class Tl:
    __slots__ = ("ap", "name", "w", "r")

    def __init__(self, ap, name=""):
        self.ap = ap
        self.name = name
        self.w = None
        self.r = {}

    def __getitem__(self, k):
        return self.ap[k]


class Op:
    __slots__ = ("idx", "eng", "fn", "deps", "dma_key", "val", "signal", "dma_waits")


class FW:
    ENGS = ("pe", "act", "dve", "pool", "sp")

    def __init__(self, nc):
        self.nc = nc
        self.ops = []
        self.dma_cnt = {}

    def op(self, eng, fn, reads=(), writes=(), dma_key=None):
        o = Op()
        o.idx = len(self.ops)
        o.eng = eng
        o.fn = fn
        o.dma_key = dma_key
        o.signal = False
        o.val = 0
        deps = set()
        for t in reads:
            if t.w is not None:
                deps.add(t.w)
        for t in writes:
            if t.w is not None:
                deps.add(t.w)
            deps.update(t.r.values())
        o.deps = deps
        o.dma_waits = {}
        for d in deps:
            od = self.ops[d]
            if od.dma_key is not None:
                o.dma_waits[od.dma_key] = self.dma_cnt[od.dma_key]
        if dma_key is not None:
            self.dma_cnt[dma_key] = self.dma_cnt.get(dma_key, 0) + 16
            o.val = self.dma_cnt[dma_key]
        rkey = ("dma", dma_key) if dma_key is not None else eng
        for t in reads:
            t.r[rkey] = o.idx
        for t in writes:
            t.w = o.idx
            t.r = {}
        self.ops.append(o)
        return o

    def emit(self, stack):
        nc = self.nc
        ops = self.ops
        for o in ops:
            for d in o.deps:
                if ops[d].dma_key is None:
                    ops[d].signal = True
        cnt = {e: 0 for e in self.ENGS}
        for o in ops:
            if o.dma_key is None and o.signal:
                cnt[o.eng] += 1
                o.val = cnt[o.eng]
        esem = {e: stack.enter_context(nc.semaphore("s_" + e)) for e in self.ENGS}
        dsem = {k: stack.enter_context(nc.semaphore("d_%d" % i))
                for i, k in enumerate(self.dma_cnt)}
        streams = {e: [o for o in ops if o.eng == e] for e in self.ENGS}

        def run(eng_name, e):
            waited = {}
            for o in streams[eng_name]:
                waits = {}
                for d in o.deps:
                    od = ops[d]
                    if od.dma_key is not None:
                        key = ("d", od.dma_key)
                        v = o.dma_waits[od.dma_key]
                    else:
                        if od.eng == eng_name and eng_name == "pe":
                            continue
                        key = ("e", od.eng)
                        v = od.val
                    if waits.get(key, 0) < v:
                        waits[key] = v
                for key, v in waits.items():
                    if waited.get(key, 0) >= v:
                        continue
                    s = dsem[key[1]] if key[0] == "d" else esem[key[1]]
                    e.wait_ge(s, v)
                    waited[key] = v
                ins = o.fn(e)
                if o.dma_key is not None:
                    ins.then_inc(dsem[o.dma_key], 16)
                elif o.signal:
                    ins.then_inc(esem[eng_name], 1)

        block = stack.enter_context(nc.Block())

        @block.tensor
        def _(e):
            run("pe", e)

        @block.scalar
        def _(e):
            run("act", e)

        @block.vector
        def _(e):
            run("dve", e)

        @block.gpsimd
        def _(e):
            run("pool", e)

        @block.sync
        def _(e):
            run("sp", e)

    def barrier(self):
        last = {}
        for o in self.ops:
            last[o.eng if o.dma_key is None else ("d", o.dma_key)] = o.idx
        deps = set(last.values())
        for e in self.ENGS:
            o = self.op(e, lambda en: en.nop())
            o.deps |= deps
            for d in deps:
                k = self.ops[d].dma_key
                if k is not None:
                    o.dma_waits[k] = self.dma_cnt[k]


D = 2048
KC = 16
NE = 16384
EPS = 1e-6
IN_W = 5152


class Cfg:
    def __init__(self, seq, ctx=256):
        self.SEQ = seq
        self.CTX = ctx
        self.TPC = seq // 4
        self.NB = self.TPC // 128
        self.TS = min(4, self.NB)


class Arena:
    def __init__(self, ap, words):
        self.ap = ap
        self.ptr = 0
        self.words = words

    def f32(self, n, name=""):
        off = self.ptr
        self.ptr += n
        assert self.ptr <= self.words, (name, self.ptr, self.words)
        return Tl(self.ap[:, off:off + n], name)

    def bf16(self, n, name=""):
        w = (n + 1) // 2
        off = self.ptr
        self.ptr += w
        assert self.ptr <= self.words, (name, self.ptr, self.words)
        return Tl(self.ap[:, off:off + w].bitcast(BF16), name)


def v3(ap, a):
    return ap.rearrange("p (a b) -> p a b", a=a)


class KB:
    def __init__(self, nc, fw):
        self.nc = nc
        self.fw = fw

    def dma(self, q, out_ap, in_ap, key, reads=(), writes=(), nc_ok=False):
        nc = self.nc

        def fn(e):
            if nc_ok:
                with nc.allow_non_contiguous_dma(reason="small strided load"):
                    return e.dma_start(out=out_ap, in_=in_ap)
            return e.dma_start(out=out_ap, in_=in_ap)
        return self.fw.op(q, fn, reads, writes, dma_key=key)

    def act(self, out, in_, func, reads, writes, **kw):
        return self.fw.op("act", lambda e: e.activation(out=out, in_=in_, func=func, **kw), reads, writes)

    def acts(self, specs, reads, writes):
        def fn(e):
            ins = None
            for (out, in_, func, kw) in specs:
                ins = e.activation(out=out, in_=in_, func=func, **kw)
            return ins
        return self.fw.op("act", fn, reads, writes)

    def tt(self, eng, out, in0, in1, op, reads, writes):
        return self.fw.op(eng, lambda e: e.tensor_tensor(out=out, in0=in0, in1=in1, op=op), reads, writes)

    def ts(self, eng, out, in0, s1, s2, op0, op1, reads, writes):
        if s2 is None:
            return self.fw.op(eng, lambda e: e.tensor_scalar(out=out, in0=in0, scalar1=s1, scalar2=None, op0=op0), reads, writes)
        return self.fw.op(eng, lambda e: e.tensor_scalar(out=out, in0=in0, scalar1=s1, scalar2=s2, op0=op0, op1=op1), reads, writes)

    def stt(self, eng, out, in0, scalar, in1, op0, op1, reads, writes):
        return self.fw.op(eng, lambda e: e.scalar_tensor_tensor(out=out, in0=in0, scalar=scalar, in1=in1, op0=op0, op1=op1), reads, writes)

    def stts(self, eng, specs, reads, writes):
        def fn(e):
            ins = None
            for (out, in0, scalar, in1, op0, op1) in specs:
                ins = e.scalar_tensor_tensor(out=out, in0=in0, scalar=scalar, in1=in1, op0=op0, op1=op1)
            return ins
        return self.fw.op(eng, fn, reads, writes)

    def copy(self, eng, out, in_, reads, writes):
        if eng == "act":
            return self.fw.op("act", lambda e: e.activation(out=out, in_=in_, func=AF.Copy), reads, writes)
        return self.fw.op(eng, lambda e: e.tensor_copy(out=out, in_=in_), reads, writes)

    def memset(self, eng, out, val, writes):
        return self.fw.op(eng, lambda e: e.memset(out, val), (), writes)

    def mm(self, specs, reads, writes):
        def fn(e):
            ins = None
            for (out, lhsT, rhs, start, stop) in specs:
                ins = e.matmul(out, lhsT=lhsT, rhs=rhs, start=start, stop=stop)
            return ins
        return self.fw.op("pe", fn, reads, writes)

    def tr(self, specs, reads, writes):
        def fn(e):
            ins = None
            for (out, in_, ident) in specs:
                ins = e.transpose(out=out, in_=in_, identity=ident)
            return ins
        return self.fw.op("pe", fn, reads, writes)

    def reduce(self, eng, out, in_, axis, op, reads, writes):
        nc = self.nc

        def fn(e):
            with nc.allow_low_precision(reason="bf16 gate sum feeds a bf16 matmul operand"):
                return e.tensor_reduce(out=out, in_=in_, axis=axis, op=op)
        return self.fw.op(eng, fn, reads, writes)

    def recip(self, out, in_, reads, writes):
        return self.fw.op("dve", lambda e: e.reciprocal(out=out, in_=in_), reads, writes)

    def rstd(self, out, ss, n, tmp, reads, writes):
        self.ts("dve", tmp, ss, 1.0 / n, EPS, ALU.mult, ALU.add, reads, writes)
        self.act(tmp, tmp, AF.Sqrt, writes, writes)
        self.recip(out, tmp, writes, writes)


AW = 53200


def build_program(cfg, debug=False):
    from contextlib import ExitStack
    NB, TPC, TS = cfg.NB, cfg.TPC, cfg.TS
    nc = bass.Bass("TRN2", target_bir_lowering=False)

    def din(name, shape, dt=F32):
        return nc.dram_tensor(name, list(shape), dt, kind="ExternalInput").ap()

    def dscr(name, shape, dt):
        return nc.dram_tensor(name, list(shape), dt, kind="ExternalOutput" if debug else "Internal").ap()

    xm = din("xm", [TPC, D])
    xo = din("xo", [3 * TPC, D])
    ctxb = din("ctxb", [cfg.CTX, D])
    cvec = din("cvec", [D])
    cctx = din("cctx", [D])
    flags = din("flags", [128, 8])
    norm1_g = din("norm1_g", [D])
    norm2_g = din("norm2_g", [D])
    w_mod = din("w_mod", [D, 6 * D])
    b_mod = din("b_mod", [6 * D])
    w_in = din("w_in", [D, IN_W])
    w_gate_up = din("w_gate_up", [2, 16, 512])
    b_gate = din("b_gate", [2, 512])
    gla_norm_g = din("gla_norm_g", [1024])
    cmlp_ln_g = din("cmlp_ln_g", [1024])
    cmlp_ln_b = din("cmlp_ln_b", [1024])
    w_spatial = din("w_spatial", [8, 128, 128])
    b_spatial = din("b_spatial", [8, 128])
    w_out = din("w_out", [D, D])
    peer_wq = din("peer_wq", [D, D])
    peer_sk = din("peer_sk", [2, 8, 128, 128])
    peer_u = din("peer_u", [NE, D])
    peer_v = din("peer_v", [NE, D])
    final_g = din("final_g", [D])
    y = nc.dram_tensor("y", [TPC, D], F32, kind="ExternalOutput").ap()

    of_d = dscr("of_d", [TPC, 1024], F32)
    mix_d = dscr("mix_d", [TPC, D], BF16)
    h1_d = dscr("h1_d", [TPC, D], F32)
    xn2T_d = dscr("xn2T_d", [NB, 128, D], BF16)
    st_nega = dscr("st_nega", [NB, 128, 1024], F32)
    st_s2 = dscr("st_s2", [NB, 128, 1024], F32)
    st_ea = dscr("st_ea", [NB, 128, 1024], BF16)
    st_eb = dscr("st_eb", [NB, 128, 1024], BF16)
    UT_d = dscr("UT_d", [32, 128, 8192], BF16)
    V_d = dscr("V_d", [NE, D], BF16)

    st = ExitStack()
    with st:
        fw = FW(nc)
        kb = KB(nc, fw)
        arena_t = st.enter_context(nc.sbuf_tensor("arena", [128, AW], F32))
        psum_t = st.enter_context(nc.psum_tensor("psum", [128, 4096], F32))
        ar = Arena(arena_t, AW)
        PB = [Tl(psum_t[:, b * 512:(b + 1) * 512], "B%d" % b) for b in range(8)]

        def pf(b0, nb=1):
            return psum_t[:, b0 * 512:(b0 + nb) * 512]

        def pbf(b0, nb=1):
            return psum_t[:, b0 * 512:(b0 + nb) * 512].bitcast(BF16)

        ident_f = ar.f32(128, "ident_f")
        A_le = ar.f32(128, "A_le")
        A_ge = ar.f32(128, "A_ge")
        A_gt = ar.f32(128, "A_gt")
        A_lt = ar.f32(128, "A_lt")
        ident_b = ar.bf16(128, "ident_b")
        cols = ar.f32(16 * 8, "cols")
        fl = ar.f32(8, "fl")
        fl2 = ar.f32(24, "fl2")
        g2_bc = ar.f32(D, "g2_bc")

        def tri(tl, cmp, sign):
            kb.memset("pool", tl.ap, 0.0, [tl])
            fw.op("pool", lambda e: e.affine_select(out=tl.ap, in_=tl.ap, pattern=[[-sign, 128]], compare_op=cmp,
                                                    fill=1.0, base=0, channel_multiplier=sign), [tl], [tl])
        tri(ident_f, ALU.not_equal, 1)
        tri(A_le, ALU.is_gt, 1)
        tri(A_ge, ALU.is_gt, -1)
        kb.tt("dve", A_gt.ap, A_ge.ap, ident_f.ap, ALU.subtract, [A_ge, ident_f], [A_gt])
        kb.tt("dve", A_lt.ap, A_le.ap, ident_f.ap, ALU.subtract, [A_le, ident_f], [A_lt])
        kb.copy("dve", ident_b.ap, ident_f.ap, [ident_f], [ident_b])
        kb.dma("sp", fl.ap, flags[:, :], "misc", (), [fl])
        kb.ts("dve", fl2[:, 0:8], fl.ap, -1.0 / 16.0, None, ALU.mult, None, [fl], [fl2])
        kb.ts("dve", fl2[:, 16:24], fl.ap, -1.0, 1.0, ALU.mult, ALU.add, [fl], [fl2])
        kb.ts("dve", fl2[:, 8:16], fl2[:, 16:24], -1.0 / 16.0, None, ALU.mult, None, [fl2], [fl2])
        colv = v3(cols.ap, 8)
        C_G1, C_SH1, C_CG1, C_CSH1, C_G2, C_SH2, C_N1, C_N2 = range(8)
        const_end_p6 = ar.ptr
        g1_bc = ar.f32(D, "g1_bc")
        const_end = ar.ptr

        M = ar.f32(6 * D, "M")
        Mc = ar.f32(2 * D, "Mc")
        rep = ar.f32(2 * KC * 128, "rep")
        ccol = ar.f32(32, "ccol")
        wb = [ar.f32(KC * 512, "wb0"), ar.f32(KC * 512, "wb1")]
        repv = rep.ap.rearrange("p (t k m) -> p t k m", t=2, k=KC)
        kb.dma("sp", ccol[:, 0:16], cvec.rearrange("(k p) -> p k", p=128), "misc", (), [ccol], nc_ok=True)
        kb.dma("sp", ccol[:, 16:32], cctx.rearrange("(k p) -> p k", p=128), "misc", (), [ccol], nc_ok=True)
        kb.dma("sp", colv[:, C_N1, :], norm1_g.rearrange("(k p) -> p k", p=128), "misc", (), [cols], nc_ok=True)
        kb.dma("sp", colv[:, C_N2, :], norm2_g.rearrange("(k p) -> p k", p=128), "misc", (), [cols], nc_ok=True)
        kb.dma("sp", M.ap, b_mod.partition_broadcast(128), "misc", (), [M])
        kb.dma("sp", Mc.ap, b_mod[0:2 * D].partition_broadcast(128), "misc", (), [Mc])
        kb.act(ccol.ap, ccol.ap, AF.Silu, [ccol], [ccol])
        kb.copy("dve", repv[:, 0], ccol[:, 0:16].unsqueeze(2).to_broadcast([128, KC, 128]), [ccol], [rep])
        kb.copy("dve", repv[:, 1], ccol[:, 16:32].unsqueeze(2).to_broadcast([128, KC, 128]), [ccol], [rep])
        wmv = w_mod.rearrange("(k p) n -> p k n", p=128)
        for cb in range(24):
            w = wb[cb % 2]
            kb.dma("sp", v3(w.ap, KC), wmv[:, :, cb * 512:(cb + 1) * 512], "wb%d" % (cb % 2), (), [w])
            bk = cb % 2
            kb.mm([(pf(bk), repv[:, 0, k, :], v3(w.ap, KC)[:, k, :], k == 0, k == KC - 1) for k in range(KC)],
                  [rep, w], [PB[bk]])
            kb.tt("dve", M[:, cb * 512:(cb + 1) * 512], pf(bk), M[:, cb * 512:(cb + 1) * 512], ALU.add, [PB[bk], M], [M])
            if cb < 8:
                bk2 = 2 + cb % 2
                kb.mm([(pf(bk2), repv[:, 1, k, :], v3(w.ap, KC)[:, k, :], k == 0, k == KC - 1) for k in range(KC)],
                      [rep, w], [PB[bk2]])
                kb.tt("dve", Mc[:, cb * 512:(cb + 1) * 512], pf(bk2), Mc[:, cb * 512:(cb + 1) * 512], ALU.add, [PB[bk2], Mc], [Mc])
        tmpc = ar.f32(6 * 16, "tmpc")
        tmpcv = v3(tmpc.ap, 6)
        srcs = [(M, 0), (M, 1), (M, 3), (M, 4), (Mc, 0), (Mc, 1)]
        for i, (src, ch) in enumerate(srcs):
            kb.tr([(pf(4, 4)[:, k * 128:(k + 1) * 128], src[:, ch * D + k * 128: ch * D + (k + 1) * 128], ident_f.ap)
                   for k in range(KC)], [src, ident_f], PB[4:8])
            kb.copy("dve", tmpcv[:, i, :], v3(pf(4, 4), KC)[:, :, 0], PB[4:8], [tmpc])
        kb.copy("act", g1_bc.ap, M[:, 2 * D:3 * D], [M], [g1_bc])
        kb.copy("act", g2_bc.ap, M[:, 5 * D:6 * D], [M], [g2_bc])
        for (dst, sc_i, n_i) in ((C_G1, 1, C_N1), (C_G2, 3, C_N2), (C_CG1, 5, C_N1)):
            kb.stt("dve", colv[:, dst, :], tmpcv[:, sc_i, :], 1.0, colv[:, n_i, :], ALU.add, ALU.mult, [tmpc, cols], [cols])
        for (dst, sh_i) in ((C_SH1, 0), (C_SH2, 2), (C_CSH1, 4)):
            kb.copy("dve", colv[:, dst, :], tmpcv[:, sh_i, :], [tmpc], [cols])
        fw.barrier()
        ar.ptr = const_end

        class FE:
            def __init__(self, tb0):
                self.xt = [ar.f32(D, "xt0"), ar.f32(D, "xt1")]
                self.xs = ar.bf16(D, "xs")
                self.xnT = ar.bf16(D, "xnT")
                self.stt_ = ar.f32(4, "fe_st")
                self.n = 0
                self.tb0 = tb0

            def load(self, rows_ap, key="xt"):
                t = self.xt[self.n % 2]
                kb.dma("sp", t.ap, rows_ap, "%s%d" % (key, self.n % 2), (), [t])
                return t

            def norm_T(self, t, gi, si):
                s = self.stt_
                tb0 = self.tb0
                kb.act(self.xs.ap, t.ap, AF.Square, [t], [self.xs, s], accum_out=s[:, 0:1])
                kb.ts("dve", s[:, 1:2], s[:, 0:1], 1.0 / D, EPS, ALU.mult, ALU.add, [s], [s])
                kb.act(s[:, 1:2], s[:, 1:2], AF.Sqrt, [s], [s])
                kb.recip(s[:, 2:3], s[:, 1:2], [s], [s])
                kb.ts("dve", self.xs.ap, t.ap, s[:, 2:3], None, ALU.mult, None, [t, s], [self.xs])
                pv = v3(pbf(tb0, 2), KC)
                kb.tr([(pv[:, k, :], self.xs[:, k * 128:(k + 1) * 128], ident_b.ap) for k in range(KC)],
                      [self.xs, ident_b], PB[tb0:tb0 + 2])
                xv = v3(self.xnT.ap, KC)
                kb.acts([(xv[:, k, :], pv[:, k, :], AF.Identity,
                          dict(scale=colv[:, gi, k:k + 1], bias=colv[:, si, k:k + 1])) for k in range(KC)],
                        PB[tb0:tb0 + 2] + [cols], [self.xnT])
                self.n += 1
                return xv

        GW = 3104
        wg = ar.bf16(KC * GW, "wg")
        wgv = v3(wg.ap, KC)
        kb.dma("pool", wgv, w_in.rearrange("(k p) n -> p k n", p=128)[:, :, 0:GW], "wg", (), [wg])
        for et in range(64):
            kb.dma("pool", V_d[et * 256:(et + 1) * 256, :], peer_v[et * 256:(et + 1) * 256, :], "vcast", (), ())
        Wga = [ar.f32(512, "Wga0"), ar.f32(512, "Wga1")]
        for d in range(2):
            kb.memset("pool", Wga[d].ap, 0.0, [Wga[d]])
            kb.dma("sp", Wga[d][16 * d:16 * d + 16, :], w_gate_up[d], "misc", (), [Wga[d]])
            kb.dma("sp", Wga[d][32:33, :], b_gate[d:d + 1, :], "misc", (), [Wga[d]])
        ngbc = ar.f32(1024, "ngbc")
        kb.dma("sp", ngbc.ap, gla_norm_g.partition_broadcast(128), "misc", (), [ngbc])
        S = [ar.f32(1024, "S_f"), ar.f32(1024, "S_b")]
        Sbf = ar.bf16(1024, "Sbf")
        for d in range(2):
            kb.memset("pool", S[d].ap, 0.0, [S[d]])
        fe = FE(6)
        qT = ar.f32(512, "qT")
        kT = ar.f32(512, "kT")
        k_sb = ar.f32(512, "k_sb")
        v_sb = ar.bf16(1024, "v_sb")
        lrT = ar.f32(128, "lrT")
        e1 = ar.f32(512, "e1")
        la = ar.f32(512, "la")
        ecT = ar.f32(512, "ecT")
        encT = ar.f32(512, "encT")
        ercum = ar.f32(512, "ercum")
        kdec = ar.bf16(512, "kdec")
        qd = ar.bf16(512, "qd")
        ki = ar.bf16(512, "ki")
        scm = ar.bf16(512, "scm")
        o_sb = ar.f32(1024, "o_sb")
        of_sb = ar.f32(1024, "of_sb")
        silug = ar.f32(1024, "silug")
        ybf = ar.bf16(1024, "ybf")
        gst = ar.f32(16, "gst")
        kb.memset("pool", lrT.ap, 1.0, [lrT])
        ONE_NF16 = fl2[:, 3:4]
        ONE_F = fl[:, 3:4]

        TRI = {0: (A_le, A_gt, 127), 1: (A_ge, A_lt, 0)}

        def inproj_state(xv):
            kb.mm([(pf(2), xv[:, k, :], wgv[:, k, 512:1024], k == 0, k == KC - 1) for k in range(KC)],
                  [fe.xnT, wg], [PB[2]])
            kb.copy("act", k_sb.ap, pf(2), [PB[2]], [k_sb])
            for hf in range(2):
                kb.mm([(pf(3 + hf), xv[:, k, :], wgv[:, k, 1024 + hf * 512:1536 + hf * 512], k == 0, k == KC - 1)
                       for k in range(KC)], [fe.xnT, wg], [PB[3 + hf]])
            kb.copy("act", v_sb.ap, pf(3, 2), PB[3:5], [v_sb])
            kb.mm([(pf(5)[0:32, 0:128], wgv[:, k, 3072:3104], xv[:, k, :], k == 0, k == KC - 1) for k in range(KC)],
                  [fe.xnT, wg], [PB[5]])
            kb.copy("dve", lrT[0:32, :], pf(5)[0:32, 0:128], [PB[5]], [lrT])

        def decay_parts(d, nf16_ap):
            cumm, rcm, _ = TRI[d]
            kb.mm([(pf(2), lrT[0:33, :], Wga[d][0:33, :], True, True)], [lrT, Wga[d]], [PB[2]])
            kb.act(e1.ap, pf(2), AF.Exp, [PB[2]], [e1], scale=-1.0)
            kb.act(e1.ap, e1.ap, AF.Ln, [e1], [e1], bias=1.0)
            kb.ts("dve", la.ap, e1.ap, nf16_ap, None, ALU.mult, None, [e1, fl2], [la])
            kb.mm([(pf(5), rcm.ap, la.ap, True, True)], [rcm, la], [PB[5]])
            kb.mm([(pf(1)[:, h * 128:(h + 1) * 128], la[:, h * 128:(h + 1) * 128], cumm.ap, True, True)
                   for h in range(4)], [la, cumm], [PB[1]])
            kb.act(ecT.ap, pf(1), AF.Exp, [PB[1]], [ecT])
            kb.act(ercum.ap, pf(5), AF.Exp, [PB[5]], [ercum])

        def state_update(d, f_ap):
            col = TRI[d][2]
            kb.stt("dve", kdec.ap, ercum.ap, f_ap, k_sb.ap, ALU.mult, ALU.mult, [ercum, k_sb, fl, fl2], [kdec])
            kb.mm([(pf(3, 2)[:, h * 256:(h + 1) * 256], kdec[:, h * 128:(h + 1) * 128],
                    v_sb[:, h * 256:(h + 1) * 256], True, True) for h in range(4)], [kdec, v_sb], PB[3:5])
            kb.stts("dve", [(S[d][:, h * 256:(h + 1) * 256], S[d][:, h * 256:(h + 1) * 256],
                             ecT[:, h * 128 + col:h * 128 + col + 1], pf(3, 2)[:, h * 256:(h + 1) * 256],
                             ALU.mult, ALU.add) for h in range(4)], [S[d], ecT] + PB[3:5], [S[d]])

        def state_block(rows_ap, gi, si, fcol):
            t = fe.load(rows_ap)
            xv = fe.norm_T(t, gi, si)
            inproj_state(xv)
            decay_parts(0, fl2[:, fcol:fcol + 1])
            state_update(0, fl[:, fcol:fcol + 1])
            decay_parts(1, fl2[:, 8 + fcol:9 + fcol])
            state_update(1, fl2[:, 16 + fcol:17 + fcol])

        def main_front(blk):
            t = fe.load(xm[blk * 128:(blk + 1) * 128, :])
            return fe.norm_T(t, C_G1, C_SH1)

        def main_block(d, blk, xv, nxt):
            cumm, rcm, col = TRI[d]
            if d == 1:
                for hf in range(2):
                    kb.mm([(pf(6 + hf), xv[:, k, :], wgv[:, k, 2048 + hf * 512:2560 + hf * 512], k == 0, k == KC - 1)
                           for k in range(KC)], [fe.xnT, wg], [PB[6 + hf]])
                kb.act(silug.ap, pf(6, 2), AF.Silu, PB[6:8], [silug])
                kb.dma("sp", of_sb.ap, of_d[blk * 128:(blk + 1) * 128, :], "of_sb", [ofd_tl[blk]], [of_sb])
            kb.mm([(pf(0)[:, h * 128:(h + 1) * 128], wgv[:, k, h * 128:(h + 1) * 128], xv[:, k, :], k == 0, k == KC - 1)
                   for h in range(4) for k in range(KC)], [fe.xnT, wg], [PB[0]])
            kb.act(qT.ap, pf(0), AF.Identity, [PB[0]], [qT], scale=128.0 ** -0.5)
            kb.mm([(pf(1)[:, h * 128:(h + 1) * 128], wgv[:, k, 512 + h * 128:512 + (h + 1) * 128], xv[:, k, :], k == 0, k == KC - 1)
                   for h in range(4) for k in range(KC)], [fe.xnT, wg], [PB[1]])
            kb.copy("dve", kT.ap, pf(1), [PB[1]], [kT])
            inproj_state(xv)
            nxt_xv = main_front(nxt) if nxt is not None else None
            decay_parts(d, ONE_NF16)
            kb.act(encT.ap, pf(1), AF.Exp, [PB[1]], [encT], scale=-1.0)
            kb.tt("dve", qd.ap, qT.ap, ecT.ap, ALU.mult, [qT, ecT], [qd])
            kb.tt("dve", ki.ap, kT.ap, encT.ap, ALU.mult, [kT, encT], [ki])
            kb.mm([(pf(0)[:, h * 128:(h + 1) * 128], ki[:, h * 128:(h + 1) * 128], qd[:, h * 128:(h + 1) * 128], True, True)
                   for h in range(4)], [ki, qd], [PB[0]])
            kb.tt("dve", v3(scm.ap, 4), v3(pf(0), 4), cumm.ap.unsqueeze(1).to_broadcast([128, 4, 128]), ALU.mult,
                  [PB[0], cumm], [scm])
            sp = []
            for h in range(4):
                o_ap = pf(6, 2)[:, h * 256:(h + 1) * 256]
                sp.append((o_ap, scm[:, h * 128:(h + 1) * 128], v_sb[:, h * 256:(h + 1) * 256], True, False))
                sp.append((o_ap, qd[:, h * 128:(h + 1) * 128], Sbf[:, h * 256:(h + 1) * 256], False, True))
            kb.mm(sp, [scm, v_sb, qd, Sbf], PB[6:8])
            if d == 0:
                kb.copy("act", o_sb.ap, pf(6, 2), PB[6:8], [o_sb])
                kb.dma("sp", of_d[blk * 128:(blk + 1) * 128, :], o_sb.ap, "of_st", [o_sb], [ofd_tl[blk]])
            else:
                kb.tt("dve", o_sb.ap, pf(6, 2), of_sb.ap, ALU.add, PB[6:8] + [of_sb], [o_sb])
                kb.acts([(of_sb[:, h * 256:(h + 1) * 256], o_sb[:, h * 256:(h + 1) * 256], AF.Square,
                          dict(accum_out=gst[:, h:h + 1])) for h in range(4)], [o_sb], [of_sb, gst])
                kb.ts("dve", gst[:, 4:8], gst[:, 0:4], 1.0 / 256.0, EPS, ALU.mult, ALU.add, [gst], [gst])
                kb.act(gst[:, 4:8], gst[:, 4:8], AF.Sqrt, [gst], [gst])
                kb.recip(gst[:, 8:12], gst[:, 4:8], [gst], [gst])
                kb.stts("dve", [(o_sb[:, h * 256:(h + 1) * 256], o_sb[:, h * 256:(h + 1) * 256], gst[:, 8 + h:9 + h],
                                 ngbc[:, h * 256:(h + 1) * 256], ALU.mult, ALU.mult) for h in range(4)],
                        [o_sb, gst, ngbc], [o_sb])
                kb.tt("dve", ybf.ap, o_sb.ap, silug.ap, ALU.mult, [o_sb, silug], [ybf])
                kb.dma("sp", mix_d[blk * 128:(blk + 1) * 128, 0:1024], ybf.ap, "y_st", [ybf], ())
            state_update(d, ONE_F)
            kb.copy("pool", Sbf.ap, S[d].ap, [S[d]], [Sbf])
            return nxt_xv

        ofd_tl = [Tl(None, 'ofd') for _ in range(NB)]
        nctx = cfg.CTX // 128
        for b in range(nctx):
            state_block(ctxb[b * 128:(b + 1) * 128, :], C_CG1, C_CSH1, 3)
        for b in range(nctx - 1, -1, -1):
            state_block(ctxb[b * 128:(b + 1) * 128, :], C_CG1, C_CSH1, 4)
        for j in range(3):
            for b in range(NB):
                r0 = (j * NB + b) * 128
                state_block(xo[r0:r0 + 128, :], C_G1, C_SH1, j)
        kb.copy("pool", Sbf.ap, S[0].ap, [S[0]], [Sbf])
        xv_ = main_front(0)
        for b in range(NB):
            xv_ = main_block(0, b, xv_, b + 1 if b + 1 < NB else None)
        kb.copy("pool", Sbf.ap, S[1].ap, [S[1]], [Sbf])
        xv_ = main_front(NB - 1)
        for b in range(NB - 1, -1, -1):
            xv_ = main_block(1, b, xv_, b - 1 if b - 1 >= 0 else None)
        fw.barrier()
        ar.ptr = const_end

        wc = ar.bf16(KC * 2048, "wc")
        wcv = v3(wc.ap, KC)
        kb.dma("pool", wcv, w_in.rearrange("(k p) n -> p k n", p=128)[:, :, GW:IN_W], "wc", (), [wc])
        wsT = ar.bf16(1024, "wsT")
        wstg = ar.f32(1024, "wstg")
        bs_col = ar.f32(8, "bs_col")
        lng = ar.f32(1024, "lng")
        lnb = ar.f32(1024, "lnb")
        kb.dma("sp", v3(wstg.ap, 8), w_spatial.rearrange("g p q -> p g q"), "misc", (), [wstg])
        kb.dma("sp", bs_col.ap, b_spatial.rearrange("g p -> p g"), "misc", (), [bs_col], nc_ok=True)
        kb.dma("sp", lng.ap, cmlp_ln_g.partition_broadcast(128), "misc", (), [lng])
        kb.dma("sp", lnb.ap, cmlp_ln_b.partition_broadcast(128), "misc", (), [lnb])
        kb.tr([(pf(0, 2)[:, g * 128:(g + 1) * 128], wstg[:, g * 128:(g + 1) * 128], ident_f.ap) for g in range(8)],
              [wstg, ident_f], PB[0:2])
        kb.copy("dve", wsT.ap, pf(0, 2), PB[0:2], [wsT])
        fe = FE(6)
        gu = ar.f32(1024, "gu")
        gv = ar.f32(1024, "gv")
        vn = ar.bf16(1024, "vn")
        cm = ar.bf16(1024, "cm")
        cst = ar.f32(8, "cst")
        def p4_front(blk):
            t = fe.load(xm[blk * 128:(blk + 1) * 128, :])
            return fe.norm_T(t, C_G1, C_SH1)
        xv = p4_front(0)
        for blk in range(NB):
            for q4 in range(4):
                kb.mm([(pf(q4), xv[:, k, :], wcv[:, k, q4 * 512:(q4 + 1) * 512], k == 0, k == KC - 1) for k in range(KC)],
                      [fe.xnT, wc], [PB[q4]])
            if blk + 1 < NB:
                xv = p4_front(blk + 1)
            kb.act(gu.ap, pf(0, 2), AF.Gelu, PB[0:2], [gu])
            kb.act(gv.ap, pf(2, 2), AF.Gelu, PB[2:4], [gv, cst], accum_out=cst[:, 0:1])
            kb.ts("dve", cst[:, 1:2], cst[:, 0:1], -1.0 / 1024.0, None, ALU.mult, None, [cst], [cst])
            kb.act(vn.ap, gv.ap, AF.Square, [gv, cst], [vn, cst], bias=cst[:, 1:2], accum_out=cst[:, 2:3])
            kb.ts("dve", cst[:, 3:4], cst[:, 2:3], 1.0 / 1024.0, EPS, ALU.mult, ALU.add, [cst], [cst])
            kb.act(cst[:, 3:4], cst[:, 3:4], AF.Sqrt, [cst], [cst])
            kb.recip(cst[:, 4:5], cst[:, 3:4], [cst], [cst])
            kb.ts("dve", gv.ap, gv.ap, cst[:, 1:2], cst[:, 4:5], ALU.add, ALU.mult, [gv, cst], [gv])
            kb.tt("dve", gv.ap, gv.ap, lng.ap, ALU.mult, [gv, lng], [gv])
            kb.tt("dve", vn.ap, gv.ap, lnb.ap, ALU.add, [gv, lnb], [vn])
            kb.mm([(pf(4, 2)[:, g * 128:(g + 1) * 128], wsT[:, g * 128:(g + 1) * 128], vn[:, g * 128:(g + 1) * 128], True, True)
                   for g in range(8)], [wsT, vn], PB[4:6])
            kb.stts("dve", [(cm[:, g * 128:(g + 1) * 128], pf(4, 2)[:, g * 128:(g + 1) * 128], bs_col[:, g:g + 1],
                             gu[:, g * 128:(g + 1) * 128], ALU.add, ALU.mult) for g in range(8)],
                    PB[4:6] + [bs_col, gu], [cm])
            kb.dma("sp", mix_d[blk * 128:(blk + 1) * 128, 1024:2048], cm.ap, "cm_st", [cm], ())
        fw.barrier()
        ar.ptr = const_end

        wo = ar.bf16(KC * D, "wo")
        wov = v3(wo.ap, KC)
        wq = ar.bf16(KC * D, "wq")
        wqv = v3(wq.ap, KC)
        skT = ar.bf16(D, "skT")
        skTv = v3(skT.ap, 16)
        kb.dma("pool", wqv, peer_wq.rearrange("(k p) n -> p k n", p=128), "wq", (), [wq])
        h1 = ar.f32(D, "h1")
        sc = ar.f32(D, "sc")
        tmp = ar.f32(D, "tmp")
        stg = [h1, sc]
        for k in range(KC):
            sg = stg[k % 2]
            kb.dma("sp", sg.ap, w_out[k * 128:(k + 1) * 128, :], "wo_st%d" % (k % 2), (), [sg])
            kb.tt("dve", wov[:, k, :], sg.ap, g1_bc.ap, ALU.mult, [sg, g1_bc], [wo])
        kb.dma("sp", v3(tmp.ap, 16), peer_sk.rearrange("t h k d -> k (t h) d"), "misc", (), [tmp])
        specs = []
        for half in range(2):
            for hh in range(8):
                c = hh * 2 + half
                specs.append((pf(0, 4)[:, c * 128:(c + 1) * 128], tmp[:, (half * 8 + hh) * 128:(half * 8 + hh + 1) * 128], ident_f.ap))
        kb.tr(specs, [tmp, ident_f], PB[0:4])
        kb.copy("dve", skT.ap, pf(0, 4), PB[0:4], [skT])
        mixT = ar.bf16(D, "mixT")
        mixTv = v3(mixT.ap, KC)
        xt5 = sc
        xn2T = ar.bf16(D, "xn2T")
        xn2v = v3(xn2T.ap, KC)
        qTb = ar.bf16(D, "qTb")
        qTv = v3(qTb.ap, 16)
        v16 = ar.f32(256, "v16")
        v16v = v3(v16.ap, 16)
        cand = ar.f32(D, "cand")
        mix_sb = cand
        mix_ap = cand.ap[:, 0:1024].bitcast(BF16)
        xs2 = mixT
        best = ar.f32(128, "best")
        bestv = v3(best.ap, 8)
        nega = tmp
        nega_ap = tmp.ap[:, 1024:2048]
        eat = tmp
        eat_ap = tmp.ap[:, 0:1024]
        pst = ar.f32(64, "pst")
        exw = ar.f32(128, "exw")
        scv = sc.ap.rearrange("p (h t k) -> p h t k", h=8, t=2)
        v16q = v16.ap.rearrange("p (h t r) -> p h t r", h=8, t=2)
        for blk in range(NB):
            r0 = blk * 128
            kb.dma("sp", mix_ap, mix_d[r0:r0 + 128, :], "mix_ld", (), [mix_sb])
            kb.dma("sp", xt5.ap, xm[r0:r0 + 128, :], "xt5", (), [xt5])
            pv = v3(pbf(0, 2), KC)
            kb.tr([(pv[:, k, :], mix_ap[:, k * 128:(k + 1) * 128], ident_b.ap) for k in range(KC)], [mix_sb, ident_b], PB[0:2])
            kb.copy("act", mixT.ap, pbf(0, 2), PB[0:2], [mixT])
            for q4 in range(4):
                kb.mm([(pf(2 + q4), mixTv[:, k, :], wov[:, k, q4 * 512:(q4 + 1) * 512], k == 0, k == KC - 1) for k in range(KC)],
                      [mixT, wo], [PB[2 + q4]])
            kb.tt("dve", h1.ap, pf(2, 4), xt5.ap, ALU.add, PB[2:6] + [xt5], [h1])
            kb.dma("sp", h1_d[r0:r0 + 128, :], h1.ap, "h1_st", [h1], ())
            kb.act(xs2.ap, h1.ap, AF.Square, [h1], [xs2, pst], accum_out=pst[:, 0:1])
            kb.ts("dve", pst[:, 1:2], pst[:, 0:1], 1.0 / D, EPS, ALU.mult, ALU.add, [pst], [pst])
            kb.act(pst[:, 1:2], pst[:, 1:2], AF.Sqrt, [pst], [pst])
            kb.recip(pst[:, 2:3], pst[:, 1:2], [pst], [pst])
            kb.ts("dve", xs2.ap, h1.ap, pst[:, 2:3], None, ALU.mult, None, [h1, pst], [xs2])
            pv2 = v3(pbf(6, 2), KC)
            kb.tr([(pv2[:, k, :], xs2[:, k * 128:(k + 1) * 128], ident_b.ap) for k in range(KC)], [xs2, ident_b], PB[6:8])
            kb.acts([(xn2v[:, k, :], pv2[:, k, :], AF.Identity,
                      dict(scale=colv[:, C_G2, k:k + 1], bias=colv[:, C_SH2, k:k + 1])) for k in range(KC)],
                    PB[6:8] + [cols], [xn2T])
            kb.dma("sp", xn2T_d[blk], xn2T.ap, "xn2_st", [xn2T], ())
            kb.mm([(pf(2, 4)[:, c * 128:(c + 1) * 128], wqv[:, k, c * 128:(c + 1) * 128], xn2v[:, k, :], k == 0, k == KC - 1)
                   for c in range(16) for k in range(KC)], [wq, xn2T], PB[2:6])
            kb.copy("act", qTb.ap, pf(2, 4), PB[2:6], [qTb])
            kb.mm([(pf(0, 2)[:, c * 128:(c + 1) * 128], qTv[:, c, :], skTv[:, c, :], True, True) for c in range(8)],
                  [qTb, skT], PB[0:2])
            kb.mm([(pf(6, 2)[:, (c - 8) * 128:(c - 7) * 128], qTv[:, c, :], skTv[:, c, :], True, True) for c in range(8, 16)],
                  [qTb, skT], PB[6:8])
            kb.copy("act", sc[:, 0:1024], pf(0, 2), PB[0:2], [sc])
            kb.copy("act", sc[:, 1024:2048], pf(6, 2), PB[6:8], [sc])
            fw.op("dve", lambda e: [e.max(out=v16v[:, c, 0:8], in_=sc[:, c * 128:(c + 1) * 128]) for c in range(16)][-1], [sc], [v16])
            fw.op("dve", lambda e: [e.match_replace(out=tmp[:, c * 128:(c + 1) * 128], in_to_replace=v16v[:, c, 0:8],
                                                    in_values=sc[:, c * 128:(c + 1) * 128], imm_value=-1e30) for c in range(16)][-1],
                  [sc, v16], [tmp])
            fw.op("dve", lambda e: [e.max(out=v16v[:, c, 8:16], in_=tmp[:, c * 128:(c + 1) * 128]) for c in range(16)][-1], [tmp], [v16])
            candv = cand.ap.rearrange("p (h r c) -> p h r c", h=8, r=16)
            kb.tt("dve", candv, v16q[:, :, 0, :].unsqueeze(3).to_broadcast([128, 8, 16, 16]),
                  v16q[:, :, 1, :].unsqueeze(2).to_broadcast([128, 8, 16, 16]), ALU.add, [v16], [cand])
            fw.op("dve", lambda e: [e.max(out=bestv[:, hh, 0:8], in_=cand[:, hh * 256:(hh + 1) * 256]) for hh in range(8)][-1], [cand], [best])
            fw.op("dve", lambda e: [e.match_replace(out=tmp[:, hh * 256:(hh + 1) * 256], in_to_replace=bestv[:, hh, 0:8],
                                                    in_values=cand[:, hh * 256:(hh + 1) * 256], imm_value=-1e30) for hh in range(8)][-1],
                  [cand, best], [tmp])
            fw.op("dve", lambda e: [e.max(out=bestv[:, hh, 8:16], in_=tmp[:, hh * 256:(hh + 1) * 256]) for hh in range(8)][-1], [tmp], [best])
            kb.tt("dve", v3(exw.ap, 8), bestv, bestv[:, :, 0:1].to_broadcast([128, 8, 16]), ALU.subtract, [best], [exw])
            kb.act(exw.ap, exw.ap, AF.Exp, [exw], [exw])
            kb.reduce("dve", pst[:, 8:16], v3(exw.ap, 8), AX.X, ALU.add, [exw], [pst])
            kb.recip(pst[:, 16:24], pst[:, 8:16], [pst], [pst])
            negav = v3(nega_ap, 8)
            kb.tt("dve", negav, bestv[:, :, 15:16].to_broadcast([128, 8, 128]), scv[:, :, 0, :], ALU.subtract, [best, sc], [nega])
            kb.ts("dve", nega_ap, nega_ap, -1e-5, None, ALU.add, None, [nega], [nega])
            kb.dma("sp", st_nega[blk], nega_ap, "st_st", [nega], ())
            kb.dma("sp", v3(st_s2[blk], 8), scv[:, :, 1, :], "st_st", [sc], ())
            eatv = v3(eat_ap, 8)
            kb.tt("dve", eatv, scv[:, :, 0, :], v16q[:, :, 0, 0:1].to_broadcast([128, 8, 128]), ALU.subtract, [sc, v16], [eat])
            kb.act(eat_ap, eat_ap, AF.Exp, [eat], [eat])
            ea_ap = cand.ap[:, 0:512].bitcast(BF16)
            eb_ap = cand.ap[:, 512:1024].bitcast(BF16)
            kb.tt("dve", v3(ea_ap, 8), eatv, pst[:, 16:24].unsqueeze(2).to_broadcast([128, 8, 128]), ALU.mult, [eat, pst], [cand])
            kb.tt("dve", eatv, scv[:, :, 1, :], v16q[:, :, 1, 0:1].to_broadcast([128, 8, 128]), ALU.subtract, [sc, v16], [eat])
            kb.act(eb_ap, eat_ap, AF.Exp, [eat], [cand])
            kb.dma("sp", st_ea[blk], ea_ap, "st_st", [cand], ())
            kb.dma("sp", st_eb[blk], eb_ap, "st_st", [cand], ())
        fw.barrier()
        ar.ptr = const_end

        ust = [ar.bf16(D, "ust0"), ar.bf16(D, "ust1")]
        ugr = [ar.bf16(KC * 512, "ugr0"), ar.bf16(KC * 512, "ugr1")]
        for et in range(128):
            g, sub = divmod(et, 4)
            us = ust[et % 2]
            ug = ugr[g % 2]
            ugv = v3(ug.ap, KC)
            kb.dma("pool", us.ap, peer_u[et * 128:(et + 1) * 128, :], "ust%d" % (et % 2), (), [us])
            bk = 2 * (et % 2)
            pv = v3(pbf(bk, 2), KC)
            kb.tr([(pv[:, k, :], us[:, k * 128:(k + 1) * 128], ident_b.ap) for k in range(KC)], [us, ident_b], PB[bk:bk + 2])
            if et % 2 == 0:
                kb.copy("act", ugv[:, :, sub * 128:(sub + 1) * 128], pv, PB[bk:bk + 2], [ug])
            else:
                kb.copy("dve", ugv[:, :, sub * 128:(sub + 1) * 128], pv, PB[bk:bk + 2], [ug])
            if sub == 3:
                kb.dma("sp", UT_d[g], ug.ap, "ut_st%d" % (g % 2), [ug], ())
        fw.barrier()
        ar.ptr = const_end

        ar.ptr = const_end_p6
        ut_raw = [ar.f32(KC * 256, "ut0"), ar.f32(KC * 256, "ut1")]
        vt_raw = [ar.f32(2 * D, "vt0"), ar.f32(2 * D, "vt1")]
        for t_ in ut_raw + vt_raw:
            t_.ap, t_.name = t_.ap.bitcast(BF16), t_.ap
        ut, vt = ut_raw, vt_raw
        xq = ar.bf16(TS * D, "xq")
        xqv = xq.ap.rearrange("p (t k m) -> p t k m", t=TS, k=KC)
        sn = ar.f32(TS * 1024, "sn")
        s2t = ar.f32(TS * 1024, "s2t")
        eaT = ar.bf16(TS * 1024, "eaT")
        ebT = ar.bf16(TS * 1024, "ebT")
        snv = sn.ap.rearrange("p (t h k) -> p t h k", t=TS, h=8)
        s2v = s2t.ap.rearrange("p (t h k) -> p t h k", t=TS, h=8)
        eav = eaT.ap.rearrange("p (t h k) -> p t h k", t=TS, h=8)
        ebv = ebT.ap.rearrange("p (t h k) -> p t h k", t=TS, h=8)
        acc = ar.f32(TS * D, "acc")
        accv = v3(acc.ap, TS)
        actg = [ar.bf16(512, "actg%d" % i) for i in range(2)]
        Mk = [ar.bf16(4096, "Mk%d" % i) for i in range(3)]
        Gs = [ar.bf16(512, "G0"), ar.bf16(512, "G1")]
        HmT = [ar.bf16(512, "HmT0"), ar.bf16(512, "HmT1")]
        fst = ar.f32(4, "fst")
        fng = vt[1]
        fng_ap = vt[1].name[:, 0:D]
        h2 = ut[0]
        h2_ap = ut[0].name[:, 0:D]
        junk = Mk[0]
        junk_ap = Mk[0].ap[:, 0:D]
        for stile in range(NB // TS):
            b0 = stile * TS
            for tb in range(TS):
                kb.dma("sp", xq[:, tb * D:(tb + 1) * D], xn2T_d[b0 + tb], "xq", (), [xq])
                kb.dma("sp", sn[:, tb * 1024:(tb + 1) * 1024], st_nega[b0 + tb], "sn", (), [sn])
                kb.dma("sp", s2t[:, tb * 1024:(tb + 1) * 1024], st_s2[b0 + tb], "s2t", (), [s2t])
                kb.dma("sp", eaT[:, tb * 1024:(tb + 1) * 1024], st_ea[b0 + tb], "eaT", (), [eaT])
                kb.dma("sp", ebT[:, tb * 1024:(tb + 1) * 1024], st_eb[b0 + tb], "ebT", (), [ebT])
            kb.memset("pool", acc.ap, 0.0, [acc])
            its = [(g, tb) for g in range(32) for tb in range(TS)]
            N = len(its)
            wts = {}

            def load_u(g):
                u = ut[g % 2]
                kb.dma("sp", u.ap, UT_d[g], "ut%d" % (g % 2), (), [u])

            def load_v(g):
                vv = vt[g % 2]
                kb.dma("sp", v3(vv.ap, 4), V_d[g * 512:(g + 1) * 512, :].rearrange("(i e) n -> e i n", e=128),
                       "vt%d" % (g % 2), (), [vv])

            def stA(s_):
                g, tb = its[s_]
                if tb == 0:
                    load_u(g)
                mkv = Mk[s_ % 3].ap.rearrange("p (h i j) -> p h i j", h=8, i=4)
                kb.tt("dve", mkv, s2v[:, tb].unsqueeze(2).to_broadcast([128, 8, 4, 128]),
                      snv[:, tb, :, 4 * g:4 * g + 4].unsqueeze(3).to_broadcast([128, 8, 4, 128]), ALU.is_ge, [s2t, sn], [Mk[s_ % 3]])

            def stB(s_):
                g, tb = its[s_]
                mk = Mk[s_ % 3]
                mkv = mk.ap.rearrange("p (h i j) -> p h i j", h=8, i=4)
                kb.tt("dve", mkv, mkv, ebv[:, tb].unsqueeze(2).to_broadcast([128, 8, 4, 128]), ALU.mult, [mk, ebT], [mk])
                kb.tt("dve", mkv, mkv, eav[:, tb, :, 4 * g:4 * g + 4].unsqueeze(3).to_broadcast([128, 8, 4, 128]), ALU.mult,
                      [mk, eaT], [mk])

            def stM(s_):
                g, tb = its[s_]
                uv = v3(ut[g % 2].ap, KC)
                p = s_ % 2
                kb.mm([(pf(p), xqv[:, tb, k, :], uv[:, k, :], k == 0, k == KC - 1) for k in range(KC)], [xq, ut[g % 2]], [PB[p]])
                kb.act(actg[p].ap, pf(p), AF.Gelu, [PB[p]], [actg[p]])

            def stC(s_):
                mk = Mk[s_ % 3]
                gs = Gs[s_ % 2]
                kb.tt("dve", mk[:, 0:2048], mk[:, 0:2048], mk[:, 2048:4096], ALU.add, [mk], [mk])
                kb.tt("dve", mk[:, 0:1024], mk[:, 0:1024], mk[:, 1024:2048], ALU.add, [mk], [mk])
                kb.tt("dve", gs.ap, mk[:, 0:512], mk[:, 512:1024], ALU.add, [mk], [gs])
                kb.tt("dve", gs.ap, gs.ap, actg[s_ % 2].ap, ALU.mult, [gs, actg[s_ % 2]], [gs])

            def stD(s_):
                g, tb = its[s_]
                p = s_ % 2
                gs, hT = Gs[p], HmT[p]
                vvv = v3(vt[g % 2].ap, 4)
                kb.tr([(pbf(2 + p)[:, i * 128:(i + 1) * 128], gs[:, i * 128:(i + 1) * 128], ident_b.ap) for i in range(4)],
                      [gs, ident_b], [PB[2 + p]])
                kb.copy("act", hT.ap, pbf(2 + p)[:, 0:512], [PB[2 + p]], [hT])
                sp_ = []
                for fq in range(4):
                    sp_.append((pf(4 + fq), ident_f.ap, accv[:, tb, fq * 512:(fq + 1) * 512], True, False))
                    for i in range(4):
                        sp_.append((pf(4 + fq), hT[:, i * 128:(i + 1) * 128], vvv[:, i, fq * 512:(fq + 1) * 512], False, i == 3))
                kb.mm(sp_, [hT, vt[g % 2], acc, ident_f], PB[4:8])
                kb.copy("act", accv[:, tb, :], pf(4, 4), PB[4:8], [acc])

            load_v(0)
            load_v(1)
            for s_ in range(N + 4):
                if 0 <= s_ - 3 < N:
                    stC(s_ - 3)
                if s_ < N:
                    stA(s_)
                if 0 <= s_ - 1 < N:
                    stB(s_ - 1)
                if 0 <= s_ - 4 < N:
                    stD(s_ - 4)
                    g_, tb_ = its[s_ - 4]
                    if tb_ == TS - 1 and g_ + 2 < 32:
                        load_v(g_ + 2)
                if 0 <= s_ - 2 < N:
                    stM(s_ - 2)
            kb.dma("sp", fng_ap, final_g.partition_broadcast(128), "fng", (), [fng])
            for tb in range(TS):
                r0 = (b0 + tb) * 128
                kb.dma("sp", h2_ap, h1_d[r0:r0 + 128, :], "h2_ld", (), [h2])
                kb.tt("dve", accv[:, tb, :], accv[:, tb, :], g2_bc.ap, ALU.mult, [acc, g2_bc], [acc])
                kb.tt("dve", h2_ap, h2_ap, accv[:, tb, :], ALU.add, [h2, acc], [h2])
                kb.act(junk_ap, h2_ap, AF.Square, [h2], [junk, fst], accum_out=fst[:, 0:1])
                kb.ts("dve", fst[:, 1:2], fst[:, 0:1], 1.0 / D, EPS, ALU.mult, ALU.add, [fst], [fst])
                kb.act(fst[:, 1:2], fst[:, 1:2], AF.Sqrt, [fst], [fst])
                kb.recip(fst[:, 2:3], fst[:, 1:2], [fst], [fst])
                kb.stt("dve", h2_ap, h2_ap, fst[:, 2:3], fng_ap, ALU.mult, ALU.mult, [h2, fst, fng], [h2])
                kb.dma("sp", y[r0:r0 + 128, :], h2_ap, "y_out", [h2], ())
        fw.barrier()
        fw.emit(st)
    return nc


_PROG_CACHE = {}


def make_in_maps(cfg, inp):
    NB, T = cfg.NB, cfg.TPC
    f32 = lambda a: np.ascontiguousarray(np.asarray(a, dtype=np.float32))
    shared = dict(
        cctx=f32(inp["c_ctx"]), norm1_g=f32(inp["norm1_g"][0]), norm2_g=f32(inp["norm2_g"][0]),
        w_mod=f32(inp["w_mod"][0]), b_mod=f32(inp["b_mod"][0]), w_in=f32(inp["w_in"][0]),
        w_gate_up=f32(inp["w_gate_up"][0]), b_gate=f32(inp["b_gate"][0]), gla_norm_g=f32(inp["gla_norm_g"][0]),
        cmlp_ln_g=f32(inp["cmlp_ln_g"][0]), cmlp_ln_b=f32(inp["cmlp_ln_b"][0]), w_spatial=f32(inp["w_spatial"][0]),
        b_spatial=f32(inp["b_spatial"][0]), w_out=f32(inp["w_out"][0]), peer_wq=f32(inp["peer_wq"][0]),
        peer_sk=f32(inp["peer_sub_keys"][0]), peer_u=f32(inp["peer_u"][0]), peer_v=f32(inp["peer_v"][0]),
        final_g=f32(inp["final_norm_g"]),
    )
    x = np.asarray(inp["x"], dtype=np.float32)
    ctx = np.asarray(inp["ctx"], dtype=np.float32)
    c = np.asarray(inp["c"], dtype=np.float32)
    maps = []
    for core in range(8):
        b, seg = divmod(core, 4)
        xb = x[b]
        slots = [(s, 1.0) for s in range(seg)] + [(s, 0.0) for s in range(3, seg, -1)]
        flags = np.zeros((128, 8), np.float32)
        parts = []
        for j, (s, f) in enumerate(slots):
            blocks = xb[s * T:(s + 1) * T].reshape(NB, 128, D)
            if f == 0.0:
                blocks = blocks[::-1]
            parts.append(blocks.reshape(T, D))
            flags[:, j] = f
        flags[:, 3] = 1.0
        m = dict(shared)
        m.update(xm=f32(xb[seg * T:(seg + 1) * T]), xo=f32(np.concatenate(parts, 0)), ctxb=f32(ctx[b]),
                 cvec=f32(c[b]), flags=flags)
        maps.append(m)
    return maps


def kernel(**inp):
    seq = int(np.asarray(inp["x"]).shape[1])
    cfg = Cfg(seq, int(np.asarray(inp["ctx"]).shape[1]))
    if seq not in _PROG_CACHE:
        _PROG_CACHE[seq] = build_program(cfg)
    nc = _PROG_CACHE[seq]
    maps = make_in_maps(cfg, inp)
    res = run_bass_kernel_spmd(nc, maps, core_ids=list(range(8)))
    out = np.empty((2, seq, D), np.float32)
    for core in range(8):
        b, seg = divmod(core, 4)
        out[b, seg * cfg.TPC:(seg + 1) * cfg.TPC] = res.results[core]["y"]
    return out
```

```python
import numpy as np
import concourse.bass as bass
import concourse.mybir as mybir
from concourse.bass_utils import run_bass_kernel_spmd

F32 = mybir.dt.float32
BF16 = mybir.dt.bfloat16
ALU = mybir.AluOpType
AF = mybir.ActivationFunctionType
AX = mybir.AxisListType


class Tl:
    __slots__ = ("ap", "name", "w", "r")

    def __init__(self, ap, name=""):
        self.ap = ap
        self.name = name
        self.w = None
        self.r = {}

    def __getitem__(self, k):
        return self.ap[k]


class Op:
    __slots__ = ("idx", "eng", "fn", "deps", "dma_key", "val", "signal", "dma_waits")


class FW:
    ENGS = ("pe", "act", "dve", "pool", "sp")

    def __init__(self, nc):
        self.nc = nc
        self.ops = []
        self.dma_cnt = {}

    def op(self, eng, fn, reads=(), writes=(), dma_key=None):
        o = Op()
        o.idx = len(self.ops)
        o.eng = eng
        o.fn = fn
        o.dma_key = dma_key
        o.signal = False
        o.val = 0
        deps = set()
        for t in reads:
            if t.w is not None:
                deps.add(t.w)
        for t in writes:
            if t.w is not None:
                deps.add(t.w)
            deps.update(t.r.values())
        o.deps = deps
        o.dma_waits = {}
        for d in deps:
            od = self.ops[d]
            if od.dma_key is not None:
                o.dma_waits[od.dma_key] = self.dma_cnt[od.dma_key]
        if dma_key is not None:
            self.dma_cnt[dma_key] = self.dma_cnt.get(dma_key, 0) + 16
            o.val = self.dma_cnt[dma_key]
        rkey = ("dma", dma_key) if dma_key is not None else eng
        for t in reads:
            t.r[rkey] = o.idx
        for t in writes:
            t.w = o.idx
            t.r = {}
        self.ops.append(o)
        return o

    def emit(self, stack):
        nc = self.nc
        ops = self.ops
        for o in ops:
            for d in o.deps:
                if ops[d].dma_key is None:
                    ops[d].signal = True
        cnt = {e: 0 for e in self.ENGS}
        for o in ops:
            if o.dma_key is None and o.signal:
                cnt[o.eng] += 1
                o.val = cnt[o.eng]
        esem = {e: stack.enter_context(nc.semaphore("s_" + e)) for e in self.ENGS}
        dsem = {k: stack.enter_context(nc.semaphore("d_%d" % i))
                for i, k in enumerate(self.dma_cnt)}
        streams = {e: [o for o in ops if o.eng == e] for e in self.ENGS}

        def run(eng_name, e):
            waited = {}
            for o in streams[eng_name]:
                waits = {}
                for d in o.deps:
                    od = ops[d]
                    if od.dma_key is not None:
                        key = ("d", od.dma_key)
                        v = o.dma_waits[od.dma_key]
                    else:
                        if od.eng == eng_name and eng_name == "pe":
                            continue
                        key = ("e", od.eng)
                        v = od.val
                    if waits.get(key, 0) < v:
                        waits[key] = v
                for key, v in waits.items():
                    if waited.get(key, 0) >= v:
                        continue
                    s = dsem[key[1]] if key[0] == "d" else esem[key[1]]
                    e.wait_ge(s, v)
                    waited[key] = v
                ins = o.fn(e)
                if o.dma_key is not None:
                    ins.then_inc(dsem[o.dma_key], 16)
                elif o.signal:
                    ins.then_inc(esem[eng_name], 1)

        block = stack.enter_context(nc.Block())

        @block.tensor
        def _(e):
            run("pe", e)

        @block.scalar
        def _(e):
            run("act", e)

        @block.vector
        def _(e):
            run("dve", e)

        @block.gpsimd
        def _(e):
            run("pool", e)

        @block.sync
        def _(e):
            run("sp", e)

    def barrier(self):
        last = {}
        for o in self.ops:
            last[o.eng if o.dma_key is None else ("d", o.dma_key)] = o.idx
        deps = set(last.values())
        for e in self.ENGS:
            o = self.op(e, lambda en: en.nop())
            o.deps |= deps
            for d in deps:
                k = self.ops[d].dma_key
                if k is not None:
                    o.dma_waits[k] = self.dma_cnt[k]


D = 2048
KC = 16
NE = 16384
EPS = 1e-6
IN_W = 5152


class Cfg:
    def __init__(self, seq, ctx=256):
        self.SEQ = seq
        self.CTX = ctx
        self.TPC = seq // 4
        self.NB = self.TPC // 128
        self.TS = min(4, self.NB)


class Arena:
    def __init__(self, ap, words):
        self.ap = ap
        self.ptr = 0
        self.words = words

    def f32(self, n, name=""):
        off = self.ptr
        self.ptr += n
        assert self.ptr <= self.words, (name, self.ptr, self.words)
        return Tl(self.ap[:, off:off + n], name)

    def bf16(self, n, name=""):
        w = (n + 1) // 2
        off = self.ptr
        self.ptr += w
        assert self.ptr <= self.words, (name, self.ptr, self.words)
        return Tl(self.ap[:, off:off + w].bitcast(BF16), name)


def v3(ap, a):
    return ap.rearrange("p (a b) -> p a b", a=a)


class KB:
    def __init__(self, nc, fw):
        self.nc = nc
        self.fw = fw

    def dma(self, q, out_ap, in_ap, key, reads=(), writes=(), nc_ok=False):
        nc = self.nc

        def fn(e):
            if nc_ok:
                with nc.allow_non_contiguous_dma(reason="small strided load"):
                    return e.dma_start(out=out_ap, in_=in_ap)
            return e.dma_start(out=out_ap, in_=in_ap)
        return self.fw.op(q, fn, reads, writes, dma_key=key)

    def act(self, out, in_, func, reads, writes, **kw):
        return self.fw.op("act", lambda e: e.activation(out=out, in_=in_, func=func, **kw), reads, writes)

    def acts(self, specs, reads, writes):
        def fn(e):
            ins = None
            for (out, in_, func, kw) in specs:
                ins = e.activation(out=out, in_=in_, func=func, **kw)
            return ins
        return self.fw.op("act", fn, reads, writes)

    def tt(self, eng, out, in0, in1, op, reads, writes):
        return self.fw.op(eng, lambda e: e.tensor_tensor(out=out, in0=in0, in1=in1, op=op), reads, writes)

    def ts(self, eng, out, in0, s1, s2, op0, op1, reads, writes):
        if s2 is None:
            return self.fw.op(eng, lambda e: e.tensor_scalar(out=out, in0=in0, scalar1=s1, scalar2=None, op0=op0), reads, writes)
        return self.fw.op(eng, lambda e: e.tensor_scalar(out=out, in0=in0, scalar1=s1, scalar2=s2, op0=op0, op1=op1), reads, writes)

    def stt(self, eng, out, in0, scalar, in1, op0, op1, reads, writes):
        return self.fw.op(eng, lambda e: e.scalar_tensor_tensor(out=out, in0=in0, scalar=scalar, in1=in1, op0=op0, op1=op1), reads, writes)

    def stts(self, eng, specs, reads, writes):
        def fn(e):
            ins = None
            for (out, in0, scalar, in1, op0, op1) in specs:
                ins = e.scalar_tensor_tensor(out=out, in0=in0, scalar=scalar, in1=in1, op0=op0, op1=op1)
            return ins
        return self.fw.op(eng, fn, reads, writes)

    def copy(self, eng, out, in_, reads, writes):
        if eng == "act":
            return self.fw.op("act", lambda e: e.activation(out=out, in_=in_, func=AF.Copy), reads, writes)
        return self.fw.op(eng, lambda e: e.tensor_copy(out=out, in_=in_), reads, writes)

    def memset(self, eng, out, val, writes):
        return self.fw.op(eng, lambda e: e.memset(out, val), (), writes)

    def mm(self, specs, reads, writes):
        def fn(e):
            ins = None
            for (out, lhsT, rhs, start, stop) in specs:
                ins = e.matmul(out, lhsT=lhsT, rhs=rhs, start=start, stop=stop)
            return ins
        return self.fw.op("pe", fn, reads, writes)

    def tr(self, specs, reads, writes):
        def fn(e):
            ins = None
            for (out, in_, ident) in specs:
                ins = e.transpose(out=out, in_=in_, identity=ident)
            return ins
        return self.fw.op("pe", fn, reads, writes)

    def reduce(self, eng, out, in_, axis, op, reads, writes):
        nc = self.nc

        def fn(e):
            with nc.allow_low_precision(reason="bf16 gate sum feeds a bf16 matmul operand"):
                return e.tensor_reduce(out=out, in_=in_, axis=axis, op=op)
        return self.fw.op(eng, fn, reads, writes)

    def recip(self, out, in_, reads, writes):
        return self.fw.op("dve", lambda e: e.reciprocal(out=out, in_=in_), reads, writes)

    def rstd(self, out, ss, n, tmp, reads, writes):
        self.ts("dve", tmp, ss, 1.0 / n, EPS, ALU.mult, ALU.add, reads, writes)
        self.act(tmp, tmp, AF.Sqrt, writes, writes)
        self.recip(out, tmp, writes, writes)


AW = 53200


def build_program(cfg, debug=False):
    from contextlib import ExitStack
    NB, TPC, TS = cfg.NB, cfg.TPC, cfg.TS
    nc = bass.Bass("TRN2", target_bir_lowering=False)

    def din(name, shape, dt=F32):
        return nc.dram_tensor(name, list(shape), dt, kind="ExternalInput").ap()

    def dscr(name, shape, dt):
        return nc.dram_tensor(name, list(shape), dt, kind="ExternalOutput" if debug else "Internal").ap()

    xm = din("xm", [TPC, D])
    xo = din("xo", [3 * TPC, D])
    ctxb = din("ctxb", [cfg.CTX, D])
    cvec = din("cvec", [D])
    cctx = din("cctx", [D])
    flags = din("flags", [128, 8])
    norm1_g = din("norm1_g", [D])
    norm2_g = din("norm2_g", [D])
    w_mod = din("w_mod", [D, 6 * D])
    b_mod = din("b_mod", [6 * D])
    w_in = din("w_in", [D, IN_W])
    w_gate_up = din("w_gate_up", [2, 16, 512])
    b_gate = din("b_gate", [2, 512])
    gla_norm_g = din("gla_norm_g", [1024])
    cmlp_ln_g = din("cmlp_ln_g", [1024])
    cmlp_ln_b = din("cmlp_ln_b", [1024])
    w_spatial = din("w_spatial", [8, 128, 128])
    b_spatial = din("b_spatial", [8, 128])
    w_out = din("w_out", [D, D])
    peer_wq = din("peer_wq", [D, D])
    peer_sk = din("peer_sk", [2, 8, 128, 128])
    peer_u = din("peer_u", [NE, D])
    peer_v = din("peer_v", [NE, D])
    final_g = din("final_g", [D])
    y = nc.dram_tensor("y", [TPC, D], F32, kind="ExternalOutput").ap()

    of_d = dscr("of_d", [TPC, 1024], F32)
    mix_d = dscr("mix_d", [TPC, D], BF16)
    h1_d = dscr("h1_d", [TPC, D], F32)
    xn2T_d = dscr("xn2T_d", [NB, 128, D], BF16)
    st_nega = dscr("st_nega", [NB, 128, 1024], F32)
    st_s2 = dscr("st_s2", [NB, 128, 1024], F32)
    st_ea = dscr("st_ea", [NB, 128, 1024], BF16)
    st_eb = dscr("st_eb", [NB, 128, 1024], BF16)
    UT_d = dscr("UT_d", [32, 128, 8192], BF16)
    V_d = dscr("V_d", [NE, D], BF16)

    st = ExitStack()
    with st:
        fw = FW(nc)
        kb = KB(nc, fw)
        arena_t = st.enter_context(nc.sbuf_tensor("arena", [128, AW], F32))
        psum_t = st.enter_context(nc.psum_tensor("psum", [128, 4096], F32))
        ar = Arena(arena_t, AW)
        PB = [Tl(psum_t[:, b * 512:(b + 1) * 512], "B%d" % b) for b in range(8)]

        def pf(b0, nb=1):
            return psum_t[:, b0 * 512:(b0 + nb) * 512]

        def pbf(b0, nb=1):
            return psum_t[:, b0 * 512:(b0 + nb) * 512].bitcast(BF16)

        ident_f = ar.f32(128, "ident_f")
        A_le = ar.f32(128, "A_le")
        A_ge = ar.f32(128, "A_ge")
        A_gt = ar.f32(128, "A_gt")
        A_lt = ar.f32(128, "A_lt")
        ident_b = ar.bf16(128, "ident_b")
        cols = ar.f32(16 * 8, "cols")
        fl = ar.f32(8, "fl")
        fl2 = ar.f32(24, "fl2")
        g2_bc = ar.f32(D, "g2_bc")

        def tri(tl, cmp, sign):
            kb.memset("pool", tl.ap, 0.0, [tl])
            fw.op("pool", lambda e: e.affine_select(out=tl.ap, in_=tl.ap, pattern=[[-sign, 128]], compare_op=cmp,
                                                    fill=1.0, base=0, channel_multiplier=sign), [tl], [tl])
        tri(ident_f, ALU.not_equal, 1)
        tri(A_le, ALU.is_gt, 1)
        tri(A_ge, ALU.is_gt, -1)
        kb.tt("dve", A_gt.ap, A_ge.ap, ident_f.ap, ALU.subtract, [A_ge, ident_f], [A_gt])
        kb.tt("dve", A_lt.ap, A_le.ap, ident_f.ap, ALU.subtract, [A_le, ident_f], [A_lt])
        kb.copy("dve", ident_b.ap, ident_f.ap, [ident_f], [ident_b])
        kb.dma("sp", fl.ap, flags[:, :], "m_p0_0", (), [fl])
        kb.ts("dve", fl2[:, 0:8], fl.ap, -1.0 / 16.0, None, ALU.mult, None, [fl], [fl2])
        kb.ts("dve", fl2[:, 16:24], fl.ap, -1.0, 1.0, ALU.mult, ALU.add, [fl], [fl2])
        kb.ts("dve", fl2[:, 8:16], fl2[:, 16:24], -1.0 / 16.0, None, ALU.mult, None, [fl2], [fl2])
        colv = v3(cols.ap, 8)
        C_G1, C_SH1, C_CG1, C_CSH1, C_G2, C_SH2, C_N1, C_N2 = range(8)
        const_end_p6 = ar.ptr
        g1_bc = ar.f32(D, "g1_bc")
        const_end = ar.ptr

        M = ar.f32(6 * D, "M")
        Mc = ar.f32(2 * D, "Mc")
        rep = ar.f32(2 * KC * 128, "rep")
        ccol = ar.f32(32, "ccol")
        wb = [ar.f32(KC * 512, "wb0"), ar.f32(KC * 512, "wb1")]
        repv = rep.ap.rearrange("p (t k m) -> p t k m", t=2, k=KC)
        kb.dma("sp", ccol[:, 0:16], cvec.rearrange("(k p) -> p k", p=128), "m_p0_1", (), [ccol], nc_ok=True)
        kb.dma("sp", ccol[:, 16:32], cctx.rearrange("(k p) -> p k", p=128), "m_p0_2", (), [ccol], nc_ok=True)
        kb.dma("sp", colv[:, C_N1, :], norm1_g.rearrange("(k p) -> p k", p=128), "m_p0_3", (), [cols], nc_ok=True)
        kb.dma("sp", colv[:, C_N2, :], norm2_g.rearrange("(k p) -> p k", p=128), "m_p0_4", (), [cols], nc_ok=True)
        kb.dma("sp", M.ap, b_mod.partition_broadcast(128), "m_p0_5", (), [M])
        kb.dma("sp", Mc.ap, b_mod[0:2 * D].partition_broadcast(128), "m_p0_6", (), [Mc])
        kb.act(ccol.ap, ccol.ap, AF.Silu, [ccol], [ccol])
        kb.copy("dve", repv[:, 0], ccol[:, 0:16].unsqueeze(2).to_broadcast([128, KC, 128]), [ccol], [rep])
        kb.copy("dve", repv[:, 1], ccol[:, 16:32].unsqueeze(2).to_broadcast([128, KC, 128]), [ccol], [rep])
        wmv = w_mod.rearrange("(k p) n -> p k n", p=128)
        for cb in range(24):
            w = wb[cb % 2]
            kb.dma("sp", v3(w.ap, KC), wmv[:, :, cb * 512:(cb + 1) * 512], "wb%d" % (cb % 2), (), [w])
            bk = cb % 2
            kb.mm([(pf(bk), repv[:, 0, k, :], v3(w.ap, KC)[:, k, :], k == 0, k == KC - 1) for k in range(KC)],
                  [rep, w], [PB[bk]])
            kb.tt("dve", M[:, cb * 512:(cb + 1) * 512], pf(bk), M[:, cb * 512:(cb + 1) * 512], ALU.add, [PB[bk], M], [M])
            if cb < 8:
                bk2 = 2 + cb % 2
                kb.mm([(pf(bk2), repv[:, 1, k, :], v3(w.ap, KC)[:, k, :], k == 0, k == KC - 1) for k in range(KC)],
                      [rep, w], [PB[bk2]])
                kb.tt("dve", Mc[:, cb * 512:(cb + 1) * 512], pf(bk2), Mc[:, cb * 512:(cb + 1) * 512], ALU.add, [PB[bk2], Mc], [Mc])
        tmpc = ar.f32(6 * 16, "tmpc")
        tmpcv = v3(tmpc.ap, 6)
        srcs = [(M, 0), (M, 1), (M, 3), (M, 4), (Mc, 0), (Mc, 1)]
        for i, (src, ch) in enumerate(srcs):
            kb.tr([(pf(4, 4)[:, k * 128:(k + 1) * 128], src[:, ch * D + k * 128: ch * D + (k + 1) * 128], ident_f.ap)
                   for k in range(KC)], [src, ident_f], PB[4:8])
            kb.copy("dve", tmpcv[:, i, :], v3(pf(4, 4), KC)[:, :, 0], PB[4:8], [tmpc])
        kb.copy("act", g1_bc.ap, M[:, 2 * D:3 * D], [M], [g1_bc])
        kb.copy("act", g2_bc.ap, M[:, 5 * D:6 * D], [M], [g2_bc])
        for (dst, sc_i, n_i) in ((C_G1, 1, C_N1), (C_G2, 3, C_N2), (C_CG1, 5, C_N1)):
            kb.stt("dve", colv[:, dst, :], tmpcv[:, sc_i, :], 1.0, colv[:, n_i, :], ALU.add, ALU.mult, [tmpc, cols], [cols])
        for (dst, sh_i) in ((C_SH1, 0), (C_SH2, 2), (C_CSH1, 4)):
            kb.copy("dve", colv[:, dst, :], tmpcv[:, sh_i, :], [tmpc], [cols])
        fw.barrier()
        ar.ptr = const_end

        class FE:
            def __init__(self, tb0):
                self.xt = [ar.f32(D, "xt0"), ar.f32(D, "xt1")]
                self.xs = ar.bf16(D, "xs")
                self.xnT = ar.bf16(D, "xnT")
                self.stt_ = ar.f32(4, "fe_st")
                self.n = 0
                self.tb0 = tb0

            def load(self, rows_ap, key="xt"):
                t = self.xt[self.n % 2]
                kb.dma("sp", t.ap, rows_ap, "%s%d" % (key, self.n % 2), (), [t])
                return t

            def norm_T(self, t, gi, si):
                s = self.stt_
                tb0 = self.tb0
                kb.act(self.xs.ap, t.ap, AF.Square, [t], [self.xs, s], accum_out=s[:, 0:1])
                kb.ts("dve", s[:, 1:2], s[:, 0:1], 1.0 / D, EPS, ALU.mult, ALU.add, [s], [s])
                kb.act(s[:, 1:2], s[:, 1:2], AF.Sqrt, [s], [s])
                kb.recip(s[:, 2:3], s[:, 1:2], [s], [s])
                kb.ts("dve", self.xs.ap, t.ap, s[:, 2:3], None, ALU.mult, None, [t, s], [self.xs])
                pv = v3(pbf(tb0, 2), KC)
                kb.tr([(pv[:, k, :], self.xs[:, k * 128:(k + 1) * 128], ident_b.ap) for k in range(KC)],
                      [self.xs, ident_b], PB[tb0:tb0 + 2])
                xv = v3(self.xnT.ap, KC)
                kb.acts([(xv[:, k, :], pv[:, k, :], AF.Identity,
                          dict(scale=colv[:, gi, k:k + 1], bias=colv[:, si, k:k + 1])) for k in range(KC)],
                        PB[tb0:tb0 + 2] + [cols], [self.xnT])
                self.n += 1
                return xv

        GW = 3104
        wg = ar.bf16(KC * GW, "wg")
        wgv = v3(wg.ap, KC)
        kb.dma("pool", wgv, w_in.rearrange("(k p) n -> p k n", p=128)[:, :, 0:GW], "wg", (), [wg])
        for et in range(64):
            kb.dma("pool", V_d[et * 256:(et + 1) * 256, :], peer_v[et * 256:(et + 1) * 256, :], "vcast", (), ())
        Wga = [ar.f32(512, "Wga0"), ar.f32(512, "Wga1")]
        for d in range(2):
            kb.memset("pool", Wga[d].ap, 0.0, [Wga[d]])
            kb.dma("sp", Wga[d][16 * d:16 * d + 16, :], w_gate_up[d], "misc", (), [Wga[d]])
            kb.dma("sp", Wga[d][32:33, :], b_gate[d:d + 1, :], "misc", (), [Wga[d]])
        ngbc = ar.f32(1024, "ngbc")
        kb.dma("sp", ngbc.ap, gla_norm_g.partition_broadcast(128), "misc", (), [ngbc])
        S = [ar.f32(1024, "S_f"), ar.f32(1024, "S_b")]
        Sbf = ar.bf16(1024, "Sbf")
        for d in range(2):
            kb.memset("pool", S[d].ap, 0.0, [S[d]])
        fe = FE(6)
        qT = ar.f32(512, "qT")
        kT = ar.f32(512, "kT")
        k_sb = ar.f32(512, "k_sb")
        v_sb = ar.bf16(1024, "v_sb")
        lrT = ar.f32(128, "lrT")
        e1 = ar.f32(512, "e1")
        la = ar.f32(512, "la")
        ecT = ar.f32(512, "ecT")
        encT = ar.f32(512, "encT")
        ercum = ar.f32(512, "ercum")
        kdec = ar.bf16(512, "kdec")
        qd = ar.bf16(512, "qd")
        ki = ar.bf16(512, "ki")
        scm = ar.bf16(512, "scm")
        o_sb = ar.f32(1024, "o_sb")
        of_sb = ar.f32(1024, "of_sb")
        silug = ar.f32(1024, "silug")
        ybf = ar.bf16(1024, "ybf")
        gst = ar.f32(16, "gst")
        kb.memset("pool", lrT.ap, 1.0, [lrT])
        ONE_NF16 = fl2[:, 3:4]
        ONE_F = fl[:, 3:4]

        TRI = {0: (A_le, A_gt, 127), 1: (A_ge, A_lt, 0)}

        def inproj_state(xv):
            kb.mm([(pf(2), xv[:, k, :], wgv[:, k, 512:1024], k == 0, k == KC - 1) for k in range(KC)],
                  [fe.xnT, wg], [PB[2]])
            kb.copy("act", k_sb.ap, pf(2), [PB[2]], [k_sb])
            for hf in range(2):
                kb.mm([(pf(3 + hf), xv[:, k, :], wgv[:, k, 1024 + hf * 512:1536 + hf * 512], k == 0, k == KC - 1)
                       for k in range(KC)], [fe.xnT, wg], [PB[3 + hf]])
            kb.copy("act", v_sb.ap, pf(3, 2), PB[3:5], [v_sb])
            kb.mm([(pf(5)[0:32, 0:128], wgv[:, k, 3072:3104], xv[:, k, :], k == 0, k == KC - 1) for k in range(KC)],
                  [fe.xnT, wg], [PB[5]])
            kb.copy("dve", lrT[0:32, :], pf(5)[0:32, 0:128], [PB[5]], [lrT])

        def decay_parts(d, nf16_ap):
            cumm, rcm, _ = TRI[d]
            kb.mm([(pf(2), lrT[0:33, :], Wga[d][0:33, :], True, True)], [lrT, Wga[d]], [PB[2]])
            kb.act(e1.ap, pf(2), AF.Exp, [PB[2]], [e1], scale=-1.0)
            kb.act(e1.ap, e1.ap, AF.Ln, [e1], [e1], bias=1.0)
            kb.ts("dve", la.ap, e1.ap, nf16_ap, None, ALU.mult, None, [e1, fl2], [la])
            kb.mm([(pf(5), rcm.ap, la.ap, True, True)], [rcm, la], [PB[5]])
            kb.mm([(pf(1)[:, h * 128:(h + 1) * 128], la[:, h * 128:(h + 1) * 128], cumm.ap, True, True)
                   for h in range(4)], [la, cumm], [PB[1]])
            kb.act(ecT.ap, pf(1), AF.Exp, [PB[1]], [ecT])
            kb.act(ercum.ap, pf(5), AF.Exp, [PB[5]], [ercum])

        def state_update(d, f_ap):
            col = TRI[d][2]
            kb.stt("dve", kdec.ap, ercum.ap, f_ap, k_sb.ap, ALU.mult, ALU.mult, [ercum, k_sb, fl, fl2], [kdec])
            kb.mm([(pf(3, 2)[:, h * 256:(h + 1) * 256], kdec[:, h * 128:(h + 1) * 128],
                    v_sb[:, h * 256:(h + 1) * 256], True, True) for h in range(4)], [kdec, v_sb], PB[3:5])
            kb.stts("dve", [(S[d][:, h * 256:(h + 1) * 256], S[d][:, h * 256:(h + 1) * 256],
                             ecT[:, h * 128 + col:h * 128 + col + 1], pf(3, 2)[:, h * 256:(h + 1) * 256],
                             ALU.mult, ALU.add) for h in range(4)], [S[d], ecT] + PB[3:5], [S[d]])

        def state_block(rows_ap, gi, si, fcol):
            t = fe.load(rows_ap)
            xv = fe.norm_T(t, gi, si)
            inproj_state(xv)
            decay_parts(0, fl2[:, fcol:fcol + 1])
            state_update(0, fl[:, fcol:fcol + 1])
            decay_parts(1, fl2[:, 8 + fcol:9 + fcol])
            state_update(1, fl2[:, 16 + fcol:17 + fcol])

        def main_front(blk):
            t = fe.load(xm[blk * 128:(blk + 1) * 128, :])
            return fe.norm_T(t, C_G1, C_SH1)

        def main_block(d, blk, xv, nxt):
            cumm, rcm, col = TRI[d]
            if d == 1:
                for hf in range(2):
                    kb.mm([(pf(6 + hf), xv[:, k, :], wgv[:, k, 2048 + hf * 512:2560 + hf * 512], k == 0, k == KC - 1)
                           for k in range(KC)], [fe.xnT, wg], [PB[6 + hf]])
                kb.act(silug.ap, pf(6, 2), AF.Silu, PB[6:8], [silug])
                kb.dma("sp", of_sb.ap, of_d[blk * 128:(blk + 1) * 128, :], "of_sb", [ofd_tl[blk]], [of_sb])
            kb.mm([(pf(0)[:, h * 128:(h + 1) * 128], wgv[:, k, h * 128:(h + 1) * 128], xv[:, k, :], k == 0, k == KC - 1)
                   for h in range(4) for k in range(KC)], [fe.xnT, wg], [PB[0]])
            kb.act(qT.ap, pf(0), AF.Identity, [PB[0]], [qT], scale=128.0 ** -0.5)
            kb.mm([(pf(1)[:, h * 128:(h + 1) * 128], wgv[:, k, 512 + h * 128:512 + (h + 1) * 128], xv[:, k, :], k == 0, k == KC - 1)
                   for h in range(4) for k in range(KC)], [fe.xnT, wg], [PB[1]])
            kb.copy("dve", kT.ap, pf(1), [PB[1]], [kT])
            inproj_state(xv)
            nxt_xv = main_front(nxt) if nxt is not None else None
            decay_parts(d, ONE_NF16)
            kb.act(encT.ap, pf(1), AF.Exp, [PB[1]], [encT], scale=-1.0)
            kb.tt("dve", qd.ap, qT.ap, ecT.ap, ALU.mult, [qT, ecT], [qd])
            kb.tt("dve", ki.ap, kT.ap, encT.ap, ALU.mult, [kT, encT], [ki])
            kb.mm([(pf(0)[:, h * 128:(h + 1) * 128], ki[:, h * 128:(h + 1) * 128], qd[:, h * 128:(h + 1) * 128], True, True)
                   for h in range(4)], [ki, qd], [PB[0]])
            kb.tt("dve", v3(scm.ap, 4), v3(pf(0), 4), cumm.ap.unsqueeze(1).to_broadcast([128, 4, 128]), ALU.mult,
                  [PB[0], cumm], [scm])
            sp = []
            for h in range(4):
                o_ap = pf(6, 2)[:, h * 256:(h + 1) * 256]
                sp.append((o_ap, scm[:, h * 128:(h + 1) * 128], v_sb[:, h * 256:(h + 1) * 256], True, False))
                sp.append((o_ap, qd[:, h * 128:(h + 1) * 128], Sbf[:, h * 256:(h + 1) * 256], False, True))
            kb.mm(sp, [scm, v_sb, qd, Sbf], PB[6:8])
            if d == 0:
                kb.copy("act", o_sb.ap, pf(6, 2), PB[6:8], [o_sb])
                kb.dma("sp", of_d[blk * 128:(blk + 1) * 128, :], o_sb.ap, "of_st", [o_sb], [ofd_tl[blk]])
            else:
                kb.tt("dve", o_sb.ap, pf(6, 2), of_sb.ap, ALU.add, PB[6:8] + [of_sb], [o_sb])
                kb.acts([(of_sb[:, h * 256:(h + 1) * 256], o_sb[:, h * 256:(h + 1) * 256], AF.Square,
                          dict(accum_out=gst[:, h:h + 1])) for h in range(4)], [o_sb], [of_sb, gst])
                kb.ts("dve", gst[:, 4:8], gst[:, 0:4], 1.0 / 256.0, EPS, ALU.mult, ALU.add, [gst], [gst])
                kb.act(gst[:, 4:8], gst[:, 4:8], AF.Sqrt, [gst], [gst])
                kb.recip(gst[:, 8:12], gst[:, 4:8], [gst], [gst])
                kb.stts("dve", [(o_sb[:, h * 256:(h + 1) * 256], o_sb[:, h * 256:(h + 1) * 256], gst[:, 8 + h:9 + h],
                                 ngbc[:, h * 256:(h + 1) * 256], ALU.mult, ALU.mult) for h in range(4)],
                        [o_sb, gst, ngbc], [o_sb])
                kb.tt("dve", ybf.ap, o_sb.ap, silug.ap, ALU.mult, [o_sb, silug], [ybf])
                kb.dma("sp", mix_d[blk * 128:(blk + 1) * 128, 0:1024], ybf.ap, "y_st", [ybf], ())
            state_update(d, ONE_F)
            kb.copy("pool", Sbf.ap, S[d].ap, [S[d]], [Sbf])
            return nxt_xv

        ofd_tl = [Tl(None, 'ofd') for _ in range(NB)]
        nctx = cfg.CTX // 128
        for b in range(nctx):
            state_block(ctxb[b * 128:(b + 1) * 128, :], C_CG1, C_CSH1, 3)
        for b in range(nctx - 1, -1, -1):
            state_block(ctxb[b * 128:(b + 1) * 128, :], C_CG1, C_CSH1, 4)
        for j in range(3):
            for b in range(NB):
                r0 = (j * NB + b) * 128
                state_block(xo[r0:r0 + 128, :], C_G1, C_SH1, j)
        kb.copy("pool", Sbf.ap, S[0].ap, [S[0]], [Sbf])
        xv_ = main_front(0)
        for b in range(NB):
            xv_ = main_block(0, b, xv_, b + 1 if b + 1 < NB else None)
        kb.copy("pool", Sbf.ap, S[1].ap, [S[1]], [Sbf])
        xv_ = main_front(NB - 1)
        for b in range(NB - 1, -1, -1):
            xv_ = main_block(1, b, xv_, b - 1 if b - 1 >= 0 else None)
        fw.barrier()
        ar.ptr = const_end

        wc = ar.bf16(KC * 2048, "wc")
        wcv = v3(wc.ap, KC)
        kb.dma("pool", wcv, w_in.rearrange("(k p) n -> p k n", p=128)[:, :, GW:IN_W], "wc", (), [wc])
        wsT = ar.bf16(1024, "wsT")
        wstg = ar.f32(1024, "wstg")
        bs_col = ar.f32(8, "bs_col")
        lng = ar.f32(1024, "lng")
        lnb = ar.f32(1024, "lnb")
        kb.dma("sp", v3(wstg.ap, 8), w_spatial.rearrange("g p q -> p g q"), "misc", (), [wstg])
        kb.dma("sp", bs_col.ap, b_spatial.rearrange("g p -> p g"), "misc", (), [bs_col], nc_ok=True)
        kb.dma("sp", lng.ap, cmlp_ln_g.partition_broadcast(128), "misc", (), [lng])
        kb.dma("sp", lnb.ap, cmlp_ln_b.partition_broadcast(128), "misc", (), [lnb])
        kb.tr([(pf(0, 2)[:, g * 128:(g + 1) * 128], wstg[:, g * 128:(g + 1) * 128], ident_f.ap) for g in range(8)],
              [wstg, ident_f], PB[0:2])
        kb.copy("dve", wsT.ap, pf(0, 2), PB[0:2], [wsT])
        fe = FE(6)
        gu = ar.f32(1024, "gu")
        gv = ar.f32(1024, "gv")
        vn = ar.bf16(1024, "vn")
        cm = ar.bf16(1024, "cm")
        cst = ar.f32(8, "cst")
        def p4_front(blk):
            t = fe.load(xm[blk * 128:(blk + 1) * 128, :])
            return fe.norm_T(t, C_G1, C_SH1)
        xv = p4_front(0)
        for blk in range(NB):
            for q4 in range(4):
                kb.mm([(pf(q4), xv[:, k, :], wcv[:, k, q4 * 512:(q4 + 1) * 512], k == 0, k == KC - 1) for k in range(KC)],
                      [fe.xnT, wc], [PB[q4]])
            if blk + 1 < NB:
                xv = p4_front(blk + 1)
            kb.act(gu.ap, pf(0, 2), AF.Gelu, PB[0:2], [gu])
            kb.act(gv.ap, pf(2, 2), AF.Gelu, PB[2:4], [gv, cst], accum_out=cst[:, 0:1])
            kb.ts("dve", cst[:, 1:2], cst[:, 0:1], -1.0 / 1024.0, None, ALU.mult, None, [cst], [cst])
            kb.act(vn.ap, gv.ap, AF.Square, [gv, cst], [vn, cst], bias=cst[:, 1:2], accum_out=cst[:, 2:3])
            kb.ts("dve", cst[:, 3:4], cst[:, 2:3], 1.0 / 1024.0, EPS, ALU.mult, ALU.add, [cst], [cst])
            kb.act(cst[:, 3:4], cst[:, 3:4], AF.Sqrt, [cst], [cst])
            kb.recip(cst[:, 4:5], cst[:, 3:4], [cst], [cst])
            kb.ts("dve", gv.ap, gv.ap, cst[:, 1:2], cst[:, 4:5], ALU.add, ALU.mult, [gv, cst], [gv])
            kb.tt("dve", gv.ap, gv.ap, lng.ap, ALU.mult, [gv, lng], [gv])
            kb.tt("dve", vn.ap, gv.ap, lnb.ap, ALU.add, [gv, lnb], [vn])
            kb.mm([(pf(4, 2)[:, g * 128:(g + 1) * 128], wsT[:, g * 128:(g + 1) * 128], vn[:, g * 128:(g + 1) * 128], True, True)
                   for g in range(8)], [wsT, vn], PB[4:6])
            kb.stts("dve", [(cm[:, g * 128:(g + 1) * 128], pf(4, 2)[:, g * 128:(g + 1) * 128], bs_col[:, g:g + 1],
                             gu[:, g * 128:(g + 1) * 128], ALU.add, ALU.mult) for g in range(8)],
                    PB[4:6] + [bs_col, gu], [cm])
            kb.dma("sp", mix_d[blk * 128:(blk + 1) * 128, 1024:2048], cm.ap, "cm_st", [cm], ())
        fw.barrier()
        ar.ptr = const_end

        wo = ar.bf16(KC * D, "wo")
        wov = v3(wo.ap, KC)
        wq = ar.bf16(KC * D, "wq")
        wqv = v3(wq.ap, KC)
        skT = ar.bf16(D, "skT")
        skTv = v3(skT.ap, 16)
        kb.dma("pool", wqv, peer_wq.rearrange("(k p) n -> p k n", p=128), "wq", (), [wq])
        h1 = ar.f32(D, "h1")
        sc = ar.f32(D, "sc")
        tmp = ar.f32(D, "tmp")
        stg = [h1, sc]
        for k in range(KC):
            sg = stg[k % 2]
            kb.dma("sp", sg.ap, w_out[k * 128:(k + 1) * 128, :], "wo_st%d" % (k % 2), (), [sg])
            kb.tt("dve", wov[:, k, :], sg.ap, g1_bc.ap, ALU.mult, [sg, g1_bc], [wo])
        kb.dma("sp", v3(tmp.ap, 16), peer_sk.rearrange("t h k d -> k (t h) d"), "misc", (), [tmp])
        specs = []
        for half in range(2):
            for hh in range(8):
                c = hh * 2 + half
                specs.append((pf(0, 4)[:, c * 128:(c + 1) * 128], tmp[:, (half * 8 + hh) * 128:(half * 8 + hh + 1) * 128], ident_f.ap))
        kb.tr(specs, [tmp, ident_f], PB[0:4])
        kb.copy("dve", skT.ap, pf(0, 4), PB[0:4], [skT])
        mixT = ar.bf16(D, "mixT")
        mixTv = v3(mixT.ap, KC)
        xt5 = sc
        xn2T = ar.bf16(D, "xn2T")
        xn2v = v3(xn2T.ap, KC)
        qTb = ar.bf16(D, "qTb")
        qTv = v3(qTb.ap, 16)
        v16 = ar.f32(256, "v16")
        v16v = v3(v16.ap, 16)
        cand = ar.f32(D, "cand")
        mix_sb = cand
        mix_ap = cand.ap[:, 0:1024].bitcast(BF16)
        xs2 = mixT
        best = ar.f32(128, "best")
        bestv = v3(best.ap, 8)
        nega = tmp
        nega_ap = tmp.ap[:, 1024:2048]
        eat = tmp
        eat_ap = tmp.ap[:, 0:1024]
        pst = ar.f32(64, "pst")
        exw = ar.f32(128, "exw")
        scv = sc.ap.rearrange("p (h t k) -> p h t k", h=8, t=2)
        v16q = v16.ap.rearrange("p (h t r) -> p h t r", h=8, t=2)
        for blk in range(NB):
            r0 = blk * 128
            kb.dma("sp", mix_ap, mix_d[r0:r0 + 128, :], "mix_ld", (), [mix_sb])
            kb.dma("sp", xt5.ap, xm[r0:r0 + 128, :], "xt5", (), [xt5])
            pv = v3(pbf(0, 2), KC)
            kb.tr([(pv[:, k, :], mix_ap[:, k * 128:(k + 1) * 128], ident_b.ap) for k in range(KC)], [mix_sb, ident_b], PB[0:2])
            kb.copy("act", mixT.ap, pbf(0, 2), PB[0:2], [mixT])
            for q4 in range(4):
                kb.mm([(pf(2 + q4), mixTv[:, k, :], wov[:, k, q4 * 512:(q4 + 1) * 512], k == 0, k == KC - 1) for k in range(KC)],
                      [mixT, wo], [PB[2 + q4]])
            kb.tt("dve", h1.ap, pf(2, 4), xt5.ap, ALU.add, PB[2:6] + [xt5], [h1])
            kb.dma("sp", h1_d[r0:r0 + 128, :], h1.ap, "h1_st", [h1], ())
            kb.act(xs2.ap, h1.ap, AF.Square, [h1], [xs2, pst], accum_out=pst[:, 0:1])
            kb.ts("dve", pst[:, 1:2], pst[:, 0:1], 1.0 / D, EPS, ALU.mult, ALU.add, [pst], [pst])
            kb.act(pst[:, 1:2], pst[:, 1:2], AF.Sqrt, [pst], [pst])
            kb.recip(pst[:, 2:3], pst[:, 1:2], [pst], [pst])
            kb.ts("dve", xs2.ap, h1.ap, pst[:, 2:3], None, ALU.mult, None, [h1, pst], [xs2])
            pv2 = v3(pbf(6, 2), KC)
            kb.tr([(pv2[:, k, :], xs2[:, k * 128:(k + 1) * 128], ident_b.ap) for k in range(KC)], [xs2, ident_b], PB[6:8])
            kb.acts([(xn2v[:, k, :], pv2[:, k, :], AF.Identity,
                      dict(scale=colv[:, C_G2, k:k + 1], bias=colv[:, C_SH2, k:k + 1])) for k in range(KC)],
                    PB[6:8] + [cols], [xn2T])
            kb.dma("sp", xn2T_d[blk], xn2T.ap, "xn2_st", [xn2T], ())
            kb.mm([(pf(2, 4)[:, c * 128:(c + 1) * 128], wqv[:, k, c * 128:(c + 1) * 128], xn2v[:, k, :], k == 0, k == KC - 1)
                   for c in range(16) for k in range(KC)], [wq, xn2T], PB[2:6])
            kb.copy("act", qTb.ap, pf(2, 4), PB[2:6], [qTb])
            kb.mm([(pf(0, 2)[:, c * 128:(c + 1) * 128], qTv[:, c, :], skTv[:, c, :], True, True) for c in range(8)],
                  [qTb, skT], PB[0:2])
            kb.mm([(pf(6, 2)[:, (c - 8) * 128:(c - 7) * 128], qTv[:, c, :], skTv[:, c, :], True, True) for c in range(8, 16)],
                  [qTb, skT], PB[6:8])
            kb.copy("act", sc[:, 0:1024], pf(0, 2), PB[0:2], [sc])
            kb.copy("act", sc[:, 1024:2048], pf(6, 2), PB[6:8], [sc])
            fw.op("dve", lambda e: [e.max(out=v16v[:, c, 0:8], in_=sc[:, c * 128:(c + 1) * 128]) for c in range(16)][-1], [sc], [v16])
            fw.op("dve", lambda e: [e.match_replace(out=tmp[:, c * 128:(c + 1) * 128], in_to_replace=v16v[:, c, 0:8],
                                                    in_values=sc[:, c * 128:(c + 1) * 128], imm_value=-1e30) for c in range(16)][-1],
                  [sc, v16], [tmp])
            fw.op("dve", lambda e: [e.max(out=v16v[:, c, 8:16], in_=tmp[:, c * 128:(c + 1) * 128]) for c in range(16)][-1], [tmp], [v16])
            candv = cand.ap.rearrange("p (h r c) -> p h r c", h=8, r=16)
            kb.tt("dve", candv, v16q[:, :, 0, :].unsqueeze(3).to_broadcast([128, 8, 16, 16]),
                  v16q[:, :, 1, :].unsqueeze(2).to_broadcast([128, 8, 16, 16]), ALU.add, [v16], [cand])
            fw.op("dve", lambda e: [e.max(out=bestv[:, hh, 0:8], in_=cand[:, hh * 256:(hh + 1) * 256]) for hh in range(8)][-1], [cand], [best])
            fw.op("dve", lambda e: [e.match_replace(out=tmp[:, hh * 256:(hh + 1) * 256], in_to_replace=bestv[:, hh, 0:8],
                                                    in_values=cand[:, hh * 256:(hh + 1) * 256], imm_value=-1e30) for hh in range(8)][-1],
                  [cand, best], [tmp])
            fw.op("dve", lambda e: [e.max(out=bestv[:, hh, 8:16], in_=tmp[:, hh * 256:(hh + 1) * 256]) for hh in range(8)][-1], [tmp], [best])
            kb.tt("dve", v3(exw.ap, 8), bestv, bestv[:, :, 0:1].to_broadcast([128, 8, 16]), ALU.subtract, [best], [exw])
            kb.act(exw.ap, exw.ap, AF.Exp, [exw], [exw])
            kb.reduce("dve", pst[:, 8:16], v3(exw.ap, 8), AX.X, ALU.add, [exw], [pst])
            kb.recip(pst[:, 16:24], pst[:, 8:16], [pst], [pst])
            negav = v3(nega_ap, 8)
            kb.tt("dve", negav, bestv[:, :, 15:16].to_broadcast([128, 8, 128]), scv[:, :, 0, :], ALU.subtract, [best, sc], [nega])
            kb.ts("dve", nega_ap, nega_ap, -1e-5, None, ALU.add, None, [nega], [nega])
            kb.dma("sp", st_nega[blk], nega_ap, "st_st", [nega], ())
            kb.dma("sp", v3(st_s2[blk], 8), scv[:, :, 1, :], "st_st", [sc], ())
            eatv = v3(eat_ap, 8)
            kb.tt("dve", eatv, scv[:, :, 0, :], v16q[:, :, 0, 0:1].to_broadcast([128, 8, 128]), ALU.subtract, [sc, v16], [eat])
            kb.act(eat_ap, eat_ap, AF.Exp, [eat], [eat])
            ea_ap = cand.ap[:, 0:512].bitcast(BF16)
            eb_ap = cand.ap[:, 512:1024].bitcast(BF16)
            kb.tt("dve", v3(ea_ap, 8), eatv, pst[:, 16:24].unsqueeze(2).to_broadcast([128, 8, 128]), ALU.mult, [eat, pst], [cand])
            kb.tt("dve", eatv, scv[:, :, 1, :], v16q[:, :, 1, 0:1].to_broadcast([128, 8, 128]), ALU.subtract, [sc, v16], [eat])
            kb.act(eb_ap, eat_ap, AF.Exp, [eat], [cand])
            kb.dma("sp", st_ea[blk], ea_ap, "st_st", [cand], ())
            kb.dma("sp", st_eb[blk], eb_ap, "st_st", [cand], ())
        fw.barrier()
        ar.ptr = const_end

        ust = [ar.bf16(D, "ust0"), ar.bf16(D, "ust1")]
        ugr = [ar.bf16(KC * 512, "ugr0"), ar.bf16(KC * 512, "ugr1")]
        for et in range(128):
            g, sub = divmod(et, 4)
            us = ust[et % 2]
            ug = ugr[g % 2]
            ugv = v3(ug.ap, KC)
            kb.dma("pool", us.ap, peer_u[et * 128:(et + 1) * 128, :], "ust%d" % (et % 2), (), [us])
            bk = 2 * (et % 2)
            pv = v3(pbf(bk, 2), KC)
            kb.tr([(pv[:, k, :], us[:, k * 128:(k + 1) * 128], ident_b.ap) for k in range(KC)], [us, ident_b], PB[bk:bk + 2])
            if et % 2 == 0:
                kb.copy("act", ugv[:, :, sub * 128:(sub + 1) * 128], pv, PB[bk:bk + 2], [ug])
            else:
                kb.copy("dve", ugv[:, :, sub * 128:(sub + 1) * 128], pv, PB[bk:bk + 2], [ug])
            if sub == 3:
                kb.dma("sp", UT_d[g], ug.ap, "ut_st%d" % (g % 2), [ug], ())
        fw.barrier()
        ar.ptr = const_end

        ar.ptr = const_end_p6
        ut_raw = [ar.f32(KC * 256, "ut0"), ar.f32(KC * 256, "ut1")]
        vt_raw = [ar.f32(2 * D, "vt0"), ar.f32(2 * D, "vt1")]
        for t_ in ut_raw + vt_raw:
            t_.ap, t_.name = t_.ap.bitcast(BF16), t_.ap
        ut, vt = ut_raw, vt_raw
        xq = ar.bf16(TS * D, "xq")
        xqv = xq.ap.rearrange("p (t k m) -> p t k m", t=TS, k=KC)
        sn = ar.f32(TS * 1024, "sn")
        s2t = ar.f32(TS * 1024, "s2t")
        eaT = ar.bf16(TS * 1024, "eaT")
        ebT = ar.bf16(TS * 1024, "ebT")
        snv = sn.ap.rearrange("p (t h k) -> p t h k", t=TS, h=8)
        s2v = s2t.ap.rearrange("p (t h k) -> p t h k", t=TS, h=8)
        eav = eaT.ap.rearrange("p (t h k) -> p t h k", t=TS, h=8)
        ebv = ebT.ap.rearrange("p (t h k) -> p t h k", t=TS, h=8)
        acc = ar.f32(TS * D, "acc")
        accv = v3(acc.ap, TS)
        actg = [ar.bf16(512, "actg%d" % i) for i in range(2)]
        Mk = [ar.bf16(4096, "Mk%d" % i) for i in range(3)]
        Gs = [ar.bf16(512, "G0"), ar.bf16(512, "G1")]
        HmT = [ar.bf16(512, "HmT0"), ar.bf16(512, "HmT1")]
        fst = ar.f32(4, "fst")
        fng = vt[1]
        fng_ap = vt[1].name[:, 0:D]
        h2 = ut[0]
        h2_ap = ut[0].name[:, 0:D]
        junk = Mk[0]
        junk_ap = Mk[0].ap[:, 0:D]
        for stile in range(NB // TS):
            b0 = stile * TS
            for tb in range(TS):
                kb.dma("sp", xq[:, tb * D:(tb + 1) * D], xn2T_d[b0 + tb], "xq", (), [xq])
                kb.dma("sp", sn[:, tb * 1024:(tb + 1) * 1024], st_nega[b0 + tb], "sn", (), [sn])
                kb.dma("sp", s2t[:, tb * 1024:(tb + 1) * 1024], st_s2[b0 + tb], "s2t", (), [s2t])
                kb.dma("sp", eaT[:, tb * 1024:(tb + 1) * 1024], st_ea[b0 + tb], "eaT", (), [eaT])
                kb.dma("sp", ebT[:, tb * 1024:(tb + 1) * 1024], st_eb[b0 + tb], "ebT", (), [ebT])
            kb.memset("pool", acc.ap, 0.0, [acc])
            its = [(g, tb) for g in range(32) for tb in range(TS)]
            N = len(its)
            wts = {}

            def load_u(g):
                u = ut[g % 2]
                kb.dma("sp", u.ap, UT_d[g], "ut%d" % (g % 2), (), [u])

            def load_v(g):
                vv = vt[g % 2]
                kb.dma("sp", v3(vv.ap, 4), V_d[g * 512:(g + 1) * 512, :].rearrange("(i e) n -> e i n", e=128),
                       "vt%d" % (g % 2), (), [vv])

            def stA(s_):
                g, tb = its[s_]
                if tb == 0:
                    load_u(g)
                mkv = Mk[s_ % 3].ap.rearrange("p (h i j) -> p h i j", h=8, i=4)
                kb.tt("dve", mkv, s2v[:, tb].unsqueeze(2).to_broadcast([128, 8, 4, 128]),
                      snv[:, tb, :, 4 * g:4 * g + 4].unsqueeze(3).to_broadcast([128, 8, 4, 128]), ALU.is_ge, [s2t, sn], [Mk[s_ % 3]])

            def stB(s_):
                g, tb = its[s_]
                mk = Mk[s_ % 3]
                mkv = mk.ap.rearrange("p (h i j) -> p h i j", h=8, i=4)
                kb.tt("dve", mkv, mkv, ebv[:, tb].unsqueeze(2).to_broadcast([128, 8, 4, 128]), ALU.mult, [mk, ebT], [mk])
                kb.tt("dve", mkv, mkv, eav[:, tb, :, 4 * g:4 * g + 4].unsqueeze(3).to_broadcast([128, 8, 4, 128]), ALU.mult,
                      [mk, eaT], [mk])

            def stM(s_):
                g, tb = its[s_]
                uv = v3(ut[g % 2].ap, KC)
                p = s_ % 2
                kb.mm([(pf(p), xqv[:, tb, k, :], uv[:, k, :], k == 0, k == KC - 1) for k in range(KC)], [xq, ut[g % 2]], [PB[p]])
                kb.act(actg[p].ap, pf(p), AF.Gelu, [PB[p]], [actg[p]])

            def stC(s_):
                mk = Mk[s_ % 3]
                gs = Gs[s_ % 2]
                kb.tt("dve", mk[:, 0:2048], mk[:, 0:2048], mk[:, 2048:4096], ALU.add, [mk], [mk])
                kb.tt("dve", mk[:, 0:1024], mk[:, 0:1024], mk[:, 1024:2048], ALU.add, [mk], [mk])
                kb.tt("dve", gs.ap, mk[:, 0:512], mk[:, 512:1024], ALU.add, [mk], [gs])
                kb.tt("dve", gs.ap, gs.ap, actg[s_ % 2].ap, ALU.mult, [gs, actg[s_ % 2]], [gs])

            def stD(s_):
                g, tb = its[s_]
                p = s_ % 2
                gs, hT = Gs[p], HmT[p]
                vvv = v3(vt[g % 2].ap, 4)
                kb.tr([(pbf(2 + p)[:, i * 128:(i + 1) * 128], gs[:, i * 128:(i + 1) * 128], ident_b.ap) for i in range(4)],
                      [gs, ident_b], [PB[2 + p]])
                kb.copy("act", hT.ap, pbf(2 + p)[:, 0:512], [PB[2 + p]], [hT])
                sp_ = []
                for fq in range(4):
                    sp_.append((pf(4 + fq), ident_f.ap, accv[:, tb, fq * 512:(fq + 1) * 512], True, False))
                    for i in range(4):
                        sp_.append((pf(4 + fq), hT[:, i * 128:(i + 1) * 128], vvv[:, i, fq * 512:(fq + 1) * 512], False, i == 3))
                kb.mm(sp_, [hT, vt[g % 2], acc, ident_f], PB[4:8])
                kb.copy("act", accv[:, tb, :], pf(4, 4), PB[4:8], [acc])

            load_v(0)
            load_v(1)
            for s_ in range(N + 4):
                if 0 <= s_ - 3 < N:
                    stC(s_ - 3)
                if s_ < N:
                    stA(s_)
                if 0 <= s_ - 1 < N:
                    stB(s_ - 1)
                if 0 <= s_ - 4 < N:
                    stD(s_ - 4)
                    g_, tb_ = its[s_ - 4]
                    if tb_ == TS - 1 and g_ + 2 < 32:
                        load_v(g_ + 2)
                if 0 <= s_ - 2 < N:
                    stM(s_ - 2)
            kb.dma("sp", fng_ap, final_g.partition_broadcast(128), "fng", (), [fng])
            for tb in range(TS):
                r0 = (b0 + tb) * 128
                kb.dma("sp", h2_ap, h1_d[r0:r0 + 128, :], "h2_ld", (), [h2])
                kb.tt("dve", accv[:, tb, :], accv[:, tb, :], g2_bc.ap, ALU.mult, [acc, g2_bc], [acc])
                kb.tt("dve", h2_ap, h2_ap, accv[:, tb, :], ALU.add, [h2, acc], [h2])
                kb.act(junk_ap, h2_ap, AF.Square, [h2], [junk, fst], accum_out=fst[:, 0:1])
                kb.ts("dve", fst[:, 1:2], fst[:, 0:1], 1.0 / D, EPS, ALU.mult, ALU.add, [fst], [fst])
                kb.act(fst[:, 1:2], fst[:, 1:2], AF.Sqrt, [fst], [fst])
                kb.recip(fst[:, 2:3], fst[:, 1:2], [fst], [fst])
                kb.stt("dve", h2_ap, h2_ap, fst[:, 2:3], fng_ap, ALU.mult, ALU.mult, [h2, fst, fng], [h2])
                kb.dma("sp", y[r0:r0 + 128, :], h2_ap, "y_out", [h2], ())
        fw.barrier()
        fw.emit(st)
    return nc


_PROG_CACHE = {}


def make_in_maps(cfg, inp):
    NB, T = cfg.NB, cfg.TPC
    f32 = lambda a: np.ascontiguousarray(np.asarray(a, dtype=np.float32))
    shared = dict(
        cctx=f32(inp["c_ctx"]), norm1_g=f32(inp["norm1_g"][0]), norm2_g=f32(inp["norm2_g"][0]),
        w_mod=f32(inp["w_mod"][0]), b_mod=f32(inp["b_mod"][0]), w_in=f32(inp["w_in"][0]),
        w_gate_up=f32(inp["w_gate_up"][0]), b_gate=f32(inp["b_gate"][0]), gla_norm_g=f32(inp["gla_norm_g"][0]),
        cmlp_ln_g=f32(inp["cmlp_ln_g"][0]), cmlp_ln_b=f32(inp["cmlp_ln_b"][0]), w_spatial=f32(inp["w_spatial"][0]),
        b_spatial=f32(inp["b_spatial"][0]), w_out=f32(inp["w_out"][0]), peer_wq=f32(inp["peer_wq"][0]),
        peer_sk=f32(inp["peer_sub_keys"][0]), peer_u=f32(inp["peer_u"][0]), peer_v=f32(inp["peer_v"][0]),
        final_g=f32(inp["final_norm_g"]),
    )
    x = np.asarray(inp["x"], dtype=np.float32)
    ctx = np.asarray(inp["ctx"], dtype=np.float32)
    c = np.asarray(inp["c"], dtype=np.float32)
    maps = []
    for core in range(8):
        b, seg = divmod(core, 4)
        xb = x[b]
        slots = [(s, 1.0) for s in range(seg)] + [(s, 0.0) for s in range(3, seg, -1)]
        flags = np.zeros((128, 8), np.float32)
        parts = []
        for j, (s, f) in enumerate(slots):
            blocks = xb[s * T:(s + 1) * T].reshape(NB, 128, D)
            if f == 0.0:
                blocks = blocks[::-1]
            parts.append(blocks.reshape(T, D))
            flags[:, j] = f
        flags[:, 3] = 1.0
        m = dict(shared)
        m.update(xm=f32(xb[seg * T:(seg + 1) * T]), xo=f32(np.concatenate(parts, 0)), ctxb=f32(ctx[b]),
                 cvec=f32(c[b]), flags=flags)
        maps.append(m)
    return maps


def kernel(**inp):
    seq = int(np.asarray(inp["x"]).shape[1])
    cfg = Cfg(seq, int(np.asarray(inp["ctx"]).shape[1]))
    if seq not in _PROG_CACHE:
        _PROG_CACHE[seq] = build_program(cfg)
    nc = _PROG_CACHE[seq]
    maps = make_in_maps(cfg, inp)
    res = run_bass_kernel_spmd(nc, maps, core_ids=list(range(8)))
    out = np.empty((2, seq, D), np.float32)
    for core in range(8):
        b, seg = divmod(core, 4)
        out[b, seg * cfg.TPC:(seg + 1) * cfg.TPC] = res.results[core]["y"]
    return out
```
